# Optimizing a Trainium2 kernel written in Bass

```python
import math
import jax, jax.numpy as jnp
from jax import lax
import numpy as np


D_MODEL = 2048
BATCH = 4
SEQ = 8192
DEPTH = 2

HEAD_DIM = 64
GROUP_HEADS = 8
GROUP_WIDTH = GROUP_HEADS * HEAD_DIM
N_GROUPS = 4
MIX_WIDTH = N_GROUPS * GROUP_WIDTH
D_FF = 4 * D_MODEL
Q_BLOCK = 128
EPS = 1e-6
NEG_INF = -1e30
BIG = 1e9

MLA_Q_RANK = 384
MLA_KV_RANK = 128
MLA_NOPE = 64
MLA_ROPE = 32
MLA_V = HEAD_DIM
ROPE_THETA = 10000.0

NSA_KV_HEADS = 2
NSA_KV_WIDTH = NSA_KV_HEADS * HEAD_DIM
NSA_CMP_LEN = 32
NSA_CMP_STRIDE = 16
NSA_SEL_LEN = 64
NSA_TOP_N = 8
NSA_WINDOW = 256

SWA_KV_HEADS = 2
SWA_KV_WIDTH = SWA_KV_HEADS * HEAD_DIM
SWA_WINDOW = 128

REL_BUCKETS = 32
REL_MAX_DIST = 128
REL_HEADS = 2 * GROUP_HEADS

IN_SPLITS = (
    MLA_Q_RANK, MLA_KV_RANK, MLA_ROPE,
    GROUP_WIDTH, GROUP_WIDTH, GROUP_WIDTH, GROUP_HEADS,
    GROUP_WIDTH, NSA_KV_WIDTH, NSA_KV_WIDTH, NSA_KV_WIDTH,
    NSA_KV_WIDTH, NSA_KV_WIDTH, NSA_KV_WIDTH, 3 * GROUP_HEADS,
    GROUP_WIDTH, SWA_KV_WIDTH, SWA_KV_WIDTH,
)
IN_COLS = sum(IN_SPLITS)

kernel_name = 'hybrid_mla_fox_nsa_swa_block'


def rmsnorm(x, g):
    xf = x.astype(jnp.float32)
    y = xf * lax.rsqrt(jnp.mean(xf * xf, axis=-1, keepdims=True) + EPS)
    return (y * g.astype(jnp.float32)).astype(x.dtype)


def split_heads(x, h):
    b, t, _ = x.shape
    return x.reshape(b, t, h, -1).transpose(0, 2, 1, 3)


def merge_heads(x):
    b, h, t, d = x.shape
    return x.transpose(0, 2, 1, 3).reshape(b, t, h * d)


def rope(x, pos):
    d = x.shape[-1]
    inv = ROPE_THETA ** (-jnp.arange(0, d, 2, dtype=jnp.float32) / d)
    ang = pos.astype(jnp.float32)[:, None] * inv[None, :]
    cos, sin = jnp.cos(ang), jnp.sin(ang)
    x1 = x[..., : d // 2].astype(jnp.float32)
    x2 = x[..., d // 2:].astype(jnp.float32)
    return jnp.concatenate([x1 * cos - x2 * sin, x1 * sin + x2 * cos], axis=-1).astype(x.dtype)


def t5_bucket(dist):
    max_exact = REL_BUCKETS // 2
    d = jnp.maximum(dist, 0)
    large = max_exact + (jnp.log(jnp.maximum(d, 1).astype(jnp.float32) / max_exact)
                         / math.log(REL_MAX_DIST / max_exact)
                         * (REL_BUCKETS - max_exact)).astype(jnp.int32)
    large = jnp.minimum(large, REL_BUCKETS - 1)
    return jnp.where(d < max_exact, d, large)


def dense_causal_attention(q, k, v, scale, fcum=None):
    b, h, t, dk = q.shape
    nb = t // Q_BLOCK
    kpos = jnp.arange(t)
    qb = q.reshape(b, h, nb, Q_BLOCK, dk).transpose(2, 0, 1, 3, 4)
    xs = (jnp.arange(nb), qb)
    if fcum is not None:
        xs = xs + (fcum.reshape(b, h, nb, Q_BLOCK).transpose(2, 0, 1, 3),)

    def one_block(args):
        i, qi = args[0], args[1]
        s = jnp.einsum('bhqd,bhkd->bhqk', qi, k, preferred_element_type=jnp.float32) * scale
        if fcum is not None:
            s = s + args[2][..., :, None] - fcum[:, :, None, :]
        qpos = i * Q_BLOCK + jnp.arange(Q_BLOCK)
        s = jnp.where(kpos[None, :] <= qpos[:, None], s, NEG_INF)
        p = jax.nn.softmax(s, axis=-1)
        return jnp.einsum('bhqk,bhkd->bhqd', p.astype(v.dtype), v)

    o = lax.map(one_block, xs)
    return o.transpose(1, 2, 0, 3, 4).reshape(b, h, t, v.shape[-1])


def banded_attention(q, k, v, window, tbl, sinks=None):
    b, g, r, t, d = q.shape
    nb, nw = t // Q_BLOCK, window // Q_BLOCK

    def blocks(a):
        ap = jnp.pad(a, ((0, 0), (0, 0), (window, 0), (0, 0))).reshape(b, g, nb + nw, Q_BLOCK, d)
        return jnp.concatenate([ap[:, :, i:i + nb] for i in range(nw + 1)], axis=3)

    kb, vb = blocks(k), blocks(v)
    qb = q.reshape(b, g, r, nb, Q_BLOCK, d)
    s = jnp.einsum('bgrnqd,bgnkd->bgrnqk', qb, kb, preferred_element_type=jnp.float32) * d ** -0.5
    kw = (nw + 1) * Q_BLOCK
    dist = (jnp.arange(Q_BLOCK)[:, None] + window) - jnp.arange(kw)[None, :]
    key_real = jnp.arange(nb)[:, None, None] * Q_BLOCK - window + jnp.arange(kw)[None, None, :]
    mask = (dist >= 0) & (dist < window) & (key_real >= 0)
    s = s + tbl[:, :, t5_bucket(dist)][:, :, None].astype(jnp.float32)
    s = jnp.where(mask, s, NEG_INF)
    if sinks is not None:
        sink = jnp.broadcast_to(sinks.astype(jnp.float32)[None, :, :, None, None, None], s.shape[:-1] + (1,))
        p = jax.nn.softmax(jnp.concatenate([s, sink], axis=-1), axis=-1)[..., :-1]
    else:
        p = jax.nn.softmax(s, axis=-1)
    o = jnp.einsum('bgrnqk,bgnkd->bgrnqd', p.astype(v.dtype), vb)
    return o.reshape(b, g, r, t, d)


def mla_mixer(c_q, c_kv, k_pe, q_norm, w_uq, kv_norm, w_ukv, pos):
    b, t, _ = c_q.shape
    q = split_heads(rmsnorm(c_q, q_norm) @ w_uq, GROUP_HEADS)
    q = jnp.concatenate([q[..., :MLA_NOPE], rope(q[..., MLA_NOPE:], pos)], axis=-1)
    kv = split_heads(rmsnorm(c_kv, kv_norm) @ w_ukv, GROUP_HEADS)
    k_pe = jnp.broadcast_to(rope(k_pe[:, None], pos), (b, GROUP_HEADS, t, MLA_ROPE))
    k = jnp.concatenate([kv[..., :MLA_NOPE], k_pe], axis=-1)
    v = kv[..., MLA_NOPE:]
    o = dense_causal_attention(q, k, v, (MLA_NOPE + MLA_ROPE) ** -0.5)
    return merge_heads(o)


def fox_mixer(q, k, v, f_logit, b_f):
    log_f = jax.nn.log_sigmoid(f_logit.astype(jnp.float32) + b_f.astype(jnp.float32))
    fcum = jnp.cumsum(log_f, axis=1).transpose(0, 2, 1)
    o = dense_causal_attention(split_heads(q, GROUP_HEADS), split_heads(k, GROUP_HEADS),
                               split_heads(v, GROUP_HEADS), HEAD_DIM ** -0.5, fcum)
    return merge_heads(o)


def nsa_compress(kv, pos_emb, w1, w2):
    b, g, t, d = kv.shape
    nc = (t - NSA_CMP_LEN) // NSA_CMP_STRIDE + 1
    idx = jnp.arange(nc)[:, None] * NSA_CMP_STRIDE + jnp.arange(NSA_CMP_LEN)[None, :]
    blk = kv[:, :, idx] + pos_emb
    flat = blk.reshape(b, g, nc, NSA_CMP_LEN * d)
    return jax.nn.gelu(flat @ w1) @ w2


def nsa_cmp_sel(qg, k_cmp, v_cmp, ks, vs, tbl):
    b, g, r, t, d = qg.shape
    nb = t // Q_BLOCK
    nc = k_cmp.shape[2]
    ns = t // NSA_SEL_LEN
    n_top = min(NSA_TOP_N, ns)
    scale = d ** -0.5
    cmp_end = jnp.arange(nc) * NSA_CMP_STRIDE + NSA_CMP_LEN - 1
    ci = jnp.arange(nc)[:, None]
    sj = jnp.arange(ns)[None, :]
    overlap = ((ci * NSA_CMP_STRIDE + NSA_CMP_LEN - 1 >= sj * NSA_SEL_LEN)
               & (ci * NSA_CMP_STRIDE <= sj * NSA_SEL_LEN + NSA_SEL_LEN - 1)).astype(jnp.float32)
    ks_blk = ks.reshape(b, g, ns, NSA_SEL_LEN, d)
    vs_blk = vs.reshape(b, g, ns, NSA_SEL_LEN, d)
    bi = jnp.arange(b)[:, None, None, None]
    gi = jnp.arange(g)[None, :, None, None]
    g_ix = jnp.arange(g)[None, :, None, None, None]
    r_ix = jnp.arange(r)[None, None, :, None, None]
    jsel = jnp.arange(ns)
    qb = qg.reshape(b, g, r, nb, Q_BLOCK, d).transpose(3, 0, 1, 2, 4, 5)

    def one_block(args):
        n, qi = args
        qpos = n * Q_BLOCK + jnp.arange(Q_BLOCK)
        sc = jnp.einsum('bgrqd,bgcd->bgrqc', qi, k_cmp, preferred_element_type=jnp.float32) * scale
        dist_c = qpos[:, None] - cmp_end[None, :]
        valid_c = dist_c >= 0
        sc = sc + tbl[:, :, t5_bucket(dist_c)].astype(jnp.float32)
        sc = jnp.where(valid_c, sc, NEG_INF)
        pc = jnp.where(valid_c, jax.nn.softmax(sc, axis=-1), 0.0)
        o_cmp = jnp.einsum('bgrqc,bgcd->bgrqd', pc.astype(v_cmp.dtype), v_cmp)
        imp = jnp.einsum('bgrqc,cs->bgqs', pc, overlap)
        cur = (qpos // NSA_SEL_LEN)[:, None]
        forced = (jsel[None, :] == 0) | (jsel[None, :] == cur) | (jsel[None, :] == cur - 1)
        score = jnp.where(jsel[None, :] <= cur, jnp.where(forced, BIG, imp), NEG_INF)
        _, sel = lax.top_k(score, n_top)
        k_sel = ks_blk[bi, gi, sel].reshape(b, g, Q_BLOCK, n_top * NSA_SEL_LEN, d)
        v_sel = vs_blk[bi, gi, sel].reshape(b, g, Q_BLOCK, n_top * NSA_SEL_LEN, d)
        kpos = (sel[..., None] * NSA_SEL_LEN + jnp.arange(NSA_SEL_LEN)).reshape(b, g, Q_BLOCK, n_top * NSA_SEL_LEN)
        dist_s = qpos[None, None, :, None] - kpos
        ss = jnp.einsum('bgrqd,bgqkd->bgrqk', qi, k_sel, preferred_element_type=jnp.float32) * scale
        ss = ss + tbl[g_ix, r_ix, t5_bucket(dist_s)[:, :, None]].astype(jnp.float32)
        ss = jnp.where(dist_s[:, :, None] >= 0, ss, NEG_INF)
        ps = jax.nn.softmax(ss, axis=-1)
        o_sel = jnp.einsum('bgrqk,bgqkd->bgrqd', ps.astype(v_sel.dtype), v_sel)
        return o_cmp, o_sel

    o_cmp, o_sel = lax.map(one_block, (jnp.arange(nb), qb))
    fix = lambda o: o.transpose(1, 2, 3, 0, 4, 5).reshape(b, g, r, t, d)
    return fix(o_cmp), fix(o_sel)


def nsa_mixer(q, kc, vc, ksl, vsl, kwn, vwn, gate, cmp_pos, cmp_w1, cmp_w2, tbl):
    b, t, _ = q.shape
    g, r = NSA_KV_HEADS, GROUP_HEADS // NSA_KV_HEADS
    qg = split_heads(q, GROUP_HEADS).reshape(b, g, r, t, HEAD_DIM)
    kc, vc = split_heads(kc, g), split_heads(vc, g)
    ksl, vsl = split_heads(ksl, g), split_heads(vsl, g)
    kwn, vwn = split_heads(kwn, g), split_heads(vwn, g)
    k_cmp = nsa_compress(kc, cmp_pos[0], cmp_w1[0], cmp_w2[0])
    v_cmp = nsa_compress(vc, cmp_pos[1], cmp_w1[1], cmp_w2[1])
    o_cmp, o_sel = nsa_cmp_sel(qg, k_cmp, v_cmp, ksl, vsl, tbl)
    o_win = banded_attention(qg, kwn, vwn, NSA_WINDOW, tbl)
    gts = jax.nn.sigmoid(gate).reshape(b, t, g, r, 3).transpose(0, 2, 3, 1, 4)
    o = gts[..., 0:1] * o_cmp + gts[..., 1:2] * o_sel + gts[..., 2:3] * o_win
    return merge_heads(o.reshape(b, GROUP_HEADS, t, HEAD_DIM))


def swa_mixer(q, k, v, sinks, tbl):
    b, t, _ = q.shape
    g, r = SWA_KV_HEADS, GROUP_HEADS // SWA_KV_HEADS
    qg = split_heads(q, GROUP_HEADS).reshape(b, g, r, t, HEAD_DIM)
    o = banded_attention(qg, split_heads(k, g), split_heads(v, g), SWA_WINDOW, tbl, sinks.reshape(g, r))
    return merge_heads(o.reshape(b, GROUP_HEADS, t, HEAD_DIM))


def setup_inputs(seed: int = 0) -> dict:
    key = jax.random.key(seed)
    ks = jax.random.split(key, 20)
    nrm = lambda k, shape, fan_in: jax.random.normal(k, shape, jnp.float32) * fan_in ** -0.5
    gain = lambda k, shape: 1.0 + 0.05 * jax.random.normal(k, shape, jnp.float32)
    L = DEPTH
    return {
        'x': jax.random.normal(ks[0], (BATCH, SEQ, D_MODEL), jnp.float32),
        'norm_attn': gain(ks[1], (L, D_MODEL)),
        'w_in': nrm(ks[2], (L, D_MODEL, IN_COLS), D_MODEL),
        'mla_q_norm': gain(ks[3], (L, MLA_Q_RANK)),
        'mla_w_uq': nrm(ks[4], (L, MLA_Q_RANK, GROUP_HEADS * (MLA_NOPE + MLA_ROPE)), MLA_Q_RANK),
        'mla_kv_norm': gain(ks[5], (L, MLA_KV_RANK)),
        'mla_w_ukv': nrm(ks[6], (L, MLA_KV_RANK, GROUP_HEADS * (MLA_NOPE + MLA_V)), MLA_KV_RANK),
        'fox_b_f': 3.0 + 0.5 * jax.random.normal(ks[7], (L, GROUP_HEADS), jnp.float32),
        'nsa_cmp_pos': 0.1 * jax.random.normal(ks[8], (L, 2, NSA_CMP_LEN, HEAD_DIM), jnp.float32),
        'nsa_cmp_w1': nrm(ks[9], (L, 2, NSA_CMP_LEN * HEAD_DIM, HEAD_DIM), NSA_CMP_LEN * HEAD_DIM),
        'nsa_cmp_w2': nrm(ks[10], (L, 2, HEAD_DIM, HEAD_DIM), HEAD_DIM),
        'swa_sinks': 0.5 * jax.random.normal(ks[11], (L, GROUP_HEADS), jnp.float32),
        'group_norm': gain(ks[12], (L, MIX_WIDTH)),
        'w_out': nrm(ks[13], (L, MIX_WIDTH, D_MODEL), MIX_WIDTH),
        'norm_mlp': gain(ks[14], (L, D_MODEL)),
        'w_up': nrm(ks[15], (L, D_MODEL, D_FF), D_MODEL),
        'w_down': nrm(ks[16], (L, D_FF, D_MODEL), D_FF),
        'rel_bias': 0.5 * jax.random.normal(ks[17], (REL_BUCKETS, REL_HEADS), jnp.float32),
        'final_norm': gain(ks[18], (D_MODEL,)),
    }


def reference(x, norm_attn, w_in, mla_q_norm, mla_w_uq, mla_kv_norm, mla_w_ukv, fox_b_f,
              nsa_cmp_pos, nsa_cmp_w1, nsa_cmp_w2, swa_sinks, group_norm, w_out,
              norm_mlp, w_up, w_down, rel_bias, final_norm):
    b, t, _ = x.shape
    pos = jnp.arange(t)
    r_nsa = GROUP_HEADS // NSA_KV_HEADS
    r_swa = GROUP_HEADS // SWA_KV_HEADS
    tbl_nsa = rel_bias[:, :GROUP_HEADS].T.reshape(NSA_KV_HEADS, r_nsa, REL_BUCKETS)
    tbl_swa = rel_bias[:, GROUP_HEADS:].T.reshape(SWA_KV_HEADS, r_swa, REL_BUCKETS)
    offsets = [int(o) for o in np.cumsum(IN_SPLITS)[:-1]]
    h = x
    for l in range(DEPTH):
        u = rmsnorm(h, norm_attn[l])
        p = jnp.split(u @ w_in[l], offsets, axis=-1)
        o_mla = mla_mixer(p[0], p[1], p[2], mla_q_norm[l], mla_w_uq[l], mla_kv_norm[l], mla_w_ukv[l], pos)
        o_fox = fox_mixer(p[3], p[4], p[5], p[6], fox_b_f[l])
        o_nsa = nsa_mixer(p[7], p[8], p[9], p[10], p[11], p[12], p[13], p[14],
                          nsa_cmp_pos[l], nsa_cmp_w1[l], nsa_cmp_w2[l], tbl_nsa)
        o_swa = swa_mixer(p[15], p[16], p[17], swa_sinks[l], tbl_swa)
        o = jnp.concatenate([o_mla, o_fox, o_nsa, o_swa], axis=-1).reshape(b, t, N_GROUPS, GROUP_WIDTH)
        o = rmsnorm(o, group_norm[l].reshape(N_GROUPS, GROUP_WIDTH)).reshape(b, t, MIX_WIDTH)
        h = h + o @ w_out[l]
        m = rmsnorm(h, norm_mlp[l]) @ w_up[l]
        h = h + jnp.square(jax.nn.relu(m)) @ w_down[l]
    return rmsnorm(h, final_norm)
```

```python
import contextlib
import math
import numpy as np
import ml_dtypes
import concourse.bass as bass
import concourse.mybir as mybir
from concourse.bass_utils import run_bass_kernel_spmd

F32 = mybir.dt.float32
BF16 = mybir.dt.bfloat16
I32 = mybir.dt.int32
AF = mybir.ActivationFunctionType
ALU = mybir.AluOpType
AX = mybir.AxisListType
ENGS = ("pe", "act", "dve", "pool", "sp")

D = 2048
DEPTH = 2
HD = 64
NH = 8
DFF = 8192
EPS = 1e-6
NFM = 2816
NTM = 1536
FM_FOXQ, FM_FOXK, FM_NSAQ, FM_SWAQ = 0, 512, 1024, 1536
FM_KC, FM_VC, FM_KSL, FM_KWN, FM_SWAK, FM_F = 2048, 2176, 2304, 2432, 2560, 2688
TM_CQ, TM_CKV, TM_KPE, TM_FOXV, TM_VSL, TM_VWN, TM_GATE, TM_SWAV = 0, 384, 512, 544, 1056, 1184, 1312, 1336


class Prog:
    ENG_SWITCH = 8000
    DMA_SWITCH = 12000
    LIMIT = 30000

    def __init__(self, nc):
        self.nc = nc
        self.stack = contextlib.ExitStack()
        self.ops = {e: [] for e in ENGS}
        self.count = {e: 0 for e in ENGS}
        self.gen = {e: 0 for e in ENGS}
        self.dgen = {}
        self.seen = {e: {} for e in ENGS}
        self.last_w = {}
        self.readers = {}
        self.dma_cnt = {}
        self.sems = {}
        self.n_inst = 0
        self.rr = 0
        self.rr_name = 0
        self.keymap = {}
        self.pe_entries = {}
        self.pe_marks = []
        self.maxcount = 0

    def sem(self, key):
        if key not in self.sems:
            self.sems[key] = self.stack.enter_context(
                self.nc.semaphore("s%d" % len(self.sems)))
        return self.sems[key]

    def sbuf(self, name, shape, dtype, stack=None):
        st = stack or self.stack
        self.rr_name += 1
        return st.enter_context(self.nc.sbuf_tensor("%s_u%d" % (name, self.rr_name), list(shape), dtype))

    def psum(self, name, shape, dtype, stack=None):
        st = stack or self.stack
        self.rr_name += 1
        return st.enter_context(self.nc.psum_tensor("%s_u%d" % (name, self.rr_name), list(shape), dtype))

    def _pe_value(self, idx):
        if self.pe_marks and self.pe_marks[-1][0] >= idx:
            lo, hi = 0, len(self.pe_marks) - 1
            while lo < hi:
                mid = (lo + hi) // 2
                if self.pe_marks[mid][0] >= idx:
                    hi = mid
                else:
                    lo = mid + 1
            return self.pe_marks[lo][1]
        val = len(self.pe_marks) + 1
        self.pe_marks.append((idx, val))
        self.pe_entries[idx][3] = True
        self.maxcount = max(self.maxcount, val)
        return val

    def _deps(self, eng, reads, writes):
        deps = {}

        def add(d):
            if d is None:
                return
            k, v = d
            if deps.get(k, 0) < v:
                deps[k] = v
        for b in reads:
            add(self.last_w.get(b))
        for b in writes:
            add(self.last_w.get(b))
            for k, v in self.readers.get(b, {}).items():
                add((k, v))
        waits = []
        for k, v in deps.items():
            if k[0] == "eng" and k[1] == "pe":
                if eng == "pe":
                    continue
                v = self._pe_value(v)
            if self.seen[eng].get(k, 0) < v:
                self.seen[eng][k] = v
                waits.append((k, v))
        return waits

    def _commit(self, me, reads, writes):
        for b in writes:
            self.last_w[b] = me
            self.readers[b] = {}
        for b in reads:
            r = self.readers.setdefault(b, {})
            if r.get(me[0], 0) < me[1]:
                r[me[0]] = me[1]

    def op(self, eng, fn, reads=(), writes=()):
        waits = self._deps(eng, reads, writes)
        self.count[eng] += 1
        key = ("eng", eng, self.gen[eng])
        self.sem(key)
        me = (key, self.count[eng])
        rec = ["op", fn, waits, eng != "pe", key]
        if eng == "pe":
            self.pe_entries[self.count[eng]] = rec
        else:
            self.maxcount = max(self.maxcount, self.count[eng])
        self.ops[eng].append(rec)
        self._commit(me, reads, writes)
        self.n_inst += 1 + len(waits)

    def dma(self, eng, out, in_, reads=(), writes=(), semkey=None, **kw):
        if eng is None:
            eng = ("sp", "pool")[self.rr % 2]
            self.rr += 1
        waits = self._deps(eng, reads, writes)
        lkey = semkey if semkey is not None else (writes[0] if writes else reads[0])
        if lkey not in self.keymap:
            self.keymap[lkey] = len(self.keymap)
        idx = self.keymap[lkey]
        key = ("dma", idx, self.dgen.get(idx, 0))
        self.sem(key)
        self.dma_cnt[key] = self.dma_cnt.get(key, 0) + 1
        self.maxcount = max(self.maxcount, 16 * self.dma_cnt[key])
        me = (key, 16 * self.dma_cnt[key])
        self.ops[eng].append(["dma", (out, in_, kw), waits, None, key])
        self._commit(me, reads, writes)
        self.n_inst += 1 + len(waits)

    def barrier(self):
        allk = {}
        for e in ENGS:
            if self.count[e]:
                allk[("eng", e, self.gen[e])] = self._pe_value(self.count[e]) if e == "pe" else self.count[e]
        for k, c in self.dma_cnt.items():
            allk[k] = 16 * c
        assert max(allk.values() or [0]) < self.LIMIT, ("semaphore count too large", max(allk.values()))
        for e in ENGS:
            waits = []
            for k, v in allk.items():
                if self.seen[e].get(k, 0) < v:
                    self.seen[e][k] = v
                    waits.append((k, v))
            if waits:
                self.ops[e].append(["wait", None, waits, None, None])
                self.n_inst += len(waits)
        self.last_w = {}
        self.readers = {}

    def max_sem_count(self):
        return self.maxcount

    def emit(self):
        nc = self.nc
        with nc.Block() as block:
            def body(ename):
                def run(eng):
                    for kind, payload, waits, flag, key in self.ops[ename]:
                        for k, v in waits:
                            eng.wait_ge(self.sems[k], v)
                        if kind == "op":
                            ins = payload(eng)
                            if flag:
                                ins.then_inc(self.sems[key], 1)
                        elif kind == "dma":
                            out, in_, kw = payload
                            eng.dma_start(out=out, in_=in_, **kw).then_inc(self.sems[key], 16)
                return run
            block.tensor(body("pe"))
            block.scalar(body("act"))
            block.vector(body("dve"))
            block.gpsimd(body("pool"))
            block.sync(body("sp"))
        self.ops = {e: [] for e in ENGS}
        for e in ENGS:
            val = len(self.pe_marks) if e == "pe" else self.count[e]
            if val > self.ENG_SWITCH:
                self.gen[e] += 1
                self.count[e] = 0
                if e == "pe":
                    self.pe_marks = []
                    self.pe_entries = {}
        for (tag, idx, g), c in list(self.dma_cnt.items()):
            if g == self.dgen.get(idx, 0) and 16 * c > self.DMA_SWITCH:
                self.dgen[idx] = g + 1
        self.keymap = {}

    def close(self):
        self.stack.close()


def t5_bucket_np(dist):
    d = np.maximum(dist, 0)
    large = 16 + (np.log(np.maximum(d, 1).astype(np.float32) / np.float32(16))
                  / np.float32(math.log(128 / 16)) * np.float32(16)).astype(np.int32)
    large = np.minimum(large, 31)
    return np.where(d < 16, d, large)


def host_consts(T):
    bf = ml_dtypes.bfloat16
    c = {}
    k = np.arange(128)[:, None]
    q = np.arange(128)[None, :]
    c["c_tri"] = (k <= q).astype(bf)
    c["c_ident"] = np.eye(128, dtype=bf)
    c["c_identf"] = np.eye(128, dtype=np.float32)
    inv = (10000.0 ** (-np.arange(0, 32, 2, dtype=np.float32) / np.float32(32))).astype(np.float32)
    ang = np.arange(T, dtype=np.float32)[None, :] * inv[:, None]
    cos, sin = np.cos(ang).astype(np.float32), np.sin(ang).astype(np.float32)
    cos96 = np.ones((96, T), np.float32)
    sin96 = np.zeros((96, T), np.float32)
    cos96[64:80], cos96[80:96] = cos, cos
    sin96[64:80], sin96[80:96] = sin, sin
    c["c_cos96"], c["c_sin96"] = cos96, sin96
    R = np.zeros((96, 96), np.float32)
    for i in range(16):
        R[64 + 16 + i, 64 + i] = -1.0
        R[64 + i, 64 + 16 + i] = 1.0
    c["c_rot96"] = R.astype(bf)
    LW, LC = 512, 4352
    dw = np.arange(LW) - 127
    ohw = np.zeros((32, LW), np.float32)
    ohw[t5_bucket_np(dw), np.arange(LW)] = 1.0
    ohw[:, dw < 0] = 0.0
    c["c_ohw"] = ohw
    vw = np.zeros((16, LW), np.float32)
    vw[0:8] = ((dw >= 0) & (dw < 256)).astype(np.float32)
    vw[8:16] = ((dw >= 0) & (dw < 128)).astype(np.float32)
    c["c_validw"] = vw
    dc = np.arange(LC) - 2063
    ohc = np.zeros((32, LC), np.float32)
    ohc[t5_bucket_np(dc), np.arange(LC)] = 1.0
    ohc[:, dc < 0] = 0.0
    c["c_ohc"] = ohc
    c["c_validc"] = np.broadcast_to((dc >= 0).astype(np.float32), (8, LC)).copy()
    kk = np.arange(T)
    c["c_esel"] = (kk[None, :] // 64 == np.arange(128)[:, None]).astype(bf)
    ci = np.arange(512)[:, None]
    sj = np.arange(128)[None, :]
    ncmp = (T - 32) // 16 + 1
    ov = ((ci * 16 + 31 >= sj * 64) & (ci * 16 <= sj * 64 + 63) & (ci < ncmp))
    c["c_overlap"] = ov.astype(bf)
    qpos = np.arange(T)[:, None]
    cur = qpos // 64
    s = np.arange(128)[None, :]
    forced = (s == 0) | (s == cur) | (s == cur - 1)
    valid = s <= cur
    add = np.where(valid, np.where(forced, np.float32(1e9), np.float32(0.0)), np.float32(-1.0)).astype(np.float32)
    c["c_topadd"] = add.reshape(T // 128, 128, 128)
    return c


def build(T, n_layers=1, stages="ABC", debug=False, io=None):
    NLP = n_layers
    fused = n_layers > 1
    io = io or {}
    nc = bass.Bass("TRN2", target_bir_lowering=False)
    NT = T // 128
    NG = T // 512
    P = Prog(nc)
    dt = nc.dram_tensor
    in_names = []
    dbg = {}
    sA, sB, sS, sN, sC = [c in stages for c in "ABSNC"]

    def din(name, shape, dtype=F32, need=True):
        if not need:
            return None
        in_names.append(name)
        return dt(name, list(shape), dtype, kind="ExternalInput").ap()

    def dsc(name, shape, dtype):
        return dt(name, list(shape), dtype).ap()

    def dout(name, shape, dtype=F32):
        a = dt(name, list(shape), dtype, kind="ExternalOutput").ap()
        dbg[name] = a
        return a

    def mk(name, shape, dtype):
        kind = io.get(name)
        if kind == "in":
            return din(name, shape, dtype)
        if kind == "out" or debug:
            return dout(name, shape, dtype)
        return dsc(name, shape, dtype)

    x = din("x", [T, D], need=sA or sC)
    w_fm = din("w_fm", [NLP, D, NFM], need=sA)
    w_tm = din("w_tm", [NLP, D, NTM], need=sA)
    norm_attn = din("norm_attn", [NLP, D], need=sA)
    q_norm = din("mla_q_norm", [NLP, 384], need=sA)
    w_uq = din("mla_w_uq", [NLP, 384, 768], need=sA)
    kv_norm = din("mla_kv_norm", [NLP, 128], need=sA)
    w_ukv_k = din("w_ukv_k", [NLP, 128, 512], need=sA)
    w_ukv_v = din("w_ukv_v", [NLP, 128, 512], need=sA)
    fox_b = din("fox_b_f", [NLP, 8], need=sB)
    group_norm = din("group_norm", [NLP, D], need=sC)
    w_out = din("w_out", [NLP, D, D], need=sC)
    norm_mlp = din("norm_mlp", [NLP, D], need=sC)
    w_up = din("w_up", [NLP, D, DFF], need=sC)
    w_down = din("w_down", [NLP, DFF, D], need=sC)
    final_norm = din("final_norm", [D], need=sC)
    nsa_pos = din("nsa_cmp_pos", [NLP, 2, 32, 64], need=sN)
    nsa_w1 = din("nsa_cmp_w1", [NLP, 2, 2048, 64], need=sN)
    nsa_w2 = din("nsa_cmp_w2", [NLP, 2, 64, 64], need=sN)
    swa_sinks = din("swa_sinks", [NLP, 8], need=sS)
    rel_bias = din("rel_bias", [32, 16], need=sS or sN)
    if sC:
        y_out = dt("y", [T, D], F32, kind="ExternalOutput").ap()
        if fused:
            hres = dsc("hres", [T, D], F32)
        else:
            hn_out = dt("hn", [T, D], F32, kind="ExternalOutput").ap()
        wb_out = dsc("wb_out", [NLP, D, D], BF16)
        wb_up = dsc("wb_up", [NLP, D, DFF], BF16)
        wb_dn = dsc("wb_dn", [NLP, DFF, D], BF16)
    consts = {}
    hc = host_consts(T)
    need_c = {"c_tri": sB, "c_ident": sA or sC, "c_identf": sN, "c_cos96": sA, "c_sin96": sA, "c_rot96": sA,
              "c_ohw": sS or sN, "c_validw": sS or sN, "c_ohc": sS or sN, "c_validc": sS or sN,
              "c_esel": sN, "c_overlap": sN, "c_topadd": sN}
    for k_, v_ in hc.items():
        consts[k_] = din(k_, v_.shape, BF16 if v_.dtype == ml_dtypes.bfloat16 else F32, need=need_c.get(k_, True))
    ocat_keep = [None]
    vstage_keep = [None]

    if sA:
        wb_fm = dsc("wb_fm", [NLP, D, NFM], BF16)
        wb_tm = dsc("wb_tm", [NLP, D, NTM], BF16)
        wb_uq = dsc("wb_uq", [NLP, 384, 768], BF16)
        wb_ukvk = dsc("wb_ukvk", [NLP, 128, 512], BF16)
        wb_ukvv = dsc("wb_ukvv", [NLP, 128, 512], BF16)
    pT = mk("pT", [NFM - 128, T], BF16)
    fT = mk("fT", [8, T], F32)
    pTM = mk("pTM", [T, NTM], F32)
    pTMb = mk("pTMb", [T, NTM], BF16)
    osel = mk("osel", [T, 512], BF16)
    owin = mk("owin", [T, 512], BF16)
    qT_mla = mk("qT_mla", [8, 96, T], BF16)
    kT_mla = mk("kT_mla", [8, 64, T], BF16)
    kpeT = mk("kpeT", [32, T], BF16)
    v_mla = mk("v_mla", [T, 512], BF16)
    foxq_aug = mk("foxq_aug", [8, 6, T], BF16)
    foxk_aug = mk("foxk_aug", [8, 6, T], BF16)
    if io.get("ocat") == "in":
        ocat_in = din("ocat_in", [T, D], BF16)
        ocat = dsc("ocat", [T, D], BF16)
        P.dma("sp", ocat[:, 0:1024], ocat_in[:, 0:1024], semkey="cp_ocat")
        P.barrier()
    else:
        ocat = mk("ocat", [T, D], BF16)
    ocat_keep[0] = ocat
    dbg["_inputs"] = in_names

    with contextlib.ExitStack() as st:
        gcol = P.sbuf("gcol", [128, 64], F32, st)
        stage_f = [P.sbuf("wst%d" % i, [128, 2816], F32, st) for i in range(2)]
        stage_b = [P.sbuf("wsb%d" % i, [128, 2816], BF16, st) for i in range(2)]
        cnt = [0]

        def prep(src, dst, rows, cols, gain_ap, gkey):
            nch = rows // 128
            if gain_ap is not None:
                P.dma("sp", gcol[:, 0:nch], gain_ap.rearrange("(c p) -> p c", p=128),
                      writes=["gcol"], allow_slow_non_contiguous=True)
            for c in range(nch):
                i = cnt[0] % 2
                cnt[0] += 1
                P.dma("sp", stage_f[i][:, 0:cols], src[c * 128:(c + 1) * 128, :], writes=["wst%d" % i])
                if gain_ap is not None:
                    eng = "dve" if c % 2 == 0 else "pool"
                    P.op(eng, lambda e, i=i, c=c: e.tensor_scalar(
                        out=stage_b[i][:, 0:cols], in0=stage_f[i][:, 0:cols],
                        scalar1=gcol[:, c:c + 1], scalar2=None, op0=ALU.mult),
                        reads=["wst%d" % i, "gcol"], writes=["wsb%d" % i])
                else:
                    P.op("act", lambda e, i=i: e.copy(out=stage_b[i][:, 0:cols], in_=stage_f[i][:, 0:cols]),
                         reads=["wst%d" % i], writes=["wsb%d" % i])
                P.dma("pool", dst[c * 128:(c + 1) * 128, :], stage_b[i][:, 0:cols],
                      reads=["wsb%d" % i], semkey="wprep_st%d" % i)

        for l in range(n_layers):
            if sA:
                prep(w_fm[l], wb_fm[l], D, NFM, norm_attn[l], "ga")
                prep(w_tm[l], wb_tm[l], D, NTM, norm_attn[l], "ga")
                prep(w_uq[l], wb_uq[l], 384, 768, q_norm[l], "gq")
                prep(w_ukv_k[l], wb_ukvk[l], 128, 512, kv_norm[l], "gk")
                prep(w_ukv_v[l], wb_ukvv[l], 128, 512, kv_norm[l], "gk")
            if "C" in stages:
                prep(w_out[l], wb_out[l], D, D, group_norm[l], "gg")
                for c0 in range(0, DFF, 2048):
                    prep(w_up[l][:, c0:c0 + 2048], wb_up[l][:, c0:c0 + 2048], D, 2048, norm_mlp[l], "gm")
                prep(w_down[l], wb_dn[l], DFF, D, None, None)
        P.barrier()
        P.emit()

    for l in range(n_layers):
        hsrc = x if l == 0 else hres
        if "A" in stages:
            with contextlib.ExitStack() as st:
                ident = P.sbuf("ident", [128, 128], BF16, st)
                rot96 = P.sbuf("rot96", [96, 96], BF16, st)
                P.dma("sp", ident[:], consts["c_ident"][:, :], semkey="ld_const")
                P.dma("sp", rot96[:], consts["c_rot96"][:, :], semkey="ld_const")
                rot32 = P.sbuf("rot32", [32, 32], BF16, st)
                P.dma("sp", rot32[:], consts["c_rot96"][64:96, 64:96], semkey="ld_const")
                cos32 = P.sbuf("cos32", [32, 512], F32, st)
                sin32 = P.sbuf("sin32", [32, 512], F32, st)
                hx = [P.sbuf("hx%d" % i, [128, D], F32, st) for i in range(2)]
                ub = [P.sbuf("ub%d" % i, [128, D], BF16, st) for i in range(2)]
                junk = P.sbuf("junk", [128, D], BF16, st)
                ss = P.sbuf("ss", [128, 8], F32, st)
                uT = P.sbuf("uT", [128, 16, 512], BF16, st)
                wsl = [P.sbuf("wsl%d" % i, [128, 16, 512], BF16, st) for i in range(2)]
                pfm = [P.sbuf("pfm%d" % i, [128, 512], BF16, st) for i in range(2)]
                pff = P.sbuf("pff", [128, 512], F32, st)
                ptm = [P.sbuf("ptm%d" % i, [128, 512], F32, st) for i in range(2)]
                ptmb = [P.sbuf("ptmb%d" % i, [128, 512], BF16, st) for i in range(2)]
                mla_in = P.sbuf("mla_in", [128, 4, 544], F32, st)
                cqn = P.sbuf("cqn", [128, 544], BF16, st)
                cT = P.sbuf("cT", [128, 5, 512], BF16, st)
                wuq = P.sbuf("wuq", [128, 3, 768], BF16, st)
                wkk = P.sbuf("wkk", [128, 512], BF16, st)
                wkv = P.sbuf("wkv", [128, 512], BF16, st)
                cos_t = P.sbuf("cos_t", [96, 512], F32, st)
                sin_t = P.sbuf("sin_t", [96, 512], F32, st)
                qsb = P.sbuf("qsb", [96, 512], BF16, st)
                qf1 = P.sbuf("qf1", [96, 512], F32, st)
                qf2 = P.sbuf("qf2", [96, 512], F32, st)
                qo = [P.sbuf("qo%d" % i, [96, 512], BF16, st) for i in range(2)]
                ps_t = [P.psum("ps_t%d" % i, [128, 512], BF16, st) for i in range(2)]
                ps_m = [P.psum("ps_m%d" % i, [128, 512], F32, st) for i in range(3)]
                P.dma("sp", wuq[:], wb_uq[l].rearrange("(c p) n -> p c n", p=128), semkey="ld_const")
                P.dma("sp", wkk[:], wb_ukvk[l], semkey="ld_const")
                P.dma("sp", wkv[:], wb_ukvv[l], semkey="ld_const")
                P.barrier()
                slab_i = [0]
                pm_i = [0]
                pt_i = [0]

                def load_slab(wsrc, c0, ncols):
                    i = slab_i[0] % 2
                    slab_i[0] += 1
                    P.dma(None, wsl[i][:, :, 0:ncols],
                          wsrc[:, c0:c0 + ncols].rearrange("(c p) n -> p c n", p=128),
                          writes=["wsl%d" % i])
                    return i

                for g in range(NG):
                    for tb in range(4):
                        t0 = g * 512 + tb * 128
                        i = tb % 2
                        P.dma("sp", hx[i][:], hsrc[t0:t0 + 128, :], writes=["hx%d" % i])
                        P.op("act", lambda e, i=i, tb=tb: e.activation(
                            out=junk[:], in_=hx[i][:], func=AF.Square, accum_out=ss[:, tb:tb + 1]),
                            reads=["hx%d" % i], writes=["junk", "ss%d" % tb])
                        P.op("dve", lambda e, tb=tb: e.tensor_scalar(
                            out=ss[:, tb:tb + 1], in0=ss[:, tb:tb + 1], scalar1=1.0 / D, scalar2=EPS,
                            op0=ALU.mult, op1=ALU.add), reads=["ss%d" % tb], writes=["ss%d" % tb])
                        P.op("act", lambda e, tb=tb: e.activation(
                            out=ss[:, tb:tb + 1], in_=ss[:, tb:tb + 1], func=AF.Sqrt),
                            reads=["ss%d" % tb], writes=["ss%d" % tb])
                        P.op("dve", lambda e, tb=tb: e.reciprocal(out=ss[:, tb:tb + 1], in_=ss[:, tb:tb + 1]),
                             reads=["ss%d" % tb], writes=["ss%d" % tb])
                        P.op("dve", lambda e, i=i, tb=tb: e.tensor_scalar(
                            out=ub[i][:], in0=hx[i][:], scalar1=ss[:, tb:tb + 1], scalar2=None, op0=ALU.mult),
                            reads=["hx%d" % i, "ss%d" % tb], writes=["ub%d" % i])
                        for c4 in range(4):
                            j = pt_i[0] % 2
                            pt_i[0] += 1
                            for cc in range(4):
                                c = c4 * 4 + cc
                                P.op("pe", lambda e, i=i, c=c, cc=cc, j=j: e.transpose(
                                    out=ps_t[j][:, cc * 128:(cc + 1) * 128], in_=ub[i][:, c * 128:(c + 1) * 128],
                                    identity=ident[:]), reads=["ub%d" % i, "ident"], writes=["ps_t%d" % j])
                            P.op("act" if c4 % 2 else "dve", lambda e, c4=c4, tb=tb, j=j: (
                                e.copy if hasattr(e, "copy") and not hasattr(e, "tensor_copy") else e.tensor_copy)(
                                out=uT[:, c4 * 4:(c4 + 1) * 4, tb * 128:(tb + 1) * 128],
                                in_=ps_t[j][:].rearrange("p (c t) -> p c t", c=4)),
                                reads=["ps_t%d" % j], writes=["uT%d" % tb])
                    uTk = ["uT%d" % tb for tb in range(4)]
                    for s in range(NFM // 512 + 1):
                        ncols = min(512, NFM - s * 512)
                        si = load_slab(wb_fm[l], s * 512, ncols)
                        for c in range(ncols // 128):
                            row0 = s * 512 + c * 128
                            j = pm_i[0] % 3
                            pm_i[0] += 1
                            for kc in range(16):
                                P.op("pe", lambda e, si=si, c=c, kc=kc, j=j: e.matmul(
                                    ps_m[j][:], lhsT=wsl[si][:, kc, c * 128:(c + 1) * 128], rhs=uT[:, kc, :],
                                    start=(kc == 0), stop=(kc == 15)),
                                    reads=["wsl%d" % si] + uTk, writes=["ps_m%d" % j])
                            if row0 == FM_F:
                                P.op("act", lambda e, j=j: e.copy(out=pff[0:8, :], in_=ps_m[j][0:8, :]),
                                     reads=["ps_m%d" % j], writes=["pff"])
                                P.dma("pool", fT[:, g * 512:(g + 1) * 512], pff[0:8, :], reads=["pff"], semkey="st_pff")
                            else:
                                i2 = (row0 // 128) % 2
                                P.op("act" if i2 else "dve", lambda e, j=j, i2=i2: (
                                    e.copy if not hasattr(e, "tensor_copy") else e.tensor_copy)(
                                    out=pfm[i2][:], in_=ps_m[j][:]),
                                    reads=["ps_m%d" % j], writes=["pfm%d" % i2])
                                P.dma("pool", pT[row0:row0 + 128, g * 512:(g + 1) * 512], pfm[i2][:],
                                      reads=["pfm%d" % i2], semkey="st_pfm%d" % i2)
                    for s in range(NTM // 512):
                        si = load_slab(wb_tm[l], s * 512, 512)
                        for tb in range(4):
                            t0 = g * 512 + tb * 128
                            j = pm_i[0] % 3
                            pm_i[0] += 1
                            for kc in range(16):
                                P.op("pe", lambda e, si=si, tb=tb, kc=kc, j=j: e.matmul(
                                    ps_m[j][:], lhsT=uT[:, kc, tb * 128:(tb + 1) * 128], rhs=wsl[si][:, kc, :],
                                    start=(kc == 0), stop=(kc == 15)),
                                    reads=["wsl%d" % si, "uT%d" % tb], writes=["ps_m%d" % j])
                            i2 = tb % 2
                            P.op("act" if i2 else "dve", lambda e, j=j, i2=i2: (
                                e.copy if not hasattr(e, "tensor_copy") else e.tensor_copy)(
                                out=ptm[i2][:], in_=ps_m[j][:]),
                                reads=["ps_m%d" % j], writes=["ptm%d" % i2])
                            P.dma("pool", pTM[t0:t0 + 128, s * 512:(s + 1) * 512], ptm[i2][:],
                                  reads=["ptm%d" % i2], semkey="st_ptm%d" % i2)
                            P.op("pool", lambda e, i2=i2: e.tensor_copy(out=ptmb[i2][:], in_=ptm[i2][:]),
                                 reads=["ptm%d" % i2], writes=["ptmb%d" % i2])
                            P.dma("sp", pTMb[t0:t0 + 128, s * 512:(s + 1) * 512], ptmb[i2][:],
                                  reads=["ptmb%d" % i2], semkey="st_ptmb%d" % i2)
                            if s == 0:
                                P.op("dve", lambda e, i2=i2, tb=tb: e.tensor_copy(
                                    out=mla_in[:, tb, 0:512], in_=ptm[i2][:]),
                                    reads=["ptm%d" % i2], writes=["mla_in%d" % tb])
                            if s == 1:
                                P.op("dve", lambda e, i2=i2, tb=tb: e.tensor_copy(
                                    out=mla_in[:, tb, 512:544], in_=ptm[i2][:, 0:32]),
                                    reads=["ptm%d" % i2], writes=["mla_in%d" % tb])
                    for tb in range(4):
                        for (o0, w, col) in ((0, 384, 4), (384, 128, 5)):
                            P.op("act", lambda e, tb=tb, o0=o0, w=w, col=col: e.activation(
                                out=junk[:, 0:w], in_=mla_in[:, tb, o0:o0 + w], func=AF.Square,
                                accum_out=ss[:, col:col + 1]),
                                reads=["mla_in%d" % tb], writes=["junk", "ssm%d" % col])
                            P.op("dve", lambda e, w=w, col=col: e.tensor_scalar(
                                out=ss[:, col:col + 1], in0=ss[:, col:col + 1], scalar1=1.0 / w, scalar2=EPS,
                                op0=ALU.mult, op1=ALU.add), reads=["ssm%d" % col], writes=["ssm%d" % col])
                            P.op("act", lambda e, col=col: e.activation(
                                out=ss[:, col:col + 1], in_=ss[:, col:col + 1], func=AF.Sqrt),
                                reads=["ssm%d" % col], writes=["ssm%d" % col])
                            P.op("dve", lambda e, col=col: e.reciprocal(out=ss[:, col:col + 1], in_=ss[:, col:col + 1]),
                                 reads=["ssm%d" % col], writes=["ssm%d" % col])
                            P.op("dve", lambda e, tb=tb, o0=o0, w=w, col=col: e.tensor_scalar(
                                out=cqn[:, o0:o0 + w], in0=mla_in[:, tb, o0:o0 + w], scalar1=ss[:, col:col + 1],
                                scalar2=None, op0=ALU.mult),
                                reads=["mla_in%d" % tb, "ssm%d" % col], writes=["cqn"])
                        P.op("dve", lambda e, tb=tb: e.tensor_copy(out=cqn[:, 512:544], in_=mla_in[:, tb, 512:544]),
                             reads=["mla_in%d" % tb], writes=["cqn"])
                        j = pt_i[0] % 2
                        pt_i[0] += 1
                        for c in range(4):
                            P.op("pe", lambda e, c=c, j=j: e.transpose(
                                out=ps_t[j][:, c * 128:(c + 1) * 128], in_=cqn[:, c * 128:(c + 1) * 128],
                                identity=ident[:]), reads=["cqn", "ident"], writes=["ps_t%d" % j])
                        P.op("dve", lambda e, tb=tb, j=j: e.tensor_copy(
                            out=cT[:, 0:4, tb * 128:(tb + 1) * 128],
                            in_=ps_t[j][:].rearrange("p (c t) -> p c t", c=4)),
                            reads=["ps_t%d" % j], writes=["cT%d" % tb])
                        j = pt_i[0] % 2
                        pt_i[0] += 1
                        P.op("pe", lambda e, j=j: e.transpose(
                            out=ps_t[j][0:32, 0:128], in_=cqn[:, 512:544], identity=ident[:]),
                            reads=["cqn", "ident"], writes=["ps_t%d" % j])
                        P.op("dve", lambda e, tb=tb, j=j: e.tensor_copy(
                            out=cT[0:32, 4, tb * 128:(tb + 1) * 128], in_=ps_t[j][0:32, 0:128]),
                            reads=["ps_t%d" % j], writes=["cT%d" % tb])
                    cTk = ["cT%d" % tb for tb in range(4)]
                    P.dma("sp", cos_t[:], consts["c_cos96"][:, g * 512:(g + 1) * 512], writes=["ropetab"])
                    P.dma("sp", sin_t[:], consts["c_sin96"][:, g * 512:(g + 1) * 512], writes=["ropetab"])

                    P.dma("sp", cos32[:], consts["c_cos96"][64:96, g * 512:(g + 1) * 512], writes=["ropetab"])
                    P.dma("sp", sin32[:], consts["c_sin96"][64:96, g * 512:(g + 1) * 512], writes=["ropetab"])

                    def rope_out(src_key, src_ap, rows, rot_ap, rot_key, cos_ap, sin_ap, ckeys, dst_ap):
                        P.op("act", lambda e: e.copy(out=qsb[0:rows, :], in_=src_ap),
                             reads=(cTk if src_key == "cTall" else [src_key]), writes=["qsb"])
                        jj = pm_i[0] % 3
                        pm_i[0] += 1
                        P.op("pe", lambda e, jj=jj: e.matmul(
                            ps_m[jj][0:rows, :], lhsT=rot_ap, rhs=qsb[0:rows, :], start=True, stop=True),
                            reads=["qsb", rot_key], writes=["ps_m%d" % jj])
                        P.op("dve", lambda e: e.tensor_tensor(out=qf1[0:rows, :], in0=qsb[0:rows, :],
                                                               in1=cos_ap, op=ALU.mult),
                             reads=["qsb", ckeys[0]], writes=["qf1"])
                        P.op("dve", lambda e, jj=jj: e.tensor_tensor(out=qf2[0:rows, :], in0=ps_m[jj][0:rows, :],
                                                                      in1=sin_ap, op=ALU.mult),
                             reads=["ps_m%d" % jj, ckeys[1]], writes=["qf2"])
                        oi = pm_i[0] % 2
                        P.op("pool", lambda e, oi=oi: e.tensor_tensor(out=qo[oi][0:rows, :], in0=qf1[0:rows, :],
                                                                       in1=qf2[0:rows, :], op=ALU.add),
                             reads=["qf1", "qf2"], writes=["qo%d" % oi])
                        P.dma("pool", dst_ap, qo[oi][0:rows, :], reads=["qo%d" % oi], semkey="st_qo%d" % oi)

                    for h in range(8):
                        j = pm_i[0] % 3
                        pm_i[0] += 1
                        for kc in range(3):
                            P.op("pe", lambda e, h=h, kc=kc, j=j: e.matmul(
                                ps_m[j][0:96, :], lhsT=wuq[:, kc, h * 96:(h + 1) * 96], rhs=cT[:, kc, :],
                                start=(kc == 0), stop=(kc == 2)),
                                reads=["wuq"] + cTk, writes=["ps_m%d" % j])
                        rope_out("ps_m%d" % j, ps_m[j][0:96, :], 96, rot96[:, :], "rot96", cos_t[:, :], sin_t[:, :],
                                 ("ropetab", "ropetab"), qT_mla[h, :, g * 512:(g + 1) * 512])
                        j = pm_i[0] % 3
                        pm_i[0] += 1
                        P.op("pe", lambda e, h=h, j=j: e.matmul(
                            ps_m[j][0:64, :], lhsT=wkk[:, h * 64:(h + 1) * 64], rhs=cT[:, 3, :],
                            start=True, stop=True), reads=["wkk"] + cTk, writes=["ps_m%d" % j])
                        i2 = h % 2
                        P.op("act", lambda e, j=j, i2=i2: e.copy(out=pfm[i2][0:64, :], in_=ps_m[j][0:64, :]),
                             reads=["ps_m%d" % j], writes=["pfm%d" % i2])
                        P.dma("pool", kT_mla[h, :, g * 512:(g + 1) * 512], pfm[i2][0:64, :],
                              reads=["pfm%d" % i2], semkey="st_pfm%d" % i2)
                    rope_out("cTall", cT[0:32, 4, :], 32, rot32[:, :], "rot32", cos32[:, :], sin32[:, :],
                             ("ropetab", "ropetab"), kpeT[:, g * 512:(g + 1) * 512])
                    for tb in range(4):
                        t0 = g * 512 + tb * 128
                        j = pm_i[0] % 3
                        pm_i[0] += 1
                        P.op("pe", lambda e, tb=tb, j=j: e.matmul(
                            ps_m[j][:], lhsT=cT[:, 3, tb * 128:(tb + 1) * 128], rhs=wkv[:, :], start=True, stop=True),
                            reads=["wkv", "cT%d" % tb], writes=["ps_m%d" % j])
                        i2 = tb % 2
                        P.op("act", lambda e, j=j, i2=i2: e.copy(out=pfm[i2][:], in_=ps_m[j][:]),
                             reads=["ps_m%d" % j], writes=["pfm%d" % i2])
                        P.dma("pool", v_mla[t0:t0 + 128, :], pfm[i2][:], reads=["pfm%d" % i2], semkey="st_pfm%d" % i2)
                P.barrier()
                P.emit()
        if "B" in stages:
            with contextlib.ExitStack() as st:
                fl = P.sbuf("fl", [8, T], F32, st)
                fo = P.sbuf("fo", [8, T], F32, st)
                fr = P.sbuf("fr", [8, T], F32, st)
                fb = [P.sbuf("fb%d" % i, [8, T], BF16, st) for i in range(3)]
                fnb = [P.sbuf("fnb%d" % i, [8, T], BF16, st) for i in range(2)]
                f8 = P.sbuf("f8", [8, T], BF16, st)
                nb = P.sbuf("nb", [8, 1], F32, st)
                P.dma("sp", fl[:], fT[:, :], writes=["fl"])
                P.dma("sp", nb[:], fox_b[l].rearrange("(h o) -> h o", o=1), writes=["nb"])
                P.op("dve", lambda e: e.tensor_scalar(out=nb[:], in0=nb[:], scalar1=-1.0, scalar2=None, op0=ALU.mult),
                     reads=["nb"], writes=["nb"])
                P.op("dve", lambda e: e.memset(fo[:], 1.0), writes=["fo"])
                P.op("pool", lambda e: e.memset(f8[:], 8.0), writes=["f8"])
                P.op("act", lambda e: e.activation(out=fl[:], in_=fl[:], func=AF.Exp, bias=nb[:], scale=-1.0),
                     reads=["fl", "nb"], writes=["fl"])
                P.op("act", lambda e: e.activation(out=fl[:], in_=fl[:], func=AF.Ln, bias=1.0, scale=1.0),
                     reads=["fl"], writes=["fl"])
                P.op("dve", lambda e: e.tensor_tensor_scan(out=fr[:], data0=fo[:], data1=fl[:], initial=0.0,
                                                           op0=ALU.mult, op1=ALU.add),
                     reads=["fl", "fo"], writes=["fr"])
                for i in range(3):
                    P.op("dve", lambda e, i=i: e.tensor_copy(out=fb[i][:], in_=fr[:]), reads=["fr"], writes=["fb%d" % i])
                    P.op("dve", lambda e, i=i: e.tensor_scalar(out=fnb[i % 2][:], in0=fb[i][:], scalar1=-1.0, scalar2=None,
                                                               op0=ALU.mult), reads=["fb%d" % i], writes=["fnb%d" % (i % 2)])
                    if i < 2:
                        P.op("dve", lambda e, i=i: e.tensor_tensor(out=fr[:], in0=fr[:], in1=fb[i][:], op=ALU.subtract),
                             reads=["fr", "fb%d" % i], writes=["fr"])
                    P.dma("sp", foxk_aug[:, i, :], fb[i][:], reads=["fb%d" % i], semkey="st_fb%d" % i)
                    P.dma("sp", foxq_aug[:, 3 + i, :], fnb[i % 2][:], reads=["fnb%d" % (i % 2)], semkey="st_fnb%d" % (i % 2))
                    P.dma("pool", foxk_aug[:, 3 + i, :], f8[:], reads=["f8"], semkey="st_f8")
                    P.dma("pool", foxq_aug[:, i, :], f8[:], reads=["f8"], semkey="st_f8")
                P.barrier()
                P.emit()
            with contextlib.ExitStack() as st:
                tri = P.sbuf("tri", [128, 128], BF16, st)
                P.dma("sp", tri[:], consts["c_tri"][:, :], writes=["tri"])
                qt = [P.sbuf("qt%d" % i, [128, T], BF16, st) for i in range(2)]
                kt = [P.sbuf("kt%d" % i, [128, T], BF16, st) for i in range(2)]
                vt = [P.sbuf("vt%d" % i, [128, NT, 65], BF16, st) for i in range(2)]
                pt = [P.sbuf("pt%d" % i, [128, 512], BF16, st) for i in range(3)]
                rd = P.sbuf("rd", [128, 4], F32, st)
                vstb = P.sbuf("vstb", [128, NT, 64], BF16, st)
                osb = [P.sbuf("osb%d" % i, [128, 64], BF16, st) for i in range(2)]
                ps_s = [P.psum("ps_s%d" % i, [128, 512], F32, st) for i in range(2)]
                ps_o = [P.psum("ps_o%d" % i, [128, 512], F32, st) for i in range(4)]
                for i in range(2):
                    P.op("pool", lambda e, i=i: e.memset(vt[i][:, :, 64:65], 1.0), writes=["vt%d_1" % i])
                heads = []
                for h in range(8):
                    heads.append(dict(q=[(qT_mla[h], 0, 96)], k=[(kT_mla[h], 0, 64), (kpeT, 64, 32)],
                                      v=v_mla[:, h * 64:(h + 1) * 64], d=96, scale=96 ** -0.5, col=h * 64))
                for h in range(8):
                    heads.append(dict(q=[(pT[FM_FOXQ + h * 64:FM_FOXQ + (h + 1) * 64], 0, 64), (foxq_aug[h], 64, 6)],
                                      k=[(pT[FM_FOXK + h * 64:FM_FOXK + (h + 1) * 64], 0, 64), (foxk_aug[h], 64, 6)],
                                      v=pTMb[:, TM_FOXV + h * 64:TM_FOXV + (h + 1) * 64], d=70, scale=0.125, col=512 + h * 64))
                oi = [0]
                for hi, H in enumerate(heads):
                    bi = hi % 2
                    def rkeys(pref, p0, n):
                        ks = []
                        if p0 < 64:
                            ks.append("%s%d_0" % (pref, bi))
                        if p0 + n > 64:
                            ks.append("%s%d_64" % (pref, bi))
                        return ks
                    qk_keys = []
                    for (ap_, p0, n) in H["q"]:
                        P.dma(None, qt[bi][p0:p0 + n, :], ap_, writes=rkeys("qt", p0, n), semkey="ld_qt%d_%d" % (bi, p0))
                        qk_keys += rkeys("qt", p0, n)
                    for (ap_, p0, n) in H["k"]:
                        P.dma(None, kt[bi][p0:p0 + n, :], ap_, writes=rkeys("kt", p0, n), semkey="ld_kt%d_%d" % (bi, p0))
                        qk_keys += rkeys("kt", p0, n)
                    qk_keys = sorted(set(qk_keys))
                    if H["v"] is not None:
                        P.dma(None, vstb[:], H["v"].rearrange("(j p) d -> p j d", p=128), writes=["vstb"])
                        P.op("pool", lambda e, bi=bi: e.tensor_copy(out=vt[bi][:, :, 0:64], in_=vstb[:]),
                             reads=["vstb"], writes=["vt%d" % bi])
                    d = H["d"]
                    steps = [(c, j) for c in range(NG) for j in range(4 * c + 4)]

                    def qk(idx, bi=bi, d=d, qk_keys=qk_keys):
                        c, j = steps[idx]
                        s = idx % 2
                        P.op("pe", lambda e: e.matmul(ps_s[s][:], lhsT=kt[bi][0:d, j * 128:(j + 1) * 128],
                                                      rhs=qt[bi][0:d, c * 512:(c + 1) * 512], start=True, stop=True),
                             reads=qk_keys, writes=["ps_s%d" % s])
                    qk(0)
                    for idx, (c, j) in enumerate(steps):
                        s = idx % 2
                        pi = idx % 3
                        P.op("act", lambda e, s=s, pi=pi, H=H: e.activation(out=pt[pi][:], in_=ps_s[s][:], func=AF.Exp,
                                                                       scale=H["scale"]),
                             reads=["ps_s%d" % s], writes=["pt%d" % pi])
                        if j >= 4 * c:
                            b = j - 4 * c
                            P.op("dve", lambda e, pi=pi, b=b: e.tensor_tensor(
                                out=pt[pi][:, b * 128:(b + 1) * 128], in0=pt[pi][:, b * 128:(b + 1) * 128],
                                in1=tri[:], op=ALU.mult), reads=["pt%d" % pi, "tri"], writes=["pt%d" % pi])
                        if idx + 1 < len(steps):
                            qk(idx + 1)
                        for b in range(4):
                            qb = 4 * c + b
                            if j > qb:
                                continue
                            P.op("pe", lambda e, pi=pi, b=b, j=j, qb=qb, bi=bi: e.matmul(
                                ps_o[b][:, 0:65], lhsT=pt[pi][:, b * 128:(b + 1) * 128], rhs=vt[bi][:, j, :],
                                start=(j == 0), stop=(j == qb)),
                                reads=["pt%d" % pi, "vt%d" % bi, "vt%d_1" % bi], writes=["ps_o%d" % b])
                            if j == qb:
                                P.op("dve", lambda e, b=b: e.reciprocal(out=rd[:, b:b + 1], in_=ps_o[b][:, 64:65]),
                                     reads=["ps_o%d" % b], writes=["rd%d" % b])
                                o2 = oi[0] % 2
                                oi[0] += 1
                                P.op("dve", lambda e, b=b, o2=o2: e.tensor_scalar(
                                    out=osb[o2][:], in0=ps_o[b][:, 0:64], scalar1=rd[:, b:b + 1], scalar2=None,
                                    op0=ALU.mult), reads=["ps_o%d" % b, "rd%d" % b], writes=["osb%d" % o2])
                                P.dma("sp", ocat[qb * 128:(qb + 1) * 128, H["col"]:H["col"] + 64], osb[o2][:],
                                      reads=["osb%d" % o2], semkey="st_osb%d" % o2)
                    P.barrier()
                P.barrier()
                P.emit()
        if "N" in stages or "S" in stages:
            LW, LC = 512, 4352
            if l == 0:
                Fw_h = dt("Fw", [16 * 128 * (LW + 1)], F32)
                Fc_h = dt("Fc", [8 * 128 * (LC + 16)], F32)
                tabw_d = dsc("tabw_d", [16, LW], F32)
                tabc_d = dsc("tabc_d", [8, LC], F32)
            with contextlib.ExitStack() as st:
              if l == 0:
                  rb = P.sbuf("rb", [32, 16], F32, st)
                  ohw = P.sbuf("ohw", [32, LW], F32, st)
                  ohc = P.sbuf("ohc", [32, LC], F32, st)
                  vw = P.sbuf("vw", [16, LW], F32, st)
                  vc_ = P.sbuf("vc_", [8, LC], F32, st)
                  tw = P.sbuf("tw", [16, LW], F32, st)
                  tc_ = P.sbuf("tc_", [8, LC], F32, st)
                  ps = P.psum("ps_tab", [16, 512], F32, st)
                  P.dma("sp", rb[:], rel_bias[:, :], writes=["rb"])
                  P.dma("sp", ohw[:], consts["c_ohw"][:, :], writes=["ohw"])
                  P.dma("sp", ohc[:], consts["c_ohc"][:, :], writes=["ohc"])
                  P.dma("sp", vw[:], consts["c_validw"][:, :], writes=["vw"])
                  P.dma("sp", vc_[:], consts["c_validc"][:, :], writes=["vc_"])
                  P.op("pe", lambda e: e.matmul(ps[:, 0:LW], lhsT=rb[:, :], rhs=ohw[:, :], start=True, stop=True),
                       reads=["rb", "ohw"], writes=["ps_tab"])
                  P.op("act", lambda e: e.activation(out=tw[:], in_=ps[:, 0:LW], func=AF.Exp), reads=["ps_tab"], writes=["tw"])
                  P.op("dve", lambda e: e.tensor_tensor(out=tw[:], in0=tw[:], in1=vw[:], op=ALU.mult),
                       reads=["tw", "vw"], writes=["tw"])
                  P.dma("sp", tabw_d[:, :], tw[:], reads=["tw"], semkey="st_tw")
                  for c0 in range(0, LC, 512):
                      n = min(512, LC - c0)
                      P.op("pe", lambda e, c0=c0, n=n: e.matmul(ps[0:8, 0:n], lhsT=rb[:, 0:8], rhs=ohc[:, c0:c0 + n],
                                                               start=True, stop=True),
                           reads=["rb", "ohc"], writes=["ps_tab"])
                      P.op("act", lambda e, c0=c0, n=n: e.activation(out=tc_[:, c0:c0 + n], in_=ps[0:8, 0:n], func=AF.Exp),
                           reads=["ps_tab"], writes=["tc_"])
                  P.op("dve", lambda e: e.tensor_tensor(out=tc_[:], in0=tc_[:], in1=vc_[:], op=ALU.mult),
                       reads=["tc_", "vc_"], writes=["tc_"])
                  P.dma("sp", tabc_d[:, :], tc_[:], reads=["tc_"], semkey="st_tc")
                  P.barrier()
                  for h in range(16):
                      P.dma(None, bass.AP(Fw_h, h * 128 * (LW + 1), [[LW + 1, 128], [1, LW]]),
                            tabw_d[h].partition_broadcast(128), semkey="st_fw")
                  for h in range(8):
                      P.dma(None, bass.AP(Fc_h, h * 128 * (LC + 16), [[LC + 16, 128], [1, LC]]),
                            tabc_d[h].partition_broadcast(128), semkey="st_fc")
                  P.barrier()
                  P.emit()

            def ewin_ap(h, delta):
                return bass.AP(Fw_h, h * 128 * (LW + 1) + 127 + 128 * delta, [[LW, 128], [1, 128]])

            def ecmp_ap(h, delta):
                return bass.AP(Fc_h, h * 128 * (LC + 16) + 2032 + 128 * delta, [[LC, 128], [1, 128]])

            def run_chunk(R, steps, rhs_q, qkeys, scale, fin):
                firsts, lasts = {}, {}
                for i, s_ in enumerate(steps):
                    for b in s_["blocks"]:
                        firsts.setdefault(b, i)
                        lasts[b] = i

                def qk(i):
                    s_ = steps[i]
                    si = R["si"] % 2
                    R["si"] += 1
                    s_["si"] = si
                    ex = s_.get("extra")
                    P.op("pe", lambda e: e.matmul(R["ps_s"][si][:], lhsT=s_["kl"], rhs=rhs_q, start=True,
                                                  stop=(ex is None)),
                         reads=list(s_["kkeys"]) + list(qkeys), writes=["ps_s%d" % si])
                    if ex is not None:
                        P.op("pe", lambda e: e.matmul(R["ps_s"][si][:], lhsT=ex[0], rhs=ex[1], start=False, stop=True),
                             reads=list(ex[2]), writes=["ps_s%d" % si])
                if not steps:
                    return
                qk(0)
                for i, s_ in enumerate(steps):
                    si = s_["si"]
                    pi = R["pi"] % 3
                    R["pi"] += 1
                    pt = R["pt"][pi]
                    bias = s_.get("bias")
                    if bias is not None:
                        P.op("act", lambda e, si=si, pt=pt, bias=bias: e.activation(
                            out=pt[:], in_=R["ps_s"][si][:], func=AF.Exp, bias=bias[0], scale=scale),
                            reads=["ps_s%d" % si, bias[1]], writes=["pt%d" % pi])
                    else:
                        P.op("act", lambda e, si=si, pt=pt: e.activation(
                            out=pt[:], in_=R["ps_s"][si][:], func=AF.Exp, scale=scale),
                            reads=["ps_s%d" % si], writes=["pt%d" % pi])
                    for b, act in s_["blocks"].items():
                        sub = pt[:, b * 128:(b + 1) * 128]
                        if act[0] == "mul":
                            P.op("dve", lambda e, sub=sub, act=act: e.tensor_tensor(out=sub, in0=sub, in1=act[1], op=ALU.mult),
                                 reads=["pt%d" % pi, act[2]], writes=["pt%d" % pi])
                        elif act[0] == "mulsc":
                            P.op("dve", lambda e, sub=sub, act=act: e.tensor_scalar(out=sub, in0=sub, scalar1=act[1],
                                                                                   scalar2=None, op0=ALU.mult),
                                 reads=["pt%d" % pi, act[2]], writes=["pt%d" % pi])
                        elif act[0] == "mul2":
                            P.op("dve", lambda e, sub=sub, act=act: e.scalar_tensor_tensor(
                                out=sub, in0=sub, scalar=act[1], in1=act[3], op0=ALU.mult, op1=ALU.mult),
                                reads=["pt%d" % pi, act[2], act[4]], writes=["pt%d" % pi])
                    if i + 1 < len(steps):
                        qk(i + 1)
                    for b in s_["blocks"]:
                        P.op("pe", lambda e, b=b, pt=pt, s_=s_, i=i: e.matmul(
                            R["ps_o"][b][:, 0:s_["vn"]], lhsT=pt[:, b * 128:(b + 1) * 128], rhs=s_["v"],
                            start=(firsts[b] == i), stop=(lasts[b] == i)),
                            reads=["pt%d" % pi] + list(s_["vkeys"]), writes=["ps_o%d" % b])
                        if lasts[b] == i:
                            fin(b, R["ps_o"][b])

            def load_v(R, dst, dst_key, src_ap):
                P.dma(None, R["vstb"][:], src_ap.rearrange("(j p) d -> p j d", p=128), writes=["vstb"])
                P.op("pool", lambda e: e.tensor_copy(out=dst[:, :, 0:64], in_=R["vstb"][:]),
                     reads=["vstb"], writes=[dst_key])

            def common_res(st, tag):
                R = dict(si=0, pi=0, oi=0)
                R["pt"] = [P.sbuf("pt%s%d" % (tag, i), [128, 512], BF16, st) for i in range(3)]
                R["ps_s"] = [P.psum("pss%s%d" % (tag, i), [128, 512], F32, st) for i in range(2)]
                R["ps_o"] = [P.psum("pso%s%d" % (tag, i), [128, 512], F32, st) for i in range(4)]
                R["vstb"] = P.sbuf("vstb%s" % tag, [128, NT, 64], BF16, st)
                R["rd"] = P.sbuf("rd%s" % tag, [128, 8], F32, st)
                R["osb"] = [P.sbuf("osb%s%d" % (tag, i), [128, 64], BF16, st) for i in range(2)]
                return R

            if "S" in stages:
                with contextlib.ExitStack() as st:
                    R = common_res(st, "s")
                    esw = P.sbuf("esw", [128, 16, 128], F32, st)
                    for h in range(8):
                        for dl in range(2):
                            P.dma(None, esw[:, h * 2 + dl, :], ewin_ap(8 + h, dl), writes=["esw"])
                    sk = P.sbuf("sk", [128, 8], F32, st)
                    P.dma("sp", sk[:], swa_sinks[l].partition_broadcast(128), writes=["sk"])
                    P.op("act", lambda e: e.activation(out=sk[:], in_=sk[:], func=AF.Exp), reads=["sk"], writes=["sk"])
                    qt = [P.sbuf("qts%d" % i, [64, T], BF16, st) for i in range(2)]
                    kt = P.sbuf("kts", [64, T], BF16, st)
                    vt = P.sbuf("vts", [128, NT, 65], BF16, st)
                    P.op("pool", lambda e: e.memset(vt[:, :, 64:65], 1.0), writes=["vt_1"])
                    for h in range(8):
                        g = h // 4
                        bi = h % 2
                        if h % 4 == 0:
                            P.dma(None, kt[:], pT[FM_SWAK + g * 64:FM_SWAK + (g + 1) * 64], writes=["kt"])
                            load_v(R, vt, "vt", pTMb[:, TM_SWAV + g * 64:TM_SWAV + (g + 1) * 64])
                        P.dma(None, qt[bi][:], pT[FM_SWAQ + h * 64:FM_SWAQ + (h + 1) * 64], writes=["qt%d" % bi])

                        def fin(b, pso, h=h, cbox=None):
                            qb = fin.c * 4 + b
                            P.op("dve", lambda e: e.tensor_scalar(out=R["rd"][:, b:b + 1], in0=pso[:, 64:65],
                                                                  scalar1=sk[:, h:h + 1], scalar2=None, op0=ALU.add),
                                 reads=["ps_o%d" % b, "sk"], writes=["rd%d" % b])
                            P.op("dve", lambda e: e.reciprocal(out=R["rd"][:, b:b + 1], in_=R["rd"][:, b:b + 1]),
                                 reads=["rd%d" % b], writes=["rd%d" % b])
                            o2 = R["oi"] % 2
                            R["oi"] += 1
                            P.op("dve", lambda e: e.tensor_scalar(out=R["osb"][o2][:], in0=pso[:, 0:64],
                                                                  scalar1=R["rd"][:, b:b + 1], scalar2=None, op0=ALU.mult),
                                 reads=["ps_o%d" % b, "rd%d" % b], writes=["osb%d" % o2])
                            P.dma("sp", ocat[qb * 128:(qb + 1) * 128, 1536 + h * 64:1536 + (h + 1) * 64], R["osb"][o2][:],
                                  reads=["osb%d" % o2], semkey="st_osb%d" % o2)
                        for c in range(NG):
                            fin.c = c
                            steps = []
                            for j in range(max(0, 4 * c - 1), 4 * c + 4):
                                blocks = {}
                                for b in range(4):
                                    dl = 4 * c + b - j
                                    if 0 <= dl <= 1:
                                        blocks[b] = ("mul", esw[:, h * 2 + dl, :], "esw")
                                steps.append(dict(kl=kt[:, j * 128:(j + 1) * 128], kkeys=["kt"], v=vt[:, j, :],
                                                  vkeys=["vt", "vt_1"], vn=65, blocks=blocks))
                            run_chunk(R, steps, qt[bi][:, c * 512:(c + 1) * 512], ["qt%d" % bi], 0.125, fin)
                    P.barrier()
                    P.emit()

            if "N" in stages:
                ncmp = (T - 32) // 16 + 1
                NCB = (ncmp + 127) // 128
                with contextlib.ExitStack() as st:
                    R = common_res(st, "n")
                    ps_x = P.psum("ps_x", [128, 512], F32, st)
                    identf = P.sbuf("identf", [128, 128], F32, st)
                    ewn = P.sbuf("ewn", [128, 24, 128], F32, st)
                    ecm = P.sbuf("ecm", [128, 2, 8, 128], F32, st)
                    b31 = P.sbuf("b31", [128, 16], F32, st)
                    e31 = P.sbuf("e31", [128, 16], F32, st)
                    em31 = P.sbuf("em31", [128, 16], F32, st)
                    gates = P.sbuf("gates", [128, NT, 24], F32, st)
                    esel = P.sbuf("esel", [128, T], BF16, st)
                    ovl = P.sbuf("ovl", [128, 4, 128], BF16, st)
                    P.dma("sp", identf[:], consts["c_identf"][:, :], semkey="ld_const")
                    for h in range(8):
                        for dl in range(3):
                            P.dma(None, ewn[:, h * 3 + dl, :], ewin_ap(h, dl), semkey="ld_const")
                    P.dma("sp", b31[:], rel_bias[31].partition_broadcast(128), semkey="ld_const")
                    P.dma("sp", gates[:], pTM[:, TM_GATE:TM_GATE + 24].rearrange("(j p) c -> p j c", p=128), semkey="ld_const")
                    P.dma("sp", esel[:], consts["c_esel"][:, :], semkey="ld_const")
                    P.dma("sp", ovl[:], consts["c_overlap"].rearrange("(j p) s -> p j s", p=128), semkey="ld_const")
                    P.barrier()
                    P.op("act", lambda e: e.activation(out=e31[:], in_=b31[:], func=AF.Exp), reads=["b31"], writes=["e31"])
                    P.op("act", lambda e: e.activation(out=em31[:], in_=b31[:], func=AF.Exp, scale=-1.0),
                         reads=["b31"], writes=["em31"])
                    P.op("act", lambda e: e.activation(out=gates[:], in_=gates[:], func=AF.Exp, scale=-1.0),
                         reads=["gates"], writes=["gates"])
                    P.op("dve", lambda e: e.tensor_scalar(out=gates[:], in0=gates[:], scalar1=1.0, scalar2=None, op0=ALU.add),
                         reads=["gates"], writes=["gates"])
                    P.op("dve", lambda e: e.reciprocal(out=gates[:], in_=gates[:]), reads=["gates"], writes=["gates"])
                    negmt = P.sbuf("negmt", [128, T], BF16, st)
                    impacc = P.sbuf("impacc", [128, 4, 128], F32, st)
                    sc_t = P.sbuf("sc_t", [128, 128], F32, st)
                    m8 = P.sbuf("m8", [128, 8], F32, st)
                    tadd = [P.sbuf("tadd%d" % i, [128, 128], F32, st) for i in range(2)]
                    qt = [P.sbuf("qtn%d" % i, [64, T], BF16, st) for i in range(2)]
                    qch = [P.sbuf("qch%d" % i, [64, 512], BF16, st) for i in range(4)]
                    kt = P.sbuf("ktn", [64, T], BF16, st)
                    vt = P.sbuf("vtn", [128, NT, 65], BF16, st)
                    P.op("pool", lambda e: e.memset(vt[:, :, 64:65], 1.0), writes=["vt_1"])
                    kvT = P.sbuf("kvT", [64, T], BF16, st)
                    w1f = P.sbuf("w1f", [64, 32, 64], F32, st)
                    w1b = P.sbuf("w1b", [64, 32, 64], BF16, st)
                    w2f = P.sbuf("w2f", [64, 64], F32, st)
                    w2b = P.sbuf("w2b", [64, 64], BF16, st)
                    posf = P.sbuf("posf", [64, 32], F32, st)
                    posb = P.sbuf("posb", [64, 32], BF16, st)
                    cbias = P.sbuf("cbias", [64, 1], F32, st)
                    gx = P.sbuf("gx", [64, 512], F32, st)
                    gy = P.sbuf("gy", [64, 512], F32, st)
                    gT = P.sbuf("gT", [64, 512], BF16, st)
                    kcT = P.sbuf("kcT", [64, 512], BF16, st)
                    vca = P.sbuf("vca", [128, 4, 193], BF16, st)
                    P.op("pool", lambda e: e.memset(vca[:, :, 64:65], 1.0), writes=["vca_1"])
                    P.op("pool", lambda e: e.tensor_copy(out=vca[:, :, 65:193], in_=ovl[:]), reads=["ovl"], writes=["vca_o"])
                    P.op("dve", lambda e: e.memset(gT[:], 0.0), writes=["gT"])

                    def compress(which, g, fm_off):
                        P.dma(None, kvT[:], pT[fm_off + g * 64:fm_off + (g + 1) * 64], writes=["kvT"])
                        P.dma("sp", w1f[:], nsa_w1[l, which].rearrange("(l i) o -> i l o", i=64), writes=["cw"])
                        P.dma("sp", w2f[:], nsa_w2[l, which], writes=["cw"])
                        P.dma("sp", posf[:], nsa_pos[l, which].rearrange("l d -> d l"), writes=["cw"],
                              allow_slow_non_contiguous=True)
                        P.op("dve", lambda e: e.tensor_copy(out=w1b[:], in_=w1f[:]), reads=["cw"], writes=["w1b"])
                        P.op("dve", lambda e: e.tensor_copy(out=w2b[:], in_=w2f[:]), reads=["cw"], writes=["w2b"])
                        P.op("dve", lambda e: e.tensor_copy(out=posb[:], in_=posf[:]), reads=["cw"], writes=["posb"])
                        for li in range(32):
                            P.op("pe", lambda e, li=li: e.matmul(ps_x[0:64, 0:1], lhsT=w1b[:, li, :], rhs=posb[:, li:li + 1],
                                                                 start=(li == 0), stop=(li == 31)),
                                 reads=["w1b", "posb"], writes=["ps_x"])
                        P.op("dve", lambda e: e.tensor_copy(out=cbias[:], in_=ps_x[0:64, 0:1]), reads=["ps_x"], writes=["cbias"])
                        for li in range(32):
                            P.op("pe", lambda e, li=li: e.matmul(
                                ps_x[0:64, 0:ncmp], lhsT=w1b[:, li, :],
                                rhs=kvT[:].rearrange("p (c s) -> p c s", s=16)[:, li // 16:li // 16 + ncmp, li % 16],
                                start=(li == 0), stop=(li == 31)),
                                reads=["w1b", "kvT"], writes=["ps_x"])
                        n = ncmp
                        P.op("act", lambda e: e.activation(out=gx[:, 0:n], in_=ps_x[0:64, 0:n], func=AF.Identity,
                                                           bias=cbias[:], scale=1.0),
                             reads=["ps_x", "cbias"], writes=["gx"])
                        P.op("dve", lambda e: e.tensor_tensor(out=gy[:, 0:n], in0=gx[:, 0:n], in1=gx[:, 0:n], op=ALU.mult),
                             reads=["gx"], writes=["gy"])
                        P.op("dve", lambda e: e.tensor_scalar(out=gy[:, 0:n], in0=gy[:, 0:n], scalar1=0.044715, scalar2=1.0,
                                                              op0=ALU.mult, op1=ALU.add), reads=["gy"], writes=["gy"])
                        P.op("dve", lambda e: e.tensor_tensor(out=gy[:, 0:n], in0=gy[:, 0:n], in1=gx[:, 0:n], op=ALU.mult),
                             reads=["gy", "gx"], writes=["gy"])
                        P.op("act", lambda e: e.activation(out=gy[:, 0:n], in_=gy[:, 0:n], func=AF.Tanh, scale=0.7978845608),
                             reads=["gy"], writes=["gy"])
                        P.op("dve", lambda e: e.tensor_scalar(out=gy[:, 0:n], in0=gy[:, 0:n], scalar1=1.0, scalar2=0.5,
                                                              op0=ALU.add, op1=ALU.mult), reads=["gy"], writes=["gy"])
                        P.op("dve", lambda e: e.tensor_tensor(out=gT[:, 0:n], in0=gy[:, 0:n], in1=gx[:, 0:n], op=ALU.mult),
                             reads=["gy", "gx"], writes=["gT"])
                        if which == 0:
                            P.op("pe", lambda e: e.matmul(ps_x[0:64, 0:512], lhsT=w2b[:, :], rhs=gT[:, :], start=True, stop=True),
                                 reads=["w2b", "gT"], writes=["ps_x"])
                            P.op("act", lambda e: e.copy(out=kcT[:], in_=ps_x[0:64, 0:512]), reads=["ps_x"], writes=["kcT"])
                        else:
                            for cb in range(4):
                                P.op("pe", lambda e, cb=cb: e.matmul(ps_x[:, 0:64], lhsT=gT[:, cb * 128:(cb + 1) * 128],
                                                                     rhs=w2b[:, :], start=True, stop=True),
                                     reads=["w2b", "gT"], writes=["ps_x"])
                                P.op("act", lambda e, cb=cb: e.copy(out=vca[:, cb, 0:64], in_=ps_x[:, 0:64]),
                                     reads=["ps_x"], writes=["vca_v"])

                    for g in range(2):
                        compress(0, g, FM_KC)
                        compress(1, g, FM_VC)
                        vkeys_c = ["vca_v", "vca_1", "vca_o"]
                        for c in range(NG):
                            for r in range(4):
                                h = g * 4 + r
                                if c == 0:
                                    pass
                                qi = R["qi"] = R.get("qi", 0) + 1
                                qc = qch[qi % 4]
                                qk_ = "qch%d" % (qi % 4)
                                P.dma(None, qc[:], pT[FM_NSAQ + h * 64:FM_NSAQ + (h + 1) * 64, c * 512:(c + 1) * 512],
                                      writes=[qk_])
                                need = sorted({4 * c + b - 16 * cb for b in range(4) for cb in range(NCB)
                                               if 0 <= 4 * c + b - 16 * cb <= 17})
                                es = qi % 2
                                slot = {dl: k_ for k_, dl in enumerate(need)}
                                for dl in need:
                                    P.dma(None, ecm[:, es, slot[dl], :], ecmp_ap(h, dl), writes=["ecm%d" % es])

                                def fin(b, pso, h=h, c=c, r=r):
                                    qb = 4 * c + b
                                    P.op("dve", lambda e: e.tensor_scalar(out=R["rd"][:, b:b + 1], in0=pso[:, 64:65],
                                                                          scalar1=1e-30, scalar2=None, op0=ALU.max),
                                         reads=["ps_o%d" % b], writes=["rd%d" % b])
                                    P.op("dve", lambda e: e.reciprocal(out=R["rd"][:, b:b + 1], in_=R["rd"][:, b:b + 1]),
                                         reads=["rd%d" % b], writes=["rd%d" % b])
                                    o2 = R["oi"] % 2
                                    R["oi"] += 1
                                    P.op("dve", lambda e: e.tensor_scalar(
                                        out=R["osb"][o2][:], in0=pso[:, 0:64], scalar1=R["rd"][:, b:b + 1],
                                        scalar2=gates[:, qb, h * 3:h * 3 + 1], op0=ALU.mult, op1=ALU.mult),
                                        reads=["ps_o%d" % b, "rd%d" % b, "gates"], writes=["osb%d" % o2])
                                    P.dma("sp", ocat[qb * 128:(qb + 1) * 128, 1024 + h * 64:1024 + (h + 1) * 64],
                                          R["osb"][o2][:], reads=["osb%d" % o2], semkey="st_osb%d" % o2)
                                    if r == 0:
                                        P.op("dve", lambda e: e.tensor_scalar(
                                            out=impacc[:, b, :], in0=pso[:, 65:193], scalar1=R["rd"][:, b:b + 1],
                                            scalar2=None, op0=ALU.mult),
                                            reads=["ps_o%d" % b, "rd%d" % b], writes=["imp%d" % b])
                                    else:
                                        P.op("dve", lambda e: e.scalar_tensor_tensor(
                                            out=impacc[:, b, :], in0=pso[:, 65:193], scalar=R["rd"][:, b:b + 1],
                                            in1=impacc[:, b, :], op0=ALU.mult, op1=ALU.add),
                                            reads=["ps_o%d" % b, "rd%d" % b, "imp%d" % b], writes=["imp%d" % b])
                                steps = []
                                for cb in range(NCB):
                                    dls = [4 * c + b - 16 * cb for b in range(4)]
                                    if max(dls) < 0:
                                        continue
                                    blocks, bias = {}, None
                                    if min(dls) >= 18:
                                        bias = (b31[:, h:h + 1], "b31")
                                        blocks = {b: ("plain",) for b in range(4)}
                                    else:
                                        for b, dl in enumerate(dls):
                                            if dl < 0:
                                                continue
                                            if dl <= 17:
                                                blocks[b] = ("mul", ecm[:, es, slot[dl], :], "ecm%d" % es)
                                            else:
                                                blocks[b] = ("mulsc", e31[:, h:h + 1], "e31")
                                    steps.append(dict(kl=kcT[:, cb * 128:(cb + 1) * 128], kkeys=["kcT"], bias=bias,
                                                      v=vca[:, cb, :], vkeys=vkeys_c, vn=193, blocks=blocks))
                                run_chunk(R, steps, qc[:, :], [qk_], 0.125, fin)
                            for b in range(4):
                                qb = 4 * c + b
                                ti = qb % 2
                                P.dma("sp", tadd[ti][:], consts["c_topadd"][qb], writes=["tadd%d" % ti])
                                P.op("dve", lambda e, b=b, ti=ti: e.tensor_tensor(out=sc_t[:], in0=impacc[:, b, :],
                                                                                   in1=tadd[ti][:], op=ALU.add),
                                     reads=["imp%d" % b, "tadd%d" % ti], writes=["sc_t"])
                                P.op("dve", lambda e: e.max(out=m8[:], in_=sc_t[:]), reads=["sc_t"], writes=["m8"])
                                P.op("dve", lambda e: e.tensor_scalar(out=sc_t[:], in0=sc_t[:], scalar1=m8[:, 7:8], scalar2=1.0,
                                                                      op0=ALU.is_ge, op1=ALU.subtract),
                                     reads=["sc_t", "m8"], writes=["sc_t"])
                                P.op("pe", lambda e: e.transpose(out=ps_x[:, 0:128], in_=sc_t[:], identity=identf[:]),
                                     reads=["sc_t", "identf"], writes=["ps_x"])
                                P.op("act", lambda e, qb=qb: e.activation(out=negmt[:, qb * 128:(qb + 1) * 128],
                                                                           in_=ps_x[:, 0:128], func=AF.Identity, scale=30000.0),
                                     reads=["ps_x"], writes=["negmt%d" % c])
                            if c % 4 == 3:
                                P.barrier()
                        for br, (fm_k, tm_v, dst, gcol_) in enumerate(((FM_KSL, TM_VSL, osel, 1), (FM_KWN, TM_VWN, owin, 2))):
                            P.dma(None, kt[:], pT[fm_k + g * 64:fm_k + (g + 1) * 64], writes=["kt"])
                            load_v(R, vt, "vt", pTMb[:, tm_v + g * 64:tm_v + (g + 1) * 64])
                            for r in range(4):
                                h = g * 4 + r
                                bi = r % 2
                                P.dma(None, qt[bi][:], pT[FM_NSAQ + h * 64:FM_NSAQ + (h + 1) * 64], writes=["qt%d" % bi])

                                def fin(b, pso, h=h, dst=dst, gcol_=gcol_):
                                    qb = fin.c * 4 + b
                                    P.op("dve", lambda e: e.reciprocal(out=R["rd"][:, b:b + 1], in_=pso[:, 64:65]),
                                         reads=["ps_o%d" % b], writes=["rd%d" % b])
                                    o2 = R["oi"] % 2
                                    R["oi"] += 1
                                    P.op("dve", lambda e: e.tensor_scalar(
                                        out=R["osb"][o2][:], in0=pso[:, 0:64], scalar1=R["rd"][:, b:b + 1],
                                        scalar2=gates[:, qb, h * 3 + gcol_:h * 3 + gcol_ + 1], op0=ALU.mult, op1=ALU.mult),
                                        reads=["ps_o%d" % b, "rd%d" % b, "gates"], writes=["osb%d" % o2])
                                    P.dma("sp", dst[qb * 128:(qb + 1) * 128, h * 64:(h + 1) * 64], R["osb"][o2][:],
                                          reads=["osb%d" % o2], semkey="st_osb%d" % o2)
                                for c in range(NG):
                                    fin.c = c
                                    steps = []
                                    if br == 0:
                                        for j in range(4 * c + 4):
                                            blocks = {}
                                            for b in range(4):
                                                dl = 4 * c + b - j
                                                if dl < 0:
                                                    continue
                                                if dl <= 1:
                                                    blocks[b] = ("mul2", em31[:, h:h + 1], "em31", ewn[:, h * 3 + dl, :], "ewn")
                                                else:
                                                    blocks[b] = ("plain",)
                                            steps.append(dict(
                                                kl=kt[:, j * 128:(j + 1) * 128], kkeys=["kt"],
                                                extra=(esel[:, j * 128:(j + 1) * 128], negmt[:, c * 512:(c + 1) * 512],
                                                       ["esel", "negmt%d" % c]),
                                                bias=(b31[:, h:h + 1], "b31"), v=vt[:, j, :], vkeys=["vt", "vt_1"], vn=65,
                                                blocks=blocks))
                                    else:
                                        for j in range(max(0, 4 * c - 2), 4 * c + 4):
                                            blocks = {}
                                            for b in range(4):
                                                dl = 4 * c + b - j
                                                if 0 <= dl <= 2:
                                                    blocks[b] = ("mul", ewn[:, h * 3 + dl, :], "ewn")
                                            steps.append(dict(kl=kt[:, j * 128:(j + 1) * 128], kkeys=["kt"], v=vt[:, j, :],
                                                              vkeys=["vt", "vt_1"], vn=65, blocks=blocks))
                                    run_chunk(R, steps, qt[bi][:, c * 512:(c + 1) * 512], ["qt%d" % bi], 0.125, fin)
                                P.barrier()
                    P.barrier()
                    P.emit()
        if "C" in stages:
            ocat = ocat_keep[0]
            last = (l == n_layers - 1)
            with contextlib.ExitStack() as st:
                ident = P.sbuf("identc", [128, 128], BF16, st)
                P.dma("sp", ident[:], consts["c_ident"][:, :], writes=["ident"])
                gfin = P.sbuf("gfin", [128, D], F32, st)
                if True:
                    P.dma("sp", gfin[:], final_norm.partition_broadcast(128), writes=["gfin"])
                ot = [P.sbuf("ot%d" % i, [128, D], BF16, st) for i in range(2)]
                on = P.sbuf("on", [128, D], BF16, st)
                oadd = [P.sbuf("oadd%d" % i, [128, 512], BF16, st) for i in range(2)]
                junk = P.sbuf("junkc", [128, D], BF16, st)
                hh = P.sbuf("hh", [128, 4, D], F32, st)
                xT = P.sbuf("xT", [128, 16, 512], BF16, st)
                actT = P.sbuf("actT", [128, 64, 512], BF16, st)
                rl = [P.sbuf("rl%d" % i, [128, 512], F32, st) for i in range(2)]
                ss = P.sbuf("ssc", [128, 8], F32, st)
                wsl = [P.sbuf("wslc%d" % i, [128, 16, 512], BF16, st) for i in range(2)]
                ps_t = [P.psum("pc_t%d" % i, [128, 512], BF16, st) for i in range(2)]
                ps_m = [P.psum("pc_m%d" % i, [128, 512], F32, st) for i in range(2)]
                ps_a = [P.psum("pc_a%d" % i, [128, 512], F32, st) for i in range(4)]
                cnt = {"slab": 0, "pt": 0, "pm": 0, "rl": 0}

                def slab(src_ap):
                    i = cnt["slab"] % 2
                    cnt["slab"] += 1
                    P.dma(None, wsl[i][:], src_ap.rearrange("(c p) n -> p c n", p=128), writes=["wsl%d" % i])
                    return i

                def rstd_of(col, w):
                    P.op("dve", lambda e: e.tensor_scalar(out=ss[:, col:col + 1], in0=ss[:, col:col + 1],
                                                          scalar1=1.0 / w, scalar2=EPS, op0=ALU.mult, op1=ALU.add),
                         reads=["ss%d" % col], writes=["ss%d" % col])
                    P.op("act", lambda e: e.activation(out=ss[:, col:col + 1], in_=ss[:, col:col + 1], func=AF.Sqrt),
                         reads=["ss%d" % col], writes=["ss%d" % col])
                    P.op("dve", lambda e: e.reciprocal(out=ss[:, col:col + 1], in_=ss[:, col:col + 1]),
                         reads=["ss%d" % col], writes=["ss%d" % col])

                def transpose_into(src_tile, src_key, tb):
                    for c4 in range(4):
                        j = cnt["pt"] % 2
                        cnt["pt"] += 1
                        for cc in range(4):
                            c = c4 * 4 + cc
                            P.op("pe", lambda e, c=c, cc=cc, j=j: e.transpose(
                                out=ps_t[j][:, cc * 128:(cc + 1) * 128], in_=src_tile[:, c * 128:(c + 1) * 128],
                                identity=ident[:]), reads=[src_key, "ident"], writes=["ps_t%d" % j])
                        P.op("dve", lambda e, c4=c4, j=j: e.tensor_copy(
                            out=xT[:, c4 * 4:(c4 + 1) * 4, tb * 128:(tb + 1) * 128],
                            in_=ps_t[j][:].rearrange("p (c t) -> p c t", c=4)),
                            reads=["ps_t%d" % j], writes=["xT%d" % tb])

                xTk = ["xT%d" % tb for tb in range(4)]
                for g in range(NG):
                    for tb in range(4):
                        t0 = g * 512 + tb * 128
                        i = tb % 2
                        P.dma("sp", ot[i][:], ocat[t0:t0 + 128, :], writes=["ot%d" % i])
                        P.dma("sp", hh[:, tb, :], hsrc[t0:t0 + 128, :], writes=["hh%d" % tb])
                        for bi_, src_ in enumerate((osel, owin)):
                            P.dma("sp", oadd[bi_][:], src_[t0:t0 + 128, :], writes=["oadd%d" % bi_])
                            P.op("pool", lambda e, i=i, bi_=bi_: e.tensor_tensor(
                                out=ot[i][:, 1024:1536], in0=ot[i][:, 1024:1536], in1=oadd[bi_][:], op=ALU.add),
                                reads=["ot%d" % i, "oadd%d" % bi_], writes=["ot%d" % i])
                        for gi in range(4):
                            P.op("act", lambda e, i=i, gi=gi: e.activation(
                                out=junk[:, 0:512], in_=ot[i][:, gi * 512:(gi + 1) * 512], func=AF.Square,
                                accum_out=ss[:, gi:gi + 1]), reads=["ot%d" % i], writes=["junk", "ss%d" % gi])
                            rstd_of(gi, 512)
                            P.op("dve", lambda e, i=i, gi=gi: e.tensor_scalar(
                                out=on[:, gi * 512:(gi + 1) * 512], in0=ot[i][:, gi * 512:(gi + 1) * 512],
                                scalar1=ss[:, gi:gi + 1], scalar2=None, op0=ALU.mult),
                                reads=["ot%d" % i, "ss%d" % gi], writes=["on"])
                        transpose_into(on, "on", tb)
                    for s in range(4):
                        si = slab(wb_out[l][:, s * 512:(s + 1) * 512])
                        for tb in range(4):
                            j = cnt["pm"] % 2
                            cnt["pm"] += 1
                            for kc in range(16):
                                P.op("pe", lambda e, si=si, tb=tb, kc=kc, j=j: e.matmul(
                                    ps_m[j][:], lhsT=xT[:, kc, tb * 128:(tb + 1) * 128], rhs=wsl[si][:, kc, :],
                                    start=(kc == 0), stop=(kc == 15)),
                                    reads=["wsl%d" % si, "xT%d" % tb], writes=["ps_m%d" % j])
                            P.op("dve", lambda e, tb=tb, s=s, j=j: e.tensor_tensor(
                                out=hh[:, tb, s * 512:(s + 1) * 512], in0=hh[:, tb, s * 512:(s + 1) * 512],
                                in1=ps_m[j][:], op=ALU.add), reads=["ps_m%d" % j, "hh%d" % tb], writes=["hh%d" % tb])
                    for tb in range(4):
                        P.op("act", lambda e, tb=tb: e.activation(out=junk[:], in_=hh[:, tb, :], func=AF.Square,
                                                                  accum_out=ss[:, 4:5]),
                             reads=["hh%d" % tb], writes=["junk", "ss4"])
                        rstd_of(4, D)
                        P.op("dve", lambda e, tb=tb: e.tensor_scalar(out=on[:], in0=hh[:, tb, :], scalar1=ss[:, 4:5],
                                                                     scalar2=None, op0=ALU.mult),
                             reads=["hh%d" % tb, "ss4"], writes=["on"])
                        transpose_into(on, "on", tb)
                    for s in range(16):
                        si = slab(wb_up[l][:, s * 512:(s + 1) * 512])
                        for c in range(4):
                            j = cnt["pm"] % 2
                            cnt["pm"] += 1
                            for kc in range(16):
                                P.op("pe", lambda e, si=si, c=c, kc=kc, j=j: e.matmul(
                                    ps_m[j][:], lhsT=wsl[si][:, kc, c * 128:(c + 1) * 128], rhs=xT[:, kc, :],
                                    start=(kc == 0), stop=(kc == 15)),
                                    reads=["wsl%d" % si] + xTk, writes=["ps_m%d" % j])
                            ri = cnt["rl"] % 2
                            cnt["rl"] += 1
                            P.op("act", lambda e, j=j, ri=ri: e.activation(out=rl[ri][:], in_=ps_m[j][:], func=AF.Relu),
                                 reads=["ps_m%d" % j], writes=["rl%d" % ri])
                            P.op("dve" if c % 2 else "pool", lambda e, ri=ri, s=s, c=c: e.tensor_tensor(
                                out=actT[:, s * 4 + c, :], in0=rl[ri][:], in1=rl[ri][:], op=ALU.mult),
                                reads=["rl%d" % ri], writes=["actT%d" % (s * 4 + c)])
                    for s in range(4):
                        for kq in range(4):
                            si = slab(wb_dn[l][kq * 2048:(kq + 1) * 2048, s * 512:(s + 1) * 512])
                            for tb in range(4):
                                for kc in range(16):
                                    kk = kq * 16 + kc
                                    P.op("pe", lambda e, si=si, tb=tb, kc=kc, kk=kk: e.matmul(
                                        ps_a[tb][:], lhsT=actT[:, kk, tb * 128:(tb + 1) * 128], rhs=wsl[si][:, kc, :],
                                        start=(kk == 0), stop=(kk == 63)),
                                        reads=["wsl%d" % si, "actT%d" % kk], writes=["ps_a%d" % tb])
                        for tb in range(4):
                            P.op("dve", lambda e, tb=tb, s=s: e.tensor_tensor(
                                out=hh[:, tb, s * 512:(s + 1) * 512], in0=hh[:, tb, s * 512:(s + 1) * 512],
                                in1=ps_a[tb][:], op=ALU.add), reads=["ps_a%d" % tb, "hh%d" % tb], writes=["hh%d" % tb])
                    for tb in range(4):
                        t0 = g * 512 + tb * 128
                        if fused and not last:
                            P.dma("pool", hres[t0:t0 + 128, :], hh[:, tb, :], reads=["hh%d" % tb], semkey="st_hh%d" % tb)
                        if not fused:
                            P.dma("pool", hn_out[t0:t0 + 128, :], hh[:, tb, :], reads=["hh%d" % tb], semkey="st_hh%d" % tb)
                        if last or not fused:
                            P.op("act", lambda e, tb=tb: e.activation(out=junk[:], in_=hh[:, tb, :], func=AF.Square,
                                                                      accum_out=ss[:, 5:6]),
                                 reads=["hh%d" % tb], writes=["junk", "ss5"])
                            rstd_of(5, D)
                            P.op("dve", lambda e, tb=tb: e.tensor_scalar(out=hh[:, tb, :], in0=hh[:, tb, :],
                                                                         scalar1=ss[:, 5:6], scalar2=None, op0=ALU.mult),
                                 reads=["hh%d" % tb, "ss5"], writes=["hh%d" % tb])
                            P.op("pool", lambda e, tb=tb: e.tensor_tensor(out=hh[:, tb, :], in0=hh[:, tb, :],
                                                                          in1=gfin[:], op=ALU.mult),
                                 reads=["hh%d" % tb, "gfin"], writes=["hh%d" % tb])
                            P.dma("pool", y_out[t0:t0 + 128, :], hh[:, tb, :], reads=["hh%d" % tb], semkey="st_hy%d" % tb)
                    P.barrier()
                P.barrier()
                P.emit()
    P.barrier()
    P.emit()
    dbg["_max_sem_count"] = P.max_sem_count()
    dbg["_n_sems"] = len(P.sems)
    P.close()
    return nc, dbg


IN_SPLITS = (384, 128, 32, 512, 512, 512, 8, 512, 128, 128, 128, 128, 128, 128, 24, 512, 128, 128)
IN_OFF = np.concatenate([[0], np.cumsum(IN_SPLITS)]).astype(int)


def relayout_weights(w_in, w_ukv):
    L = w_in.shape[0]
    seg = lambda i: w_in[:, :, IN_OFF[i]:IN_OFF[i + 1]]
    w_fm = np.zeros((L, D, NFM), np.float32)
    w_tm = np.zeros((L, D, NTM), np.float32)
    for off, i in ((FM_FOXQ, 3), (FM_FOXK, 4), (FM_NSAQ, 7), (FM_SWAQ, 15), (FM_KC, 8), (FM_VC, 9),
                   (FM_KSL, 10), (FM_KWN, 12), (FM_SWAK, 16), (FM_F, 6)):
        s = seg(i)
        w_fm[:, :, off:off + s.shape[2]] = s
    for off, i in ((TM_CQ, 0), (TM_CKV, 1), (TM_KPE, 2), (TM_FOXV, 5), (TM_VSL, 11), (TM_VWN, 13),
                   (TM_GATE, 14), (TM_SWAV, 17)):
        s = seg(i)
        w_tm[:, :, off:off + s.shape[2]] = s
    kv = w_ukv.reshape(L, 128, 8, 128)
    w_k = np.ascontiguousarray(kv[:, :, :, :64].reshape(L, 128, 512))
    w_v = np.ascontiguousarray(kv[:, :, :, 64:].reshape(L, 128, 512))
    return w_fm, w_tm, w_k, w_v


FUSED = True
L1_IO = dict(pT="out", pTM="out", pTMb="out", ocat="out")
L2_IO = dict(pT="in", pTM="in", pTMb="in", ocat="in")


def build_programs(T):
    nc1, i1 = build(T, stages="AB", io=L1_IO)
    nc2, i2 = build(T, stages="SNC", io=L2_IO)
    return (nc1, i1["_inputs"]), (nc2, i2["_inputs"])


def run_layer(progs, h, W, l, T, last):
    (nc1, in1), (nc2, in2) = progs
    B = h.shape[0]
    base = layer_inputs(None, W, l, T)
    maps1 = [dict({k: base[k] for k in in1 if k != "x"}, x=np.ascontiguousarray(h[b])) for b in range(B)]
    r1 = run_bass_kernel_spmd(nc1, maps1, core_ids=list(range(B))).results
    maps2 = []
    for b in range(B):
        m = {k: base[k] for k in in2 if k in base}
        m["x"] = np.ascontiguousarray(h[b])
        m["pT"], m["pTM"], m["pTMb"] = r1[b]["pT"], r1[b]["pTM"], r1[b]["pTMb"]
        m["ocat_in"] = r1[b]["ocat"]
        maps2.append(m)
    r2 = run_bass_kernel_spmd(nc2, maps2, core_ids=list(range(B))).results
    key = "y" if last else "hn"
    return np.stack([np.asarray(r2[b][key]) for b in range(B)], axis=0)


def kernel(x, norm_attn, w_in, mla_q_norm, mla_w_uq, mla_kv_norm, mla_w_ukv, fox_b_f,
           nsa_cmp_pos, nsa_cmp_w1, nsa_cmp_w2, swa_sinks, group_norm, w_out,
           norm_mlp, w_up, w_down, rel_bias, final_norm):
    W = dict(norm_attn=norm_attn, w_in=w_in, mla_q_norm=mla_q_norm, mla_w_uq=mla_w_uq,
             mla_kv_norm=mla_kv_norm, mla_w_ukv=mla_w_ukv, fox_b_f=fox_b_f, nsa_cmp_pos=nsa_cmp_pos,
             nsa_cmp_w1=nsa_cmp_w1, nsa_cmp_w2=nsa_cmp_w2, swa_sinks=swa_sinks, group_norm=group_norm,
             w_out=w_out, norm_mlp=norm_mlp, w_up=w_up, w_down=w_down, rel_bias=rel_bias,
             final_norm=final_norm)
    W = {k: np.asarray(v) for k, v in W.items()}
    h = np.asarray(x, dtype=np.float32)
    B, T, _ = h.shape
    depth = W["w_in"].shape[0]
    if FUSED:
        nc, info = build(T, n_layers=depth, stages="ABSNC")
        base = layer_inputs(None, W, 0, T, nl=depth)
        maps = [dict({k: base[k] for k in info["_inputs"] if k != "x"}, x=np.ascontiguousarray(h[b]))
                for b in range(B)]
        res = run_bass_kernel_spmd(nc, maps, core_ids=list(range(B))).results
        return np.stack([np.asarray(res[b]["y"]) for b in range(B)], axis=0).astype(np.float32)
    progs = build_programs(T)
    for l in range(depth):
        h = run_layer(progs, h, W, l, T, last=(l == depth - 1))
    return h.astype(np.float32)


def layer_inputs(h, W, l, T, nl=1):
    w_fm, w_tm, w_k, w_v = relayout_weights(np.asarray(W["w_in"][l:l + nl]), np.asarray(W["mla_w_ukv"][l:l + nl]))
    m = dict(w_fm=w_fm, w_tm=w_tm, w_ukv_k=w_k, w_ukv_v=w_v)
    if h is not None:
        m["x"] = np.ascontiguousarray(h, dtype=np.float32)
    for k in ("norm_attn", "mla_q_norm", "mla_w_uq", "mla_kv_norm", "fox_b_f", "group_norm", "w_out", "norm_mlp",
              "w_up", "w_down", "nsa_cmp_pos", "nsa_cmp_w1", "nsa_cmp_w2", "swa_sinks"):
        m[k] = np.ascontiguousarray(np.asarray(W[k])[l:l + nl], dtype=np.float32)
    m["rel_bias"] = np.asarray(W["rel_bias"], np.float32)
    m["final_norm"] = np.asarray(W["final_norm"], np.float32)
    m.update(host_consts(T))
    return m
```

```python
import contextlib
import math
import numpy as np
import ml_dtypes
import concourse.bass as bass
import concourse.mybir as mybir
from concourse.bass_utils import run_bass_kernel_spmd

F32 = mybir.dt.float32
BF16 = mybir.dt.bfloat16
I32 = mybir.dt.int32
AF = mybir.ActivationFunctionType
ALU = mybir.AluOpType
AX = mybir.AxisListType
ENGS = ("pe", "act", "dve", "pool", "sp")

D = 2048
DEPTH = 2
HD = 64
NH = 8
DFF = 8192
EPS = 1e-6
NFM = 2816
NTM = 1536
FM_FOXQ, FM_FOXK, FM_NSAQ, FM_SWAQ = 0, 512, 1024, 1536
FM_KC, FM_VC, FM_KSL, FM_KWN, FM_SWAK, FM_F = 2048, 2176, 2304, 2432, 2560, 2688
TM_CQ, TM_CKV, TM_KPE, TM_FOXV, TM_VSL, TM_VWN, TM_GATE, TM_SWAV = 0, 384, 512, 544, 1056, 1184, 1312, 1336


class Prog:
    ENG_SWITCH = 8000
    DMA_SWITCH = 12000
    LIMIT = 30000

    def __init__(self, nc):
        self.nc = nc
        self.stack = contextlib.ExitStack()
        self.ops = {e: [] for e in ENGS}
        self.count = {e: 0 for e in ENGS}
        self.gen = {e: 0 for e in ENGS}
        self.dgen = {}
        self.seen = {e: {} for e in ENGS}
        self.last_w = {}
        self.readers = {}
        self.dma_cnt = {}
        self.sems = {}
        self.n_inst = 0
        self.rr = 0
        self.rr_name = 0
        self.keymap = {}
        self.pe_entries = {}
        self.pe_marks = []
        self.maxcount = 0

    def sem(self, key):
        if key not in self.sems:
            self.sems[key] = self.stack.enter_context(
                self.nc.semaphore("s%d" % len(self.sems)))
        return self.sems[key]

    def sbuf(self, name, shape, dtype, stack=None):
        st = stack or self.stack
        self.rr_name += 1
        return st.enter_context(self.nc.sbuf_tensor("%s_u%d" % (name, self.rr_name), list(shape), dtype))

    def psum(self, name, shape, dtype, stack=None):
        st = stack or self.stack
        self.rr_name += 1
        return st.enter_context(self.nc.psum_tensor("%s_u%d" % (name, self.rr_name), list(shape), dtype))

    def _pe_value(self, idx):
        if self.pe_marks and self.pe_marks[-1][0] >= idx:
            lo, hi = 0, len(self.pe_marks) - 1
            while lo < hi:
                mid = (lo + hi) // 2
                if self.pe_marks[mid][0] >= idx:
                    hi = mid
                else:
                    lo = mid + 1
            return self.pe_marks[lo][1]
        val = len(self.pe_marks) + 1
        self.pe_marks.append((idx, val))
        self.pe_entries[idx][3] = True
        self.maxcount = max(self.maxcount, val)
        return val

    def _deps(self, eng, reads, writes):
        deps = {}

        def add(d):
            if d is None:
                return
            k, v = d
            if deps.get(k, 0) < v:
                deps[k] = v
        for b in reads:
            add(self.last_w.get(b))
        for b in writes:
            add(self.last_w.get(b))
            for k, v in self.readers.get(b, {}).items():
                add((k, v))
        waits = []
        for k, v in deps.items():
            if k[0] == "eng" and k[1] == "pe":
                if eng == "pe":
                    continue
                v = self._pe_value(v)
            if self.seen[eng].get(k, 0) < v:
                self.seen[eng][k] = v
                waits.append((k, v))
        return waits

    def _commit(self, me, reads, writes):
        for b in writes:
            self.last_w[b] = me
            self.readers[b] = {}
        for b in reads:
            r = self.readers.setdefault(b, {})
            if r.get(me[0], 0) < me[1]:
                r[me[0]] = me[1]

    def op(self, eng, fn, reads=(), writes=()):
        waits = self._deps(eng, reads, writes)
        self.count[eng] += 1
        key = ("eng", eng, self.gen[eng])
        self.sem(key)
        me = (key, self.count[eng])
        rec = ["op", fn, waits, eng != "pe", key]
        if eng == "pe":
            self.pe_entries[self.count[eng]] = rec
        else:
            self.maxcount = max(self.maxcount, self.count[eng])
        self.ops[eng].append(rec)
        self._commit(me, reads, writes)
        self.n_inst += 1 + len(waits)

    def dma(self, eng, out, in_, reads=(), writes=(), semkey=None, **kw):
        if eng is None:
            eng = ("sp", "pool")[self.rr % 2]
            self.rr += 1
        waits = self._deps(eng, reads, writes)
        lkey = semkey if semkey is not None else (writes[0] if writes else reads[0])
        if lkey not in self.keymap:
            self.keymap[lkey] = len(self.keymap)
        idx = self.keymap[lkey]
        key = ("dma", idx, self.dgen.get(idx, 0))
        self.sem(key)
        self.dma_cnt[key] = self.dma_cnt.get(key, 0) + 1
        self.maxcount = max(self.maxcount, 16 * self.dma_cnt[key])
        me = (key, 16 * self.dma_cnt[key])
        self.ops[eng].append(["dma", (out, in_, kw), waits, None, key])
        self._commit(me, reads, writes)
        self.n_inst += 1 + len(waits)

    def barrier(self):
        allk = {}
        for e in ENGS:
            if self.count[e]:
                allk[("eng", e, self.gen[e])] = self._pe_value(self.count[e]) if e == "pe" else self.count[e]
        for k, c in self.dma_cnt.items():
            allk[k] = 16 * c
        assert max(allk.values() or [0]) < self.LIMIT, ("semaphore count too large", max(allk.values()))
        for e in ENGS:
            waits = []
            for k, v in allk.items():
                if self.seen[e].get(k, 0) < v:
                    self.seen[e][k] = v
                    waits.append((k, v))
            if waits:
                self.ops[e].append(["wait", None, waits, None, None])
                self.n_inst += len(waits)
        self.last_w = {}
        self.readers = {}

    def max_sem_count(self):
        return self.maxcount

    def emit(self):
        nc = self.nc
        with nc.Block() as block:
            def body(ename):
                def run(eng):
                    for kind, payload, waits, flag, key in self.ops[ename]:
                        for k, v in waits:
                            eng.wait_ge(self.sems[k], v)
                        if kind == "op":
                            ins = payload(eng)
                            if flag:
                                ins.then_inc(self.sems[key], 1)
                        elif kind == "dma":
                            out, in_, kw = payload
                            eng.dma_start(out=out, in_=in_, **kw).then_inc(self.sems[key], 16)
                return run
            block.tensor(body("pe"))
            block.scalar(body("act"))
            block.vector(body("dve"))
            block.gpsimd(body("pool"))
            block.sync(body("sp"))
        self.ops = {e: [] for e in ENGS}
        for e in ENGS:
            val = len(self.pe_marks) if e == "pe" else self.count[e]
            if val > self.ENG_SWITCH:
                self.gen[e] += 1
                self.count[e] = 0
                if e == "pe":
                    self.pe_marks = []
                    self.pe_entries = {}
        for (tag, idx, g), c in list(self.dma_cnt.items()):
            if g == self.dgen.get(idx, 0) and 16 * c > self.DMA_SWITCH:
                self.dgen[idx] = g + 1
        self.keymap = {}

    def close(self):
        self.stack.close()


def t5_bucket_np(dist):
    d = np.maximum(dist, 0)
    large = 16 + (np.log(np.maximum(d, 1).astype(np.float32) / np.float32(16))
                  / np.float32(math.log(128 / 16)) * np.float32(16)).astype(np.int32)
    large = np.minimum(large, 31)
    return np.where(d < 16, d, large)


def host_consts(T):
    bf = ml_dtypes.bfloat16
    c = {}
    k = np.arange(128)[:, None]
    q = np.arange(128)[None, :]
    c["c_tri"] = (k <= q).astype(bf)
    c["c_ident"] = np.eye(128, dtype=bf)
    c["c_identf"] = np.eye(128, dtype=np.float32)
    inv = (10000.0 ** (-np.arange(0, 32, 2, dtype=np.float32) / np.float32(32))).astype(np.float32)
    ang = np.arange(T, dtype=np.float32)[None, :] * inv[:, None]
    cos, sin = np.cos(ang).astype(np.float32), np.sin(ang).astype(np.float32)
    cos96 = np.ones((96, T), np.float32)
    sin96 = np.zeros((96, T), np.float32)
    cos96[64:80], cos96[80:96] = cos, cos
    sin96[64:80], sin96[80:96] = sin, sin
    c["c_cos96"], c["c_sin96"] = cos96, sin96
    R = np.zeros((96, 96), np.float32)
    for i in range(16):
        R[64 + 16 + i, 64 + i] = -1.0
        R[64 + i, 64 + 16 + i] = 1.0
    c["c_rot96"] = R.astype(bf)
    LW, LC = 512, 4352
    dw = np.arange(LW) - 127
    ohw = np.zeros((32, LW), np.float32)
    ohw[t5_bucket_np(dw), np.arange(LW)] = 1.0
    ohw[:, dw < 0] = 0.0
    c["c_ohw"] = ohw
    vw = np.zeros((16, LW), np.float32)
    vw[0:8] = ((dw >= 0) & (dw < 256)).astype(np.float32)
    vw[8:16] = ((dw >= 0) & (dw < 128)).astype(np.float32)
    c["c_validw"] = vw
    dc = np.arange(LC) - 2063
    ohc = np.zeros((32, LC), np.float32)
    ohc[t5_bucket_np(dc), np.arange(LC)] = 1.0
    ohc[:, dc < 0] = 0.0
    c["c_ohc"] = ohc
    c["c_validc"] = np.broadcast_to((dc >= 0).astype(np.float32), (8, LC)).copy()
    kk = np.arange(T)
    c["c_esel"] = (kk[None, :] // 64 == np.arange(128)[:, None]).astype(bf)
    ci = np.arange(512)[:, None]
    sj = np.arange(128)[None, :]
    ncmp = (T - 32) // 16 + 1
    ov = ((ci * 16 + 31 >= sj * 64) & (ci * 16 <= sj * 64 + 63) & (ci < ncmp))
    c["c_overlap"] = ov.astype(bf)
    qpos = np.arange(T)[:, None]
    cur = qpos // 64
    s = np.arange(128)[None, :]
    forced = (s == 0) | (s == cur) | (s == cur - 1)
    valid = s <= cur
    add = np.where(valid, np.where(forced, np.float32(1e9), np.float32(0.0)), np.float32(-1.0)).astype(np.float32)
    c["c_topadd"] = add.reshape(T // 128, 128, 128)
    return c


def build(T, n_layers=1, stages="ABC", debug=False, io=None):
    NLP = n_layers
    fused = n_layers > 1
    io = io or {}
    nc = bass.Bass("TRN2", target_bir_lowering=False)
    NT = T // 128
    NG = T // 512
    P = Prog(nc)
    dt = nc.dram_tensor
    in_names = []
    dbg = {}
    sA, sB, sS, sN, sC = [c in stages for c in "ABSNC"]

    def din(name, shape, dtype=F32, need=True):
        if not need:
            return None
        in_names.append(name)
        return dt(name, list(shape), dtype, kind="ExternalInput").ap()

    def dsc(name, shape, dtype):
        return dt(name, list(shape), dtype).ap()

    def dout(name, shape, dtype=F32):
        a = dt(name, list(shape), dtype, kind="ExternalOutput").ap()
        dbg[name] = a
        return a

    def mk(name, shape, dtype):
        kind = io.get(name)
        if kind == "in":
            return din(name, shape, dtype)
        if kind == "out" or debug:
            return dout(name, shape, dtype)
        return dsc(name, shape, dtype)

    x = din("x", [T, D], need=sA or sC)
    w_fm = din("w_fm", [NLP, D, NFM], need=sA)
    w_tm = din("w_tm", [NLP, D, NTM], need=sA)
    norm_attn = din("norm_attn", [NLP, D], need=sA)
    q_norm = din("mla_q_norm", [NLP, 384], need=sA)
    w_uq = din("mla_w_uq", [NLP, 384, 768], need=sA)
    kv_norm = din("mla_kv_norm", [NLP, 128], need=sA)
    w_ukv_k = din("w_ukv_k", [NLP, 128, 512], need=sA)
    w_ukv_v = din("w_ukv_v", [NLP, 128, 512], need=sA)
    fox_b = din("fox_b_f", [NLP, 8], need=sB)
    group_norm = din("group_norm", [NLP, D], need=sC)
    w_out = din("w_out", [NLP, D, D], need=sC)
    norm_mlp = din("norm_mlp", [NLP, D], need=sC)
    w_up = din("w_up", [NLP, D, DFF], need=sC)
    w_down = din("w_down", [NLP, DFF, D], need=sC)
    final_norm = din("final_norm", [D], need=sC)
    nsa_pos = din("nsa_cmp_pos", [NLP, 2, 32, 64], need=sN)
    nsa_w1 = din("nsa_cmp_w1", [NLP, 2, 2048, 64], need=sN)
    nsa_w2 = din("nsa_cmp_w2", [NLP, 2, 64, 64], need=sN)
    swa_sinks = din("swa_sinks", [NLP, 8], need=sS)
    rel_bias = din("rel_bias", [32, 16], need=sS or sN)
    if sC:
        y_out = dt("y", [T, D], F32, kind="ExternalOutput").ap()
        if fused:
            hres = dsc("hres", [T, D], F32)
        else:
            hn_out = dt("hn", [T, D], F32, kind="ExternalOutput").ap()
        wb_out = dsc("wb_out", [NLP, D, D], BF16)
        wb_up = dsc("wb_up", [NLP, D, DFF], BF16)
        wb_dn = dsc("wb_dn", [NLP, DFF, D], BF16)
    consts = {}
    hc = host_consts(T)
    need_c = {"c_tri": sB, "c_ident": sA or sC, "c_identf": sN, "c_cos96": sA, "c_sin96": sA, "c_rot96": sA,
              "c_ohw": sS or sN, "c_validw": sS or sN, "c_ohc": sS or sN, "c_validc": sS or sN,
              "c_esel": sN, "c_overlap": sN, "c_topadd": sN}
    for k_, v_ in hc.items():
        consts[k_] = din(k_, v_.shape, BF16 if v_.dtype == ml_dtypes.bfloat16 else F32, need=need_c.get(k_, True))
    ocat_keep = [None]
    vstage_keep = [None]

    if sA:
        wb_fm = dsc("wb_fm", [NLP, D, NFM], BF16)
        wb_tm = dsc("wb_tm", [NLP, D, NTM], BF16)
        wb_uq = dsc("wb_uq", [NLP, 384, 768], BF16)
        wb_ukvk = dsc("wb_ukvk", [NLP, 128, 512], BF16)
        wb_ukvv = dsc("wb_ukvv", [NLP, 128, 512], BF16)
    pT = mk("pT", [NFM - 128, T], BF16)
    fT = mk("fT", [8, T], F32)
    pTM = mk("pTM", [T, NTM], F32)
    pTMb = mk("pTMb", [T, NTM], BF16)
    osel = mk("osel", [T, 512], BF16)
    owin = mk("owin", [T, 512], BF16)
    qT_mla = mk("qT_mla", [8, 96, T], BF16)
    kT_mla = mk("kT_mla", [8, 64, T], BF16)
    kpeT = mk("kpeT", [32, T], BF16)
    v_mla = mk("v_mla", [T, 512], BF16)
    foxq_aug = mk("foxq_aug", [8, 6, T], BF16)
    foxk_aug = mk("foxk_aug", [8, 6, T], BF16)
    if io.get("ocat") == "in":
        ocat_in = din("ocat_in", [T, D], BF16)
        ocat = dsc("ocat", [T, D], BF16)
        P.dma("sp", ocat[:, 0:1024], ocat_in[:, 0:1024], semkey="cp_ocat")
        P.barrier()
    else:
        ocat = mk("ocat", [T, D], BF16)
    ocat_keep[0] = ocat
    dbg["_inputs"] = in_names

    with contextlib.ExitStack() as st:
        gcol = P.sbuf("gcol", [128, 64], F32, st)
        stage_f = [P.sbuf("wst%d" % i, [128, 2816], F32, st) for i in range(2)]
        stage_b = [P.sbuf("wsb%d" % i, [128, 2816], BF16, st) for i in range(2)]
        cnt = [0]

        def prep(src, dst, rows, cols, gain_ap, gkey):
            nch = rows // 128
            if gain_ap is not None:
                P.dma("sp", gcol[:, 0:nch], gain_ap.rearrange("(c p) -> p c", p=128),
                      writes=["gcol"], allow_slow_non_contiguous=True)
            for c in range(nch):
                i = cnt[0] % 2
                cnt[0] += 1
                P.dma("sp", stage_f[i][:, 0:cols], src[c * 128:(c + 1) * 128, :], writes=["wst%d" % i])
                if gain_ap is not None:
                    eng = "dve" if c % 2 == 0 else "pool"
                    P.op(eng, lambda e, i=i, c=c: e.tensor_scalar(
                        out=stage_b[i][:, 0:cols], in0=stage_f[i][:, 0:cols],
                        scalar1=gcol[:, c:c + 1], scalar2=None, op0=ALU.mult),
                        reads=["wst%d" % i, "gcol"], writes=["wsb%d" % i])
                else:
                    P.op("act", lambda e, i=i: e.copy(out=stage_b[i][:, 0:cols], in_=stage_f[i][:, 0:cols]),
                         reads=["wst%d" % i], writes=["wsb%d" % i])
                P.dma("pool", dst[c * 128:(c + 1) * 128, :], stage_b[i][:, 0:cols],
                      reads=["wsb%d" % i], semkey="wprep_st%d" % i)

        for l in range(n_layers):
            if sA:
                prep(w_fm[l], wb_fm[l], D, NFM, norm_attn[l], "ga")
                prep(w_tm[l], wb_tm[l], D, NTM, norm_attn[l], "ga")
                prep(w_uq[l], wb_uq[l], 384, 768, q_norm[l], "gq")
                prep(w_ukv_k[l], wb_ukvk[l], 128, 512, kv_norm[l], "gk")
                prep(w_ukv_v[l], wb_ukvv[l], 128, 512, kv_norm[l], "gk")
            if "C" in stages:
                prep(w_out[l], wb_out[l], D, D, group_norm[l], "gg")
                for c0 in range(0, DFF, 2048):
                    prep(w_up[l][:, c0:c0 + 2048], wb_up[l][:, c0:c0 + 2048], D, 2048, norm_mlp[l], "gm")
                prep(w_down[l], wb_dn[l], DFF, D, None, None)
        P.barrier()
        P.emit()

    for l in range(n_layers):
        hsrc = x if l == 0 else hres
        if "A" in stages:
            with contextlib.ExitStack() as st:
                ident = P.sbuf("ident", [128, 128], BF16, st)
                rot96 = P.sbuf("rot96", [96, 96], BF16, st)
                P.dma("sp", ident[:], consts["c_ident"][:, :], semkey="ld_const")
                P.dma("sp", rot96[:], consts["c_rot96"][:, :], semkey="ld_const")
                rot32 = P.sbuf("rot32", [32, 32], BF16, st)
                P.dma("sp", rot32[:], consts["c_rot96"][64:96, 64:96], semkey="ld_const")
                cos32 = P.sbuf("cos32", [32, 512], F32, st)
                sin32 = P.sbuf("sin32", [32, 512], F32, st)
                hx = [P.sbuf("hx%d" % i, [128, D], F32, st) for i in range(2)]
                ub = [P.sbuf("ub%d" % i, [128, D], BF16, st) for i in range(2)]
                junk = P.sbuf("junk", [128, D], BF16, st)
                ss = P.sbuf("ss", [128, 8], F32, st)
                uT = P.sbuf("uT", [128, 16, 512], BF16, st)
                wsl = [P.sbuf("wsl%d" % i, [128, 16, 512], BF16, st) for i in range(2)]
                pfm = [P.sbuf("pfm%d" % i, [128, 512], BF16, st) for i in range(2)]
                pff = P.sbuf("pff", [128, 512], F32, st)
                ptm = [P.sbuf("ptm%d" % i, [128, 512], F32, st) for i in range(2)]
                ptmb = [P.sbuf("ptmb%d" % i, [128, 512], BF16, st) for i in range(2)]
                mla_in = P.sbuf("mla_in", [128, 4, 544], F32, st)
                cqn = P.sbuf("cqn", [128, 544], BF16, st)
                cT = P.sbuf("cT", [128, 5, 512], BF16, st)
                wuq = P.sbuf("wuq", [128, 3, 768], BF16, st)
                wkk = P.sbuf("wkk", [128, 512], BF16, st)
                wkv = P.sbuf("wkv", [128, 512], BF16, st)
                cos_t = P.sbuf("cos_t", [96, 512], F32, st)
                sin_t = P.sbuf("sin_t", [96, 512], F32, st)
                qsb = P.sbuf("qsb", [96, 512], BF16, st)
                qf1 = P.sbuf("qf1", [96, 512], F32, st)
                qf2 = P.sbuf("qf2", [96, 512], F32, st)
                qo = [P.sbuf("qo%d" % i, [96, 512], BF16, st) for i in range(2)]
                ps_t = [P.psum("ps_t%d" % i, [128, 512], BF16, st) for i in range(2)]
                ps_m = [P.psum("ps_m%d" % i, [128, 512], F32, st) for i in range(3)]
                P.dma("sp", wuq[:], wb_uq[l].rearrange("(c p) n -> p c n", p=128), semkey="ld_const")
                P.dma("sp", wkk[:], wb_ukvk[l], semkey="ld_const")
                P.dma("sp", wkv[:], wb_ukvv[l], semkey="ld_const")
                P.barrier()
                slab_i = [0]
                pm_i = [0]
                pt_i = [0]

                def load_slab(wsrc, c0, ncols):
                    i = slab_i[0] % 2
                    slab_i[0] += 1
                    P.dma(None, wsl[i][:, :, 0:ncols],
                          wsrc[:, c0:c0 + ncols].rearrange("(c p) n -> p c n", p=128),
                          writes=["wsl%d" % i])
                    return i

                for g in range(NG):
                    for tb in range(4):
                        t0 = g * 512 + tb * 128
                        i = tb % 2
                        P.dma("sp", hx[i][:], hsrc[t0:t0 + 128, :], writes=["hx%d" % i])
                        P.op("act", lambda e, i=i, tb=tb: e.activation(
                            out=junk[:], in_=hx[i][:], func=AF.Square, accum_out=ss[:, tb:tb + 1]),
                            reads=["hx%d" % i], writes=["junk", "ss%d" % tb])
                        P.op("dve", lambda e, tb=tb: e.tensor_scalar(
                            out=ss[:, tb:tb + 1], in0=ss[:, tb:tb + 1], scalar1=1.0 / D, scalar2=EPS,
                            op0=ALU.mult, op1=ALU.add), reads=["ss%d" % tb], writes=["ss%d" % tb])
                        P.op("act", lambda e, tb=tb: e.activation(
                            out=ss[:, tb:tb + 1], in_=ss[:, tb:tb + 1], func=AF.Sqrt),
                            reads=["ss%d" % tb], writes=["ss%d" % tb])
                        P.op("dve", lambda e, tb=tb: e.reciprocal(out=ss[:, tb:tb + 1], in_=ss[:, tb:tb + 1]),
                             reads=["ss%d" % tb], writes=["ss%d" % tb])
                        P.op("dve", lambda e, i=i, tb=tb: e.tensor_scalar(
                            out=ub[i][:], in0=hx[i][:], scalar1=ss[:, tb:tb + 1], scalar2=None, op0=ALU.mult),
                            reads=["hx%d" % i, "ss%d" % tb], writes=["ub%d" % i])
                        for c4 in range(4):
                            j = pt_i[0] % 2
                            pt_i[0] += 1
                            for cc in range(4):
                                c = c4 * 4 + cc
                                P.op("pe", lambda e, i=i, c=c, cc=cc, j=j: e.transpose(
                                    out=ps_t[j][:, cc * 128:(cc + 1) * 128], in_=ub[i][:, c * 128:(c + 1) * 128],
                                    identity=ident[:]), reads=["ub%d" % i, "ident"], writes=["ps_t%d" % j])
                            P.op("act" if c4 % 2 else "dve", lambda e, c4=c4, tb=tb, j=j: (
                                e.copy if hasattr(e, "copy") and not hasattr(e, "tensor_copy") else e.tensor_copy)(
                                out=uT[:, c4 * 4:(c4 + 1) * 4, tb * 128:(tb + 1) * 128],
                                in_=ps_t[j][:].rearrange("p (c t) -> p c t", c=4)),
                                reads=["ps_t%d" % j], writes=["uT%d" % tb])
                    uTk = ["uT%d" % tb for tb in range(4)]
                    for s in range(NFM // 512 + 1):
                        ncols = min(512, NFM - s * 512)
                        si = load_slab(wb_fm[l], s * 512, ncols)
                        for c in range(ncols // 128):
                            row0 = s * 512 + c * 128
                            j = pm_i[0] % 3
                            pm_i[0] += 1
                            for kc in range(16):
                                P.op("pe", lambda e, si=si, c=c, kc=kc, j=j: e.matmul(
                                    ps_m[j][:], lhsT=wsl[si][:, kc, c * 128:(c + 1) * 128], rhs=uT[:, kc, :],
                                    start=(kc == 0), stop=(kc == 15)),
                                    reads=["wsl%d" % si] + uTk, writes=["ps_m%d" % j])
                            if row0 == FM_F:
                                P.op("act", lambda e, j=j: e.copy(out=pff[0:8, :], in_=ps_m[j][0:8, :]),
                                     reads=["ps_m%d" % j], writes=["pff"])
                                P.dma("pool", fT[:, g * 512:(g + 1) * 512], pff[0:8, :], reads=["pff"], semkey="st_pff")
                            else:
                                i2 = (row0 // 128) % 2
                                P.op("act" if i2 else "dve", lambda e, j=j, i2=i2: (
                                    e.copy if not hasattr(e, "tensor_copy") else e.tensor_copy)(
                                    out=pfm[i2][:], in_=ps_m[j][:]),
                                    reads=["ps_m%d" % j], writes=["pfm%d" % i2])
                                P.dma("pool", pT[row0:row0 + 128, g * 512:(g + 1) * 512], pfm[i2][:],
                                      reads=["pfm%d" % i2], semkey="st_pfm%d" % i2)
                    for s in range(NTM // 512):
                        si = load_slab(wb_tm[l], s * 512, 512)
                        for tb in range(4):
                            t0 = g * 512 + tb * 128
                            j = pm_i[0] % 3
                            pm_i[0] += 1
                            for kc in range(16):
                                P.op("pe", lambda e, si=si, tb=tb, kc=kc, j=j: e.matmul(
                                    ps_m[j][:], lhsT=uT[:, kc, tb * 128:(tb + 1) * 128], rhs=wsl[si][:, kc, :],
                                    start=(kc == 0), stop=(kc == 15)),
                                    reads=["wsl%d" % si, "uT%d" % tb], writes=["ps_m%d" % j])
                            i2 = tb % 2
                            P.op("act" if i2 else "dve", lambda e, j=j, i2=i2: (
                                e.copy if not hasattr(e, "tensor_copy") else e.tensor_copy)(
                                out=ptm[i2][:], in_=ps_m[j][:]),
                                reads=["ps_m%d" % j], writes=["ptm%d" % i2])
                            P.dma("pool", pTM[t0:t0 + 128, s * 512:(s + 1) * 512], ptm[i2][:],
                                  reads=["ptm%d" % i2], semkey="st_ptm%d" % i2)
                            P.op("pool", lambda e, i2=i2: e.tensor_copy(out=ptmb[i2][:], in_=ptm[i2][:]),
                                 reads=["ptm%d" % i2], writes=["ptmb%d" % i2])
                            P.dma("sp", pTMb[t0:t0 + 128, s * 512:(s + 1) * 512], ptmb[i2][:],
                                  reads=["ptmb%d" % i2], semkey="st_ptmb%d" % i2)
                            if s == 0:
                                P.op("dve", lambda e, i2=i2, tb=tb: e.tensor_copy(
                                    out=mla_in[:, tb, 0:512], in_=ptm[i2][:]),
                                    reads=["ptm%d" % i2], writes=["mla_in%d" % tb])
                            if s == 1:
                                P.op("dve", lambda e, i2=i2, tb=tb: e.tensor_copy(
                                    out=mla_in[:, tb, 512:544], in_=ptm[i2][:, 0:32]),
                                    reads=["ptm%d" % i2], writes=["mla_in%d" % tb])
                    for tb in range(4):
                        for (o0, w, col) in ((0, 384, 4), (384, 128, 5)):
                            P.op("act", lambda e, tb=tb, o0=o0, w=w, col=col: e.activation(
                                out=junk[:, 0:w], in_=mla_in[:, tb, o0:o0 + w], func=AF.Square,
                                accum_out=ss[:, col:col + 1]),
                                reads=["mla_in%d" % tb], writes=["junk", "ssm%d" % col])
                            P.op("dve", lambda e, w=w, col=col: e.tensor_scalar(
                                out=ss[:, col:col + 1], in0=ss[:, col:col + 1], scalar1=1.0 / w, scalar2=EPS,
                                op0=ALU.mult, op1=ALU.add), reads=["ssm%d" % col], writes=["ssm%d" % col])
                            P.op("act", lambda e, col=col: e.activation(
                                out=ss[:, col:col + 1], in_=ss[:, col:col + 1], func=AF.Sqrt),
                                reads=["ssm%d" % col], writes=["ssm%d" % col])
                            P.op("dve", lambda e, col=col: e.reciprocal(out=ss[:, col:col + 1], in_=ss[:, col:col + 1]),
                                 reads=["ssm%d" % col], writes=["ssm%d" % col])
                            P.op("dve", lambda e, tb=tb, o0=o0, w=w, col=col: e.tensor_scalar(
                                out=cqn[:, o0:o0 + w], in0=mla_in[:, tb, o0:o0 + w], scalar1=ss[:, col:col + 1],
                                scalar2=None, op0=ALU.mult),
                                reads=["mla_in%d" % tb, "ssm%d" % col], writes=["cqn"])
                        P.op("dve", lambda e, tb=tb: e.tensor_copy(out=cqn[:, 512:544], in_=mla_in[:, tb, 512:544]),
                             reads=["mla_in%d" % tb], writes=["cqn"])
                        j = pt_i[0] % 2
                        pt_i[0] += 1
                        for c in range(4):
                            P.op("pe", lambda e, c=c, j=j: e.transpose(
                                out=ps_t[j][:, c * 128:(c + 1) * 128], in_=cqn[:, c * 128:(c + 1) * 128],
                                identity=ident[:]), reads=["cqn", "ident"], writes=["ps_t%d" % j])
                        P.op("dve", lambda e, tb=tb, j=j: e.tensor_copy(
                            out=cT[:, 0:4, tb * 128:(tb + 1) * 128],
                            in_=ps_t[j][:].rearrange("p (c t) -> p c t", c=4)),
                            reads=["ps_t%d" % j], writes=["cT%d" % tb])
                        j = pt_i[0] % 2
                        pt_i[0] += 1
                        P.op("pe", lambda e, j=j: e.transpose(
                            out=ps_t[j][0:32, 0:128], in_=cqn[:, 512:544], identity=ident[:]),
                            reads=["cqn", "ident"], writes=["ps_t%d" % j])
                        P.op("dve", lambda e, tb=tb, j=j: e.tensor_copy(
                            out=cT[0:32, 4, tb * 128:(tb + 1) * 128], in_=ps_t[j][0:32, 0:128]),
                            reads=["ps_t%d" % j], writes=["cT%d" % tb])
                    cTk = ["cT%d" % tb for tb in range(4)]
                    P.dma("sp", cos_t[:], consts["c_cos96"][:, g * 512:(g + 1) * 512], writes=["ropetab"])
                    P.dma("sp", sin_t[:], consts["c_sin96"][:, g * 512:(g + 1) * 512], writes=["ropetab"])

                    P.dma("sp", cos32[:], consts["c_cos96"][64:96, g * 512:(g + 1) * 512], writes=["ropetab"])
                    P.dma("sp", sin32[:], consts["c_sin96"][64:96, g * 512:(g + 1) * 512], writes=["ropetab"])

                    def rope_out(src_key, src_ap, rows, rot_ap, rot_key, cos_ap, sin_ap, ckeys, dst_ap):
                        P.op("act", lambda e: e.copy(out=qsb[0:rows, :], in_=src_ap),
                             reads=(cTk if src_key == "cTall" else [src_key]), writes=["qsb"])
                        jj = pm_i[0] % 3
                        pm_i[0] += 1
                        P.op("pe", lambda e, jj=jj: e.matmul(
                            ps_m[jj][0:rows, :], lhsT=rot_ap, rhs=qsb[0:rows, :], start=True, stop=True),
                            reads=["qsb", rot_key], writes=["ps_m%d" % jj])
                        P.op("dve", lambda e: e.tensor_tensor(out=qf1[0:rows, :], in0=qsb[0:rows, :],
                                                               in1=cos_ap, op=ALU.mult),
                             reads=["qsb", ckeys[0]], writes=["qf1"])
                        P.op("dve", lambda e, jj=jj: e.tensor_tensor(out=qf2[0:rows, :], in0=ps_m[jj][0:rows, :],
                                                                      in1=sin_ap, op=ALU.mult),
                             reads=["ps_m%d" % jj, ckeys[1]], writes=["qf2"])
                        oi = pm_i[0] % 2
                        P.op("pool", lambda e, oi=oi: e.tensor_tensor(out=qo[oi][0:rows, :], in0=qf1[0:rows, :],
                                                                       in1=qf2[0:rows, :], op=ALU.add),
                             reads=["qf1", "qf2"], writes=["qo%d" % oi])
                        P.dma("pool", dst_ap, qo[oi][0:rows, :], reads=["qo%d" % oi], semkey="st_qo%d" % oi)

                    for h in range(8):
                        j = pm_i[0] % 3
                        pm_i[0] += 1
                        for kc in range(3):
                            P.op("pe", lambda e, h=h, kc=kc, j=j: e.matmul(
                                ps_m[j][0:96, :], lhsT=wuq[:, kc, h * 96:(h + 1) * 96], rhs=cT[:, kc, :],
                                start=(kc == 0), stop=(kc == 2)),
                                reads=["wuq"] + cTk, writes=["ps_m%d" % j])
                        rope_out("ps_m%d" % j, ps_m[j][0:96, :], 96, rot96[:, :], "rot96", cos_t[:, :], sin_t[:, :],
                                 ("ropetab", "ropetab"), qT_mla[h, :, g * 512:(g + 1) * 512])
                        j = pm_i[0] % 3
                        pm_i[0] += 1
                        P.op("pe", lambda e, h=h, j=j: e.matmul(
                            ps_m[j][0:64, :], lhsT=wkk[:, h * 64:(h + 1) * 64], rhs=cT[:, 3, :],
                            start=True, stop=True), reads=["wkk"] + cTk, writes=["ps_m%d" % j])
                        i2 = h % 2
                        P.op("act", lambda e, j=j, i2=i2: e.copy(out=pfm[i2][0:64, :], in_=ps_m[j][0:64, :]),
                             reads=["ps_m%d" % j], writes=["pfm%d" % i2])
                        P.dma("pool", kT_mla[h, :, g * 512:(g + 1) * 512], pfm[i2][0:64, :],
                              reads=["pfm%d" % i2], semkey="st_pfm%d" % i2)
                    rope_out("cTall", cT[0:32, 4, :], 32, rot32[:, :], "rot32", cos32[:, :], sin32[:, :],
                             ("ropetab", "ropetab"), kpeT[:, g * 512:(g + 1) * 512])
                    for tb in range(4):
                        t0 = g * 512 + tb * 128
                        j = pm_i[0] % 3
                        pm_i[0] += 1
                        P.op("pe", lambda e, tb=tb, j=j: e.matmul(
                            ps_m[j][:], lhsT=cT[:, 3, tb * 128:(tb + 1) * 128], rhs=wkv[:, :], start=True, stop=True),
                            reads=["wkv", "cT%d" % tb], writes=["ps_m%d" % j])
                        i2 = tb % 2
                        P.op("act", lambda e, j=j, i2=i2: e.copy(out=pfm[i2][:], in_=ps_m[j][:]),
                             reads=["ps_m%d" % j], writes=["pfm%d" % i2])
                        P.dma("pool", v_mla[t0:t0 + 128, :], pfm[i2][:], reads=["pfm%d" % i2], semkey="st_pfm%d" % i2)
                P.barrier()
                P.emit()
        if "B" in stages:
            with contextlib.ExitStack() as st:
                fl = P.sbuf("fl", [8, T], F32, st)
                fo = P.sbuf("fo", [8, T], F32, st)
                fr = P.sbuf("fr", [8, T], F32, st)
                fb = [P.sbuf("fb%d" % i, [8, T], BF16, st) for i in range(3)]
                fnb = [P.sbuf("fnb%d" % i, [8, T], BF16, st) for i in range(2)]
                f8 = P.sbuf("f8", [8, T], BF16, st)
                nb = P.sbuf("nb", [8, 1], F32, st)
                P.dma("sp", fl[:], fT[:, :], writes=["fl"])
                P.dma("sp", nb[:], fox_b[l].rearrange("(h o) -> h o", o=1), writes=["nb"])
                P.op("dve", lambda e: e.tensor_scalar(out=nb[:], in0=nb[:], scalar1=-1.0, scalar2=None, op0=ALU.mult),
                     reads=["nb"], writes=["nb"])
                P.op("dve", lambda e: e.memset(fo[:], 1.0), writes=["fo"])
                P.op("pool", lambda e: e.memset(f8[:], 8.0), writes=["f8"])
                P.op("act", lambda e: e.activation(out=fl[:], in_=fl[:], func=AF.Exp, bias=nb[:], scale=-1.0),
                     reads=["fl", "nb"], writes=["fl"])
                P.op("act", lambda e: e.activation(out=fl[:], in_=fl[:], func=AF.Ln, bias=1.0, scale=1.0),
                     reads=["fl"], writes=["fl"])
                P.op("dve", lambda e: e.tensor_tensor_scan(out=fr[:], data0=fo[:], data1=fl[:], initial=0.0,
                                                           op0=ALU.mult, op1=ALU.add),
                     reads=["fl", "fo"], writes=["fr"])
                for i in range(3):
                    P.op("dve", lambda e, i=i: e.tensor_copy(out=fb[i][:], in_=fr[:]), reads=["fr"], writes=["fb%d" % i])
                    P.op("dve", lambda e, i=i: e.tensor_scalar(out=fnb[i % 2][:], in0=fb[i][:], scalar1=-1.0, scalar2=None,
                                                               op0=ALU.mult), reads=["fb%d" % i], writes=["fnb%d" % (i % 2)])
                    if i < 2:
                        P.op("dve", lambda e, i=i: e.tensor_tensor(out=fr[:], in0=fr[:], in1=fb[i][:], op=ALU.subtract),
                             reads=["fr", "fb%d" % i], writes=["fr"])
                    P.dma("sp", foxk_aug[:, i, :], fb[i][:], reads=["fb%d" % i], semkey="st_fb%d" % i)
                    P.dma("sp", foxq_aug[:, 3 + i, :], fnb[i % 2][:], reads=["fnb%d" % (i % 2)], semkey="st_fnb%d" % (i % 2))
                    P.dma("pool", foxk_aug[:, 3 + i, :], f8[:], reads=["f8"], semkey="st_f8")
                    P.dma("pool", foxq_aug[:, i, :], f8[:], reads=["f8"], semkey="st_f8")
                P.barrier()
                P.emit()
            with contextlib.ExitStack() as st:
                tri = P.sbuf("tri", [128, 128], BF16, st)
                P.dma("sp", tri[:], consts["c_tri"][:, :], writes=["tri"])
                qt = [P.sbuf("qt%d" % i, [128, T], BF16, st) for i in range(2)]
                kt = [P.sbuf("kt%d" % i, [128, T], BF16, st) for i in range(2)]
                vt = [P.sbuf("vt%d" % i, [128, NT, 65], BF16, st) for i in range(2)]
                pt = [P.sbuf("pt%d" % i, [128, 512], BF16, st) for i in range(3)]
                rd = P.sbuf("rd", [128, 4], F32, st)
                vstb = P.sbuf("vstb", [128, NT, 64], BF16, st)
                osb = [P.sbuf("osb%d" % i, [128, 64], BF16, st) for i in range(2)]
                ps_s = [P.psum("ps_s%d" % i, [128, 512], F32, st) for i in range(2)]
                ps_o = [P.psum("ps_o%d" % i, [128, 512], F32, st) for i in range(4)]
                for i in range(2):
                    P.op("pool", lambda e, i=i: e.memset(vt[i][:, :, 64:65], 1.0), writes=["vt%d_1" % i])
                heads = []
                for h in range(8):
                    heads.append(dict(q=[(qT_mla[h], 0, 96)], k=[(kT_mla[h], 0, 64), (kpeT, 64, 32)],
                                      v=v_mla[:, h * 64:(h + 1) * 64], d=96, scale=96 ** -0.5, col=h * 64))
                for h in range(8):
                    heads.append(dict(q=[(pT[FM_FOXQ + h * 64:FM_FOXQ + (h + 1) * 64], 0, 64), (foxq_aug[h], 64, 6)],
                                      k=[(pT[FM_FOXK + h * 64:FM_FOXK + (h + 1) * 64], 0, 64), (foxk_aug[h], 64, 6)],
                                      v=pTMb[:, TM_FOXV + h * 64:TM_FOXV + (h + 1) * 64], d=70, scale=0.125, col=512 + h * 64))
                oi = [0]
                for hi, H in enumerate(heads):
                    bi = hi % 2
                    def rkeys(pref, p0, n):
                        ks = []
                        if p0 < 64:
                            ks.append("%s%d_0" % (pref, bi))
                        if p0 + n > 64:
                            ks.append("%s%d_64" % (pref, bi))
                        return ks
                    qk_keys = []
                    for (ap_, p0, n) in H["q"]:
                        P.dma(None, qt[bi][p0:p0 + n, :], ap_, writes=rkeys("qt", p0, n), semkey="ld_qt%d_%d" % (bi, p0))
                        qk_keys += rkeys("qt", p0, n)
                    for (ap_, p0, n) in H["k"]:
                        P.dma(None, kt[bi][p0:p0 + n, :], ap_, writes=rkeys("kt", p0, n), semkey="ld_kt%d_%d" % (bi, p0))
                        qk_keys += rkeys("kt", p0, n)
                    qk_keys = sorted(set(qk_keys))
                    if H["v"] is not None:
                        P.dma(None, vstb[:], H["v"].rearrange("(j p) d -> p j d", p=128), writes=["vstb"])
                        P.op("pool", lambda e, bi=bi: e.tensor_copy(out=vt[bi][:, :, 0:64], in_=vstb[:]),
                             reads=["vstb"], writes=["vt%d" % bi])
                    d = H["d"]
                    steps = [(c, j) for c in range(NG) for j in range(4 * c + 4)]

                    def qk(idx, bi=bi, d=d, qk_keys=qk_keys):
                        c, j = steps[idx]
                        s = idx % 2
                        P.op("pe", lambda e: e.matmul(ps_s[s][:], lhsT=kt[bi][0:d, j * 128:(j + 1) * 128],
                                                      rhs=qt[bi][0:d, c * 512:(c + 1) * 512], start=True, stop=True),
                             reads=qk_keys, writes=["ps_s%d" % s])
                    qk(0)
                    for idx, (c, j) in enumerate(steps):
                        s = idx % 2
                        pi = idx % 3
                        P.op("act", lambda e, s=s, pi=pi, H=H: e.activation(out=pt[pi][:], in_=ps_s[s][:], func=AF.Exp,
                                                                       scale=H["scale"]),
                             reads=["ps_s%d" % s], writes=["pt%d" % pi])
                        if j >= 4 * c:
                            b = j - 4 * c
                            P.op("dve", lambda e, pi=pi, b=b: e.tensor_tensor(
                                out=pt[pi][:, b * 128:(b + 1) * 128], in0=pt[pi][:, b * 128:(b + 1) * 128],
                                in1=tri[:], op=ALU.mult), reads=["pt%d" % pi, "tri"], writes=["pt%d" % pi])
                        if idx + 1 < len(steps):
                            qk(idx + 1)
                        for b in range(4):
                            qb = 4 * c + b
                            if j > qb:
                                continue
                            P.op("pe", lambda e, pi=pi, b=b, j=j, qb=qb, bi=bi: e.matmul(
                                ps_o[b][:, 0:65], lhsT=pt[pi][:, b * 128:(b + 1) * 128], rhs=vt[bi][:, j, :],
                                start=(j == 0), stop=(j == qb)),
                                reads=["pt%d" % pi, "vt%d" % bi, "vt%d_1" % bi], writes=["ps_o%d" % b])
                            if j == qb:
                                P.op("dve", lambda e, b=b: e.reciprocal(out=rd[:, b:b + 1], in_=ps_o[b][:, 64:65]),
                                     reads=["ps_o%d" % b], writes=["rd%d" % b])
                                o2 = oi[0] % 2
                                oi[0] += 1
                                P.op("dve", lambda e, b=b, o2=o2: e.tensor_scalar(
                                    out=osb[o2][:], in0=ps_o[b][:, 0:64], scalar1=rd[:, b:b + 1], scalar2=None,
                                    op0=ALU.mult), reads=["ps_o%d" % b, "rd%d" % b], writes=["osb%d" % o2])
                                P.dma("sp", ocat[qb * 128:(qb + 1) * 128, H["col"]:H["col"] + 64], osb[o2][:],
                                      reads=["osb%d" % o2], semkey="st_osb%d" % o2)
                P.barrier()
                P.emit()
        if "N" in stages or "S" in stages:
            LW, LC = 512, 4352
            if l == 0:
                Fw_h = dt("Fw", [16 * 128 * (LW + 1)], F32)
                Fc_h = dt("Fc", [8 * 128 * (LC + 16)], F32)
                tabw_d = dsc("tabw_d", [16, LW], F32)
                tabc_d = dsc("tabc_d", [8, LC], F32)
            with contextlib.ExitStack() as st:
              if l == 0:
                  rb = P.sbuf("rb", [32, 16], F32, st)
                  ohw = P.sbuf("ohw", [32, LW], F32, st)
                  ohc = P.sbuf("ohc", [32, LC], F32, st)
                  vw = P.sbuf("vw", [16, LW], F32, st)
                  vc_ = P.sbuf("vc_", [8, LC], F32, st)
                  tw = P.sbuf("tw", [16, LW], F32, st)
                  tc_ = P.sbuf("tc_", [8, LC], F32, st)
                  ps = P.psum("ps_tab", [16, 512], F32, st)
                  P.dma("sp", rb[:], rel_bias[:, :], writes=["rb"])
                  P.dma("sp", ohw[:], consts["c_ohw"][:, :], writes=["ohw"])
                  P.dma("sp", ohc[:], consts["c_ohc"][:, :], writes=["ohc"])
                  P.dma("sp", vw[:], consts["c_validw"][:, :], writes=["vw"])
                  P.dma("sp", vc_[:], consts["c_validc"][:, :], writes=["vc_"])
                  P.op("pe", lambda e: e.matmul(ps[:, 0:LW], lhsT=rb[:, :], rhs=ohw[:, :], start=True, stop=True),
                       reads=["rb", "ohw"], writes=["ps_tab"])
                  P.op("act", lambda e: e.activation(out=tw[:], in_=ps[:, 0:LW], func=AF.Exp), reads=["ps_tab"], writes=["tw"])
                  P.op("dve", lambda e: e.tensor_tensor(out=tw[:], in0=tw[:], in1=vw[:], op=ALU.mult),
                       reads=["tw", "vw"], writes=["tw"])
                  P.dma("sp", tabw_d[:, :], tw[:], reads=["tw"], semkey="st_tw")
                  for c0 in range(0, LC, 512):
                      n = min(512, LC - c0)
                      P.op("pe", lambda e, c0=c0, n=n: e.matmul(ps[0:8, 0:n], lhsT=rb[:, 0:8], rhs=ohc[:, c0:c0 + n],
                                                               start=True, stop=True),
                           reads=["rb", "ohc"], writes=["ps_tab"])
                      P.op("act", lambda e, c0=c0, n=n: e.activation(out=tc_[:, c0:c0 + n], in_=ps[0:8, 0:n], func=AF.Exp),
                           reads=["ps_tab"], writes=["tc_"])
                  P.op("dve", lambda e: e.tensor_tensor(out=tc_[:], in0=tc_[:], in1=vc_[:], op=ALU.mult),
                       reads=["tc_", "vc_"], writes=["tc_"])
                  P.dma("sp", tabc_d[:, :], tc_[:], reads=["tc_"], semkey="st_tc")
                  P.barrier()
                  for h in range(16):
                      P.dma(None, bass.AP(Fw_h, h * 128 * (LW + 1), [[LW + 1, 128], [1, LW]]),
                            tabw_d[h].partition_broadcast(128), semkey="st_fw")
                  for h in range(8):
                      P.dma(None, bass.AP(Fc_h, h * 128 * (LC + 16), [[LC + 16, 128], [1, LC]]),
                            tabc_d[h].partition_broadcast(128), semkey="st_fc")
                  P.barrier()
                  P.emit()

            def ewin_ap(h, delta):
                return bass.AP(Fw_h, h * 128 * (LW + 1) + 127 + 128 * delta, [[LW, 128], [1, 128]])

            def ecmp_ap(h, delta):
                return bass.AP(Fc_h, h * 128 * (LC + 16) + 2032 + 128 * delta, [[LC, 128], [1, 128]])

            def run_chunk(R, steps, rhs_q, qkeys, scale, fin):
                firsts, lasts = {}, {}
                for i, s_ in enumerate(steps):
                    for b in s_["blocks"]:
                        firsts.setdefault(b, i)
                        lasts[b] = i

                def qk(i):
                    s_ = steps[i]
                    si = R["si"] % 2
                    R["si"] += 1
                    s_["si"] = si
                    ex = s_.get("extra")
                    P.op("pe", lambda e: e.matmul(R["ps_s"][si][:], lhsT=s_["kl"], rhs=rhs_q, start=True,
                                                  stop=(ex is None)),
                         reads=list(s_["kkeys"]) + list(qkeys), writes=["ps_s%d" % si])
                    if ex is not None:
                        P.op("pe", lambda e: e.matmul(R["ps_s"][si][:], lhsT=ex[0], rhs=ex[1], start=False, stop=True),
                             reads=list(ex[2]), writes=["ps_s%d" % si])
                if not steps:
                    return
                qk(0)
                for i, s_ in enumerate(steps):
                    si = s_["si"]
                    pi = R["pi"] % 3
                    R["pi"] += 1
                    pt = R["pt"][pi]
                    bias = s_.get("bias")
                    if bias is not None:
                        P.op("act", lambda e, si=si, pt=pt, bias=bias: e.activation(
                            out=pt[:], in_=R["ps_s"][si][:], func=AF.Exp, bias=bias[0], scale=scale),
                            reads=["ps_s%d" % si, bias[1]], writes=["pt%d" % pi])
                    else:
                        P.op("act", lambda e, si=si, pt=pt: e.activation(
                            out=pt[:], in_=R["ps_s"][si][:], func=AF.Exp, scale=scale),
                            reads=["ps_s%d" % si], writes=["pt%d" % pi])
                    for b, act in s_["blocks"].items():
                        sub = pt[:, b * 128:(b + 1) * 128]
                        if act[0] == "mul":
                            P.op("dve", lambda e, sub=sub, act=act: e.tensor_tensor(out=sub, in0=sub, in1=act[1], op=ALU.mult),
                                 reads=["pt%d" % pi, act[2]], writes=["pt%d" % pi])
                        elif act[0] == "mulsc":
                            P.op("dve", lambda e, sub=sub, act=act: e.tensor_scalar(out=sub, in0=sub, scalar1=act[1],
                                                                                   scalar2=None, op0=ALU.mult),
                                 reads=["pt%d" % pi, act[2]], writes=["pt%d" % pi])
                        elif act[0] == "mul2":
                            P.op("dve", lambda e, sub=sub, act=act: e.scalar_tensor_tensor(
                                out=sub, in0=sub, scalar=act[1], in1=act[3], op0=ALU.mult, op1=ALU.mult),
                                reads=["pt%d" % pi, act[2], act[4]], writes=["pt%d" % pi])
                    if i + 1 < len(steps):
                        qk(i + 1)
                    for b in s_["blocks"]:
                        P.op("pe", lambda e, b=b, pt=pt, s_=s_, i=i: e.matmul(
                            R["ps_o"][b][:, 0:s_["vn"]], lhsT=pt[:, b * 128:(b + 1) * 128], rhs=s_["v"],
                            start=(firsts[b] == i), stop=(lasts[b] == i)),
                            reads=["pt%d" % pi] + list(s_["vkeys"]), writes=["ps_o%d" % b])
                        if lasts[b] == i:
                            fin(b, R["ps_o"][b])

            def load_v(R, dst, dst_key, src_ap):
                P.dma(None, R["vstb"][:], src_ap.rearrange("(j p) d -> p j d", p=128), writes=["vstb"])
                P.op("pool", lambda e: e.tensor_copy(out=dst[:, :, 0:64], in_=R["vstb"][:]),
                     reads=["vstb"], writes=[dst_key])

            def common_res(st, tag):
                R = dict(si=0, pi=0, oi=0)
                R["pt"] = [P.sbuf("pt%s%d" % (tag, i), [128, 512], BF16, st) for i in range(3)]
                R["ps_s"] = [P.psum("pss%s%d" % (tag, i), [128, 512], F32, st) for i in range(2)]
                R["ps_o"] = [P.psum("pso%s%d" % (tag, i), [128, 512], F32, st) for i in range(4)]
                R["vstb"] = P.sbuf("vstb%s" % tag, [128, NT, 64], BF16, st)
                R["rd"] = P.sbuf("rd%s" % tag, [128, 8], F32, st)
                R["osb"] = [P.sbuf("osb%s%d" % (tag, i), [128, 64], BF16, st) for i in range(2)]
                return R

            if "S" in stages:
                with contextlib.ExitStack() as st:
                    R = common_res(st, "s")
                    esw = P.sbuf("esw", [128, 16, 128], F32, st)
                    for h in range(8):
                        for dl in range(2):
                            P.dma(None, esw[:, h * 2 + dl, :], ewin_ap(8 + h, dl), writes=["esw"])
                    sk = P.sbuf("sk", [128, 8], F32, st)
                    P.dma("sp", sk[:], swa_sinks[l].partition_broadcast(128), writes=["sk"])
                    P.op("act", lambda e: e.activation(out=sk[:], in_=sk[:], func=AF.Exp), reads=["sk"], writes=["sk"])
                    qt = [P.sbuf("qts%d" % i, [64, T], BF16, st) for i in range(2)]
                    kt = P.sbuf("kts", [64, T], BF16, st)
                    vt = P.sbuf("vts", [128, NT, 65], BF16, st)
                    P.op("pool", lambda e: e.memset(vt[:, :, 64:65], 1.0), writes=["vt_1"])
                    for h in range(8):
                        g = h // 4
                        bi = h % 2
                        if h % 4 == 0:
                            P.dma(None, kt[:], pT[FM_SWAK + g * 64:FM_SWAK + (g + 1) * 64], writes=["kt"])
                            load_v(R, vt, "vt", pTMb[:, TM_SWAV + g * 64:TM_SWAV + (g + 1) * 64])
                        P.dma(None, qt[bi][:], pT[FM_SWAQ + h * 64:FM_SWAQ + (h + 1) * 64], writes=["qt%d" % bi])

                        def fin(b, pso, h=h, cbox=None):
                            qb = fin.c * 4 + b
                            P.op("dve", lambda e: e.tensor_scalar(out=R["rd"][:, b:b + 1], in0=pso[:, 64:65],
                                                                  scalar1=sk[:, h:h + 1], scalar2=None, op0=ALU.add),
                                 reads=["ps_o%d" % b, "sk"], writes=["rd%d" % b])
                            P.op("dve", lambda e: e.reciprocal(out=R["rd"][:, b:b + 1], in_=R["rd"][:, b:b + 1]),
                                 reads=["rd%d" % b], writes=["rd%d" % b])
                            o2 = R["oi"] % 2
                            R["oi"] += 1
                            P.op("dve", lambda e: e.tensor_scalar(out=R["osb"][o2][:], in0=pso[:, 0:64],
                                                                  scalar1=R["rd"][:, b:b + 1], scalar2=None, op0=ALU.mult),
                                 reads=["ps_o%d" % b, "rd%d" % b], writes=["osb%d" % o2])
                            P.dma("sp", ocat[qb * 128:(qb + 1) * 128, 1536 + h * 64:1536 + (h + 1) * 64], R["osb"][o2][:],
                                  reads=["osb%d" % o2], semkey="st_osb%d" % o2)
                        for c in range(NG):
                            fin.c = c
                            steps = []
                            for j in range(max(0, 4 * c - 1), 4 * c + 4):
                                blocks = {}
                                for b in range(4):
                                    dl = 4 * c + b - j
                                    if 0 <= dl <= 1:
                                        blocks[b] = ("mul", esw[:, h * 2 + dl, :], "esw")
                                steps.append(dict(kl=kt[:, j * 128:(j + 1) * 128], kkeys=["kt"], v=vt[:, j, :],
                                                  vkeys=["vt", "vt_1"], vn=65, blocks=blocks))
                            run_chunk(R, steps, qt[bi][:, c * 512:(c + 1) * 512], ["qt%d" % bi], 0.125, fin)
                    P.barrier()
                    P.emit()

            if "N" in stages:
                ncmp = (T - 32) // 16 + 1
                NCB = (ncmp + 127) // 128
                with contextlib.ExitStack() as st:
                    R = common_res(st, "n")
                    ps_x = P.psum("ps_x", [128, 512], F32, st)
                    identf = P.sbuf("identf", [128, 128], F32, st)
                    ewn = P.sbuf("ewn", [128, 24, 128], F32, st)
                    ecm = P.sbuf("ecm", [128, 2, 8, 128], F32, st)
                    b31 = P.sbuf("b31", [128, 16], F32, st)
                    e31 = P.sbuf("e31", [128, 16], F32, st)
                    em31 = P.sbuf("em31", [128, 16], F32, st)
                    gates = P.sbuf("gates", [128, NT, 24], F32, st)
                    esel = P.sbuf("esel", [128, T], BF16, st)
                    ovl = P.sbuf("ovl", [128, 4, 128], BF16, st)
                    P.dma("sp", identf[:], consts["c_identf"][:, :], semkey="ld_const")
                    for h in range(8):
                        for dl in range(3):
                            P.dma(None, ewn[:, h * 3 + dl, :], ewin_ap(h, dl), semkey="ld_const")
                    P.dma("sp", b31[:], rel_bias[31].partition_broadcast(128), semkey="ld_const")
                    P.dma("sp", gates[:], pTM[:, TM_GATE:TM_GATE + 24].rearrange("(j p) c -> p j c", p=128), semkey="ld_const")
                    P.dma("sp", esel[:], consts["c_esel"][:, :], semkey="ld_const")
                    P.dma("sp", ovl[:], consts["c_overlap"].rearrange("(j p) s -> p j s", p=128), semkey="ld_const")
                    P.barrier()
                    P.op("act", lambda e: e.activation(out=e31[:], in_=b31[:], func=AF.Exp), reads=["b31"], writes=["e31"])
                    P.op("act", lambda e: e.activation(out=em31[:], in_=b31[:], func=AF.Exp, scale=-1.0),
                         reads=["b31"], writes=["em31"])
                    P.op("act", lambda e: e.activation(out=gates[:], in_=gates[:], func=AF.Exp, scale=-1.0),
                         reads=["gates"], writes=["gates"])
                    P.op("dve", lambda e: e.tensor_scalar(out=gates[:], in0=gates[:], scalar1=1.0, scalar2=None, op0=ALU.add),
                         reads=["gates"], writes=["gates"])
                    P.op("dve", lambda e: e.reciprocal(out=gates[:], in_=gates[:]), reads=["gates"], writes=["gates"])
                    negmt = P.sbuf("negmt", [128, T], BF16, st)
                    impacc = P.sbuf("impacc", [128, 4, 128], F32, st)
                    sc_t = P.sbuf("sc_t", [128, 128], F32, st)
                    m8 = P.sbuf("m8", [128, 8], F32, st)
                    tadd = [P.sbuf("tadd%d" % i, [128, 128], F32, st) for i in range(2)]
                    qt = [P.sbuf("qtn%d" % i, [64, T], BF16, st) for i in range(2)]
                    qch = [P.sbuf("qch%d" % i, [64, 512], BF16, st) for i in range(4)]
                    kt = P.sbuf("ktn", [64, T], BF16, st)
                    vt = P.sbuf("vtn", [128, NT, 65], BF16, st)
                    P.op("pool", lambda e: e.memset(vt[:, :, 64:65], 1.0), writes=["vt_1"])
                    kvT = P.sbuf("kvT", [64, T], BF16, st)
                    w1f = P.sbuf("w1f", [64, 32, 64], F32, st)
                    w1b = P.sbuf("w1b", [64, 32, 64], BF16, st)
                    w2f = P.sbuf("w2f", [64, 64], F32, st)
                    w2b = P.sbuf("w2b", [64, 64], BF16, st)
                    posf = P.sbuf("posf", [64, 32], F32, st)
                    posb = P.sbuf("posb", [64, 32], BF16, st)
                    cbias = P.sbuf("cbias", [64, 1], F32, st)
                    gx = P.sbuf("gx", [64, 512], F32, st)
                    gy = P.sbuf("gy", [64, 512], F32, st)
                    gT = P.sbuf("gT", [64, 512], BF16, st)
                    kcT = P.sbuf("kcT", [64, 512], BF16, st)
                    vca = P.sbuf("vca", [128, 4, 193], BF16, st)
                    P.op("pool", lambda e: e.memset(vca[:, :, 64:65], 1.0), writes=["vca_1"])
                    P.op("pool", lambda e: e.tensor_copy(out=vca[:, :, 65:193], in_=ovl[:]), reads=["ovl"], writes=["vca_o"])
                    P.op("dve", lambda e: e.memset(gT[:], 0.0), writes=["gT"])

                    def compress(which, g, fm_off):
                        P.dma(None, kvT[:], pT[fm_off + g * 64:fm_off + (g + 1) * 64], writes=["kvT"])
                        P.dma("sp", w1f[:], nsa_w1[l, which].rearrange("(l i) o -> i l o", i=64), writes=["cw"])
                        P.dma("sp", w2f[:], nsa_w2[l, which], writes=["cw"])
                        P.dma("sp", posf[:], nsa_pos[l, which].rearrange("l d -> d l"), writes=["cw"],
                              allow_slow_non_contiguous=True)
                        P.op("dve", lambda e: e.tensor_copy(out=w1b[:], in_=w1f[:]), reads=["cw"], writes=["w1b"])
                        P.op("dve", lambda e: e.tensor_copy(out=w2b[:], in_=w2f[:]), reads=["cw"], writes=["w2b"])
                        P.op("dve", lambda e: e.tensor_copy(out=posb[:], in_=posf[:]), reads=["cw"], writes=["posb"])
                        for li in range(32):
                            P.op("pe", lambda e, li=li: e.matmul(ps_x[0:64, 0:1], lhsT=w1b[:, li, :], rhs=posb[:, li:li + 1],
                                                                 start=(li == 0), stop=(li == 31)),
                                 reads=["w1b", "posb"], writes=["ps_x"])
                        P.op("dve", lambda e: e.tensor_copy(out=cbias[:], in_=ps_x[0:64, 0:1]), reads=["ps_x"], writes=["cbias"])
                        for li in range(32):
                            P.op("pe", lambda e, li=li: e.matmul(
                                ps_x[0:64, 0:ncmp], lhsT=w1b[:, li, :],
                                rhs=kvT[:].rearrange("p (c s) -> p c s", s=16)[:, li // 16:li // 16 + ncmp, li % 16],
                                start=(li == 0), stop=(li == 31)),
                                reads=["w1b", "kvT"], writes=["ps_x"])
                        n = ncmp
                        P.op("act", lambda e: e.activation(out=gx[:, 0:n], in_=ps_x[0:64, 0:n], func=AF.Identity,
                                                           bias=cbias[:], scale=1.0),
                             reads=["ps_x", "cbias"], writes=["gx"])
                        P.op("dve", lambda e: e.tensor_tensor(out=gy[:, 0:n], in0=gx[:, 0:n], in1=gx[:, 0:n], op=ALU.mult),
                             reads=["gx"], writes=["gy"])
                        P.op("dve", lambda e: e.tensor_scalar(out=gy[:, 0:n], in0=gy[:, 0:n], scalar1=0.044715, scalar2=1.0,
                                                              op0=ALU.mult, op1=ALU.add), reads=["gy"], writes=["gy"])
                        P.op("dve", lambda e: e.tensor_tensor(out=gy[:, 0:n], in0=gy[:, 0:n], in1=gx[:, 0:n], op=ALU.mult),
                             reads=["gy", "gx"], writes=["gy"])
                        P.op("act", lambda e: e.activation(out=gy[:, 0:n], in_=gy[:, 0:n], func=AF.Tanh, scale=0.7978845608),
                             reads=["gy"], writes=["gy"])
                        P.op("dve", lambda e: e.tensor_scalar(out=gy[:, 0:n], in0=gy[:, 0:n], scalar1=1.0, scalar2=0.5,
                                                              op0=ALU.add, op1=ALU.mult), reads=["gy"], writes=["gy"])
                        P.op("dve", lambda e: e.tensor_tensor(out=gT[:, 0:n], in0=gy[:, 0:n], in1=gx[:, 0:n], op=ALU.mult),
                             reads=["gy", "gx"], writes=["gT"])
                        if which == 0:
                            P.op("pe", lambda e: e.matmul(ps_x[0:64, 0:512], lhsT=w2b[:, :], rhs=gT[:, :], start=True, stop=True),
                                 reads=["w2b", "gT"], writes=["ps_x"])
                            P.op("act", lambda e: e.copy(out=kcT[:], in_=ps_x[0:64, 0:512]), reads=["ps_x"], writes=["kcT"])
                        else:
                            for cb in range(4):
                                P.op("pe", lambda e, cb=cb: e.matmul(ps_x[:, 0:64], lhsT=gT[:, cb * 128:(cb + 1) * 128],
                                                                     rhs=w2b[:, :], start=True, stop=True),
                                     reads=["w2b", "gT"], writes=["ps_x"])
                                P.op("act", lambda e, cb=cb: e.copy(out=vca[:, cb, 0:64], in_=ps_x[:, 0:64]),
                                     reads=["ps_x"], writes=["vca_v"])

                    for g in range(2):
                        compress(0, g, FM_KC)
                        compress(1, g, FM_VC)
                        vkeys_c = ["vca_v", "vca_1", "vca_o"]
                        for c in range(NG):
                            for r in range(4):
                                h = g * 4 + r
                                if c == 0:
                                    pass
                                qi = R["qi"] = R.get("qi", 0) + 1
                                qc = qch[qi % 4]
                                qk_ = "qch%d" % (qi % 4)
                                P.dma(None, qc[:], pT[FM_NSAQ + h * 64:FM_NSAQ + (h + 1) * 64, c * 512:(c + 1) * 512],
                                      writes=[qk_])
                                need = sorted({4 * c + b - 16 * cb for b in range(4) for cb in range(NCB)
                                               if 0 <= 4 * c + b - 16 * cb <= 17})
                                es = qi % 2
                                slot = {dl: k_ for k_, dl in enumerate(need)}
                                for dl in need:
                                    P.dma(None, ecm[:, es, slot[dl], :], ecmp_ap(h, dl), writes=["ecm%d" % es])

                                def fin(b, pso, h=h, c=c, r=r):
                                    qb = 4 * c + b
                                    P.op("dve", lambda e: e.tensor_scalar(out=R["rd"][:, b:b + 1], in0=pso[:, 64:65],
                                                                          scalar1=1e-30, scalar2=None, op0=ALU.max),
                                         reads=["ps_o%d" % b], writes=["rd%d" % b])
                                    P.op("dve", lambda e: e.reciprocal(out=R["rd"][:, b:b + 1], in_=R["rd"][:, b:b + 1]),
                                         reads=["rd%d" % b], writes=["rd%d" % b])
                                    o2 = R["oi"] % 2
                                    R["oi"] += 1
                                    P.op("dve", lambda e: e.tensor_scalar(
                                        out=R["osb"][o2][:], in0=pso[:, 0:64], scalar1=R["rd"][:, b:b + 1],
                                        scalar2=gates[:, qb, h * 3:h * 3 + 1], op0=ALU.mult, op1=ALU.mult),
                                        reads=["ps_o%d" % b, "rd%d" % b, "gates"], writes=["osb%d" % o2])
                                    P.dma("sp", ocat[qb * 128:(qb + 1) * 128, 1024 + h * 64:1024 + (h + 1) * 64],
                                          R["osb"][o2][:], reads=["osb%d" % o2], semkey="st_osb%d" % o2)
                                    if r == 0:
                                        P.op("dve", lambda e: e.tensor_scalar(
                                            out=impacc[:, b, :], in0=pso[:, 65:193], scalar1=R["rd"][:, b:b + 1],
                                            scalar2=None, op0=ALU.mult),
                                            reads=["ps_o%d" % b, "rd%d" % b], writes=["imp%d" % b])
                                    else:
                                        P.op("dve", lambda e: e.scalar_tensor_tensor(
                                            out=impacc[:, b, :], in0=pso[:, 65:193], scalar=R["rd"][:, b:b + 1],
                                            in1=impacc[:, b, :], op0=ALU.mult, op1=ALU.add),
                                            reads=["ps_o%d" % b, "rd%d" % b, "imp%d" % b], writes=["imp%d" % b])
                                steps = []
                                for cb in range(NCB):
                                    dls = [4 * c + b - 16 * cb for b in range(4)]
                                    if max(dls) < 0:
                                        continue
                                    blocks, bias = {}, None
                                    if min(dls) >= 18:
                                        bias = (b31[:, h:h + 1], "b31")
                                        blocks = {b: ("plain",) for b in range(4)}
                                    else:
                                        for b, dl in enumerate(dls):
                                            if dl < 0:
                                                continue
                                            if dl <= 17:
                                                blocks[b] = ("mul", ecm[:, es, slot[dl], :], "ecm%d" % es)
                                            else:
                                                blocks[b] = ("mulsc", e31[:, h:h + 1], "e31")
                                    steps.append(dict(kl=kcT[:, cb * 128:(cb + 1) * 128], kkeys=["kcT"], bias=bias,
                                                      v=vca[:, cb, :], vkeys=vkeys_c, vn=193, blocks=blocks))
                                run_chunk(R, steps, qc[:, :], [qk_], 0.125, fin)
                            for b in range(4):
                                qb = 4 * c + b
                                ti = qb % 2
                                P.dma("sp", tadd[ti][:], consts["c_topadd"][qb], writes=["tadd%d" % ti])
                                P.op("dve", lambda e, b=b, ti=ti: e.tensor_tensor(out=sc_t[:], in0=impacc[:, b, :],
                                                                                   in1=tadd[ti][:], op=ALU.add),
                                     reads=["imp%d" % b, "tadd%d" % ti], writes=["sc_t"])
                                P.op("dve", lambda e: e.max(out=m8[:], in_=sc_t[:]), reads=["sc_t"], writes=["m8"])
                                P.op("dve", lambda e: e.tensor_scalar(out=sc_t[:], in0=sc_t[:], scalar1=m8[:, 7:8], scalar2=1.0,
                                                                      op0=ALU.is_ge, op1=ALU.subtract),
                                     reads=["sc_t", "m8"], writes=["sc_t"])
                                P.op("pe", lambda e: e.transpose(out=ps_x[:, 0:128], in_=sc_t[:], identity=identf[:]),
                                     reads=["sc_t", "identf"], writes=["ps_x"])
                                P.op("act", lambda e, qb=qb: e.activation(out=negmt[:, qb * 128:(qb + 1) * 128],
                                                                           in_=ps_x[:, 0:128], func=AF.Identity, scale=30000.0),
                                     reads=["ps_x"], writes=["negmt%d" % c])
                            if c % 4 == 3:
                                P.barrier()
                        for br, (fm_k, tm_v, dst, gcol_) in enumerate(((FM_KSL, TM_VSL, osel, 1), (FM_KWN, TM_VWN, owin, 2))):
                            P.dma(None, kt[:], pT[fm_k + g * 64:fm_k + (g + 1) * 64], writes=["kt"])
                            load_v(R, vt, "vt", pTMb[:, tm_v + g * 64:tm_v + (g + 1) * 64])
                            for r in range(4):
                                h = g * 4 + r
                                bi = r % 2
                                P.dma(None, qt[bi][:], pT[FM_NSAQ + h * 64:FM_NSAQ + (h + 1) * 64], writes=["qt%d" % bi])

                                def fin(b, pso, h=h, dst=dst, gcol_=gcol_):
                                    qb = fin.c * 4 + b
                                    P.op("dve", lambda e: e.reciprocal(out=R["rd"][:, b:b + 1], in_=pso[:, 64:65]),
                                         reads=["ps_o%d" % b], writes=["rd%d" % b])
                                    o2 = R["oi"] % 2
                                    R["oi"] += 1
                                    P.op("dve", lambda e: e.tensor_scalar(
                                        out=R["osb"][o2][:], in0=pso[:, 0:64], scalar1=R["rd"][:, b:b + 1],
                                        scalar2=gates[:, qb, h * 3 + gcol_:h * 3 + gcol_ + 1], op0=ALU.mult, op1=ALU.mult),
                                        reads=["ps_o%d" % b, "rd%d" % b, "gates"], writes=["osb%d" % o2])
                                    P.dma("sp", dst[qb * 128:(qb + 1) * 128, h * 64:(h + 1) * 64], R["osb"][o2][:],
                                          reads=["osb%d" % o2], semkey="st_osb%d" % o2)
                                for c in range(NG):
                                    fin.c = c
                                    steps = []
                                    if br == 0:
                                        for j in range(4 * c + 4):
                                            blocks = {}
                                            for b in range(4):
                                                dl = 4 * c + b - j
                                                if dl < 0:
                                                    continue
                                                if dl <= 1:
                                                    blocks[b] = ("mul2", em31[:, h:h + 1], "em31", ewn[:, h * 3 + dl, :], "ewn")
                                                else:
                                                    blocks[b] = ("plain",)
                                            steps.append(dict(
                                                kl=kt[:, j * 128:(j + 1) * 128], kkeys=["kt"],
                                                extra=(esel[:, j * 128:(j + 1) * 128], negmt[:, c * 512:(c + 1) * 512],
                                                       ["esel", "negmt%d" % c]),
                                                bias=(b31[:, h:h + 1], "b31"), v=vt[:, j, :], vkeys=["vt", "vt_1"], vn=65,
                                                blocks=blocks))
                                    else:
                                        for j in range(max(0, 4 * c - 2), 4 * c + 4):
                                            blocks = {}
                                            for b in range(4):
                                                dl = 4 * c + b - j
                                                if 0 <= dl <= 2:
                                                    blocks[b] = ("mul", ewn[:, h * 3 + dl, :], "ewn")
                                            steps.append(dict(kl=kt[:, j * 128:(j + 1) * 128], kkeys=["kt"], v=vt[:, j, :],
                                                              vkeys=["vt", "vt_1"], vn=65, blocks=blocks))
                                    run_chunk(R, steps, qt[bi][:, c * 512:(c + 1) * 512], ["qt%d" % bi], 0.125, fin)
                    P.barrier()
                    P.emit()
        if "C" in stages:
            ocat = ocat_keep[0]
            last = (l == n_layers - 1)
            with contextlib.ExitStack() as st:
                ident = P.sbuf("identc", [128, 128], BF16, st)
                P.dma("sp", ident[:], consts["c_ident"][:, :], writes=["ident"])
                gfin = P.sbuf("gfin", [128, D], F32, st)
                if True:
                    P.dma("sp", gfin[:], final_norm.partition_broadcast(128), writes=["gfin"])
                ot = [P.sbuf("ot%d" % i, [128, D], BF16, st) for i in range(2)]
                on = P.sbuf("on", [128, D], BF16, st)
                oadd = [P.sbuf("oadd%d" % i, [128, 512], BF16, st) for i in range(2)]
                junk = P.sbuf("junkc", [128, D], BF16, st)
                hh = P.sbuf("hh", [128, 4, D], F32, st)
                xT = P.sbuf("xT", [128, 16, 512], BF16, st)
                actT = P.sbuf("actT", [128, 64, 512], BF16, st)
                rl = [P.sbuf("rl%d" % i, [128, 512], F32, st) for i in range(2)]
                ss = P.sbuf("ssc", [128, 8], F32, st)
                wsl = [P.sbuf("wslc%d" % i, [128, 16, 512], BF16, st) for i in range(2)]
                ps_t = [P.psum("pc_t%d" % i, [128, 512], BF16, st) for i in range(2)]
                ps_m = [P.psum("pc_m%d" % i, [128, 512], F32, st) for i in range(2)]
                ps_a = [P.psum("pc_a%d" % i, [128, 512], F32, st) for i in range(4)]
                cnt = {"slab": 0, "pt": 0, "pm": 0, "rl": 0}

                def slab(src_ap):
                    i = cnt["slab"] % 2
                    cnt["slab"] += 1
                    P.dma(None, wsl[i][:], src_ap.rearrange("(c p) n -> p c n", p=128), writes=["wsl%d" % i])
                    return i

                def rstd_of(col, w):
                    P.op("dve", lambda e: e.tensor_scalar(out=ss[:, col:col + 1], in0=ss[:, col:col + 1],
                                                          scalar1=1.0 / w, scalar2=EPS, op0=ALU.mult, op1=ALU.add),
                         reads=["ss%d" % col], writes=["ss%d" % col])
                    P.op("act", lambda e: e.activation(out=ss[:, col:col + 1], in_=ss[:, col:col + 1], func=AF.Sqrt),
                         reads=["ss%d" % col], writes=["ss%d" % col])
                    P.op("dve", lambda e: e.reciprocal(out=ss[:, col:col + 1], in_=ss[:, col:col + 1]),
                         reads=["ss%d" % col], writes=["ss%d" % col])

                def transpose_into(src_tile, src_key, tb):
                    for c4 in range(4):
                        j = cnt["pt"] % 2
                        cnt["pt"] += 1
                        for cc in range(4):
                            c = c4 * 4 + cc
                            P.op("pe", lambda e, c=c, cc=cc, j=j: e.transpose(
                                out=ps_t[j][:, cc * 128:(cc + 1) * 128], in_=src_tile[:, c * 128:(c + 1) * 128],
                                identity=ident[:]), reads=[src_key, "ident"], writes=["ps_t%d" % j])
                        P.op("dve", lambda e, c4=c4, j=j: e.tensor_copy(
                            out=xT[:, c4 * 4:(c4 + 1) * 4, tb * 128:(tb + 1) * 128],
                            in_=ps_t[j][:].rearrange("p (c t) -> p c t", c=4)),
                            reads=["ps_t%d" % j], writes=["xT%d" % tb])

                xTk = ["xT%d" % tb for tb in range(4)]
                for g in range(NG):
                    for tb in range(4):
                        t0 = g * 512 + tb * 128
                        i = tb % 2
                        P.dma("sp", ot[i][:], ocat[t0:t0 + 128, :], writes=["ot%d" % i])
                        P.dma("sp", hh[:, tb, :], hsrc[t0:t0 + 128, :], writes=["hh%d" % tb])
                        for bi_, src_ in enumerate((osel, owin)):
                            P.dma("sp", oadd[bi_][:], src_[t0:t0 + 128, :], writes=["oadd%d" % bi_])
                            P.op("pool", lambda e, i=i, bi_=bi_: e.tensor_tensor(
                                out=ot[i][:, 1024:1536], in0=ot[i][:, 1024:1536], in1=oadd[bi_][:], op=ALU.add),
                                reads=["ot%d" % i, "oadd%d" % bi_], writes=["ot%d" % i])
                        for gi in range(4):
                            P.op("act", lambda e, i=i, gi=gi: e.activation(
                                out=junk[:, 0:512], in_=ot[i][:, gi * 512:(gi + 1) * 512], func=AF.Square,
                                accum_out=ss[:, gi:gi + 1]), reads=["ot%d" % i], writes=["junk", "ss%d" % gi])
                            rstd_of(gi, 512)
                            P.op("dve", lambda e, i=i, gi=gi: e.tensor_scalar(
                                out=on[:, gi * 512:(gi + 1) * 512], in0=ot[i][:, gi * 512:(gi + 1) * 512],
                                scalar1=ss[:, gi:gi + 1], scalar2=None, op0=ALU.mult),
                                reads=["ot%d" % i, "ss%d" % gi], writes=["on"])
                        transpose_into(on, "on", tb)
                    for s in range(4):
                        si = slab(wb_out[l][:, s * 512:(s + 1) * 512])
                        for tb in range(4):
                            j = cnt["pm"] % 2
                            cnt["pm"] += 1
                            for kc in range(16):
                                P.op("pe", lambda e, si=si, tb=tb, kc=kc, j=j: e.matmul(
                                    ps_m[j][:], lhsT=xT[:, kc, tb * 128:(tb + 1) * 128], rhs=wsl[si][:, kc, :],
                                    start=(kc == 0), stop=(kc == 15)),
                                    reads=["wsl%d" % si, "xT%d" % tb], writes=["ps_m%d" % j])
                            P.op("dve", lambda e, tb=tb, s=s, j=j: e.tensor_tensor(
                                out=hh[:, tb, s * 512:(s + 1) * 512], in0=hh[:, tb, s * 512:(s + 1) * 512],
                                in1=ps_m[j][:], op=ALU.add), reads=["ps_m%d" % j, "hh%d" % tb], writes=["hh%d" % tb])
                    for tb in range(4):
                        P.op("act", lambda e, tb=tb: e.activation(out=junk[:], in_=hh[:, tb, :], func=AF.Square,
                                                                  accum_out=ss[:, 4:5]),
                             reads=["hh%d" % tb], writes=["junk", "ss4"])
                        rstd_of(4, D)
                        P.op("dve", lambda e, tb=tb: e.tensor_scalar(out=on[:], in0=hh[:, tb, :], scalar1=ss[:, 4:5],
                                                                     scalar2=None, op0=ALU.mult),
                             reads=["hh%d" % tb, "ss4"], writes=["on"])
                        transpose_into(on, "on", tb)
                    for s in range(16):
                        si = slab(wb_up[l][:, s * 512:(s + 1) * 512])
                        for c in range(4):
                            j = cnt["pm"] % 2
                            cnt["pm"] += 1
                            for kc in range(16):
                                P.op("pe", lambda e, si=si, c=c, kc=kc, j=j: e.matmul(
                                    ps_m[j][:], lhsT=wsl[si][:, kc, c * 128:(c + 1) * 128], rhs=xT[:, kc, :],
                                    start=(kc == 0), stop=(kc == 15)),
                                    reads=["wsl%d" % si] + xTk, writes=["ps_m%d" % j])
                            ri = cnt["rl"] % 2
                            cnt["rl"] += 1
                            P.op("act", lambda e, j=j, ri=ri: e.activation(out=rl[ri][:], in_=ps_m[j][:], func=AF.Relu),
                                 reads=["ps_m%d" % j], writes=["rl%d" % ri])
                            P.op("dve" if c % 2 else "pool", lambda e, ri=ri, s=s, c=c: e.tensor_tensor(
                                out=actT[:, s * 4 + c, :], in0=rl[ri][:], in1=rl[ri][:], op=ALU.mult),
                                reads=["rl%d" % ri], writes=["actT%d" % (s * 4 + c)])
                    for s in range(4):
                        for kq in range(4):
                            si = slab(wb_dn[l][kq * 2048:(kq + 1) * 2048, s * 512:(s + 1) * 512])
                            for tb in range(4):
                                for kc in range(16):
                                    kk = kq * 16 + kc
                                    P.op("pe", lambda e, si=si, tb=tb, kc=kc, kk=kk: e.matmul(
                                        ps_a[tb][:], lhsT=actT[:, kk, tb * 128:(tb + 1) * 128], rhs=wsl[si][:, kc, :],
                                        start=(kk == 0), stop=(kk == 63)),
                                        reads=["wsl%d" % si, "actT%d" % kk], writes=["ps_a%d" % tb])
                        for tb in range(4):
                            P.op("dve", lambda e, tb=tb, s=s: e.tensor_tensor(
                                out=hh[:, tb, s * 512:(s + 1) * 512], in0=hh[:, tb, s * 512:(s + 1) * 512],
                                in1=ps_a[tb][:], op=ALU.add), reads=["ps_a%d" % tb, "hh%d" % tb], writes=["hh%d" % tb])
                    for tb in range(4):
                        t0 = g * 512 + tb * 128
                        if fused and not last:
                            P.dma("pool", hres[t0:t0 + 128, :], hh[:, tb, :], reads=["hh%d" % tb], semkey="st_hh%d" % tb)
                        if not fused:
                            P.dma("pool", hn_out[t0:t0 + 128, :], hh[:, tb, :], reads=["hh%d" % tb], semkey="st_hh%d" % tb)
                        if last or not fused:
                            P.op("act", lambda e, tb=tb: e.activation(out=junk[:], in_=hh[:, tb, :], func=AF.Square,
                                                                      accum_out=ss[:, 5:6]),
                                 reads=["hh%d" % tb], writes=["junk", "ss5"])
                            rstd_of(5, D)
                            P.op("dve", lambda e, tb=tb: e.tensor_scalar(out=hh[:, tb, :], in0=hh[:, tb, :],
                                                                         scalar1=ss[:, 5:6], scalar2=None, op0=ALU.mult),
                                 reads=["hh%d" % tb, "ss5"], writes=["hh%d" % tb])
                            P.op("pool", lambda e, tb=tb: e.tensor_tensor(out=hh[:, tb, :], in0=hh[:, tb, :],
                                                                          in1=gfin[:], op=ALU.mult),
                                 reads=["hh%d" % tb, "gfin"], writes=["hh%d" % tb])
                            P.dma("pool", y_out[t0:t0 + 128, :], hh[:, tb, :], reads=["hh%d" % tb], semkey="st_hy%d" % tb)
                    P.barrier()
                P.barrier()
                P.emit()
    P.barrier()
    P.emit()
    dbg["_max_sem_count"] = P.max_sem_count()
    dbg["_n_sems"] = len(P.sems)
    P.close()
    return nc, dbg


IN_SPLITS = (384, 128, 32, 512, 512, 512, 8, 512, 128, 128, 128, 128, 128, 128, 24, 512, 128, 128)
IN_OFF = np.concatenate([[0], np.cumsum(IN_SPLITS)]).astype(int)


def relayout_weights(w_in, w_ukv):
    L = w_in.shape[0]
    seg = lambda i: w_in[:, :, IN_OFF[i]:IN_OFF[i + 1]]
    w_fm = np.zeros((L, D, NFM), np.float32)
    w_tm = np.zeros((L, D, NTM), np.float32)
    for off, i in ((FM_FOXQ, 3), (FM_FOXK, 4), (FM_NSAQ, 7), (FM_SWAQ, 15), (FM_KC, 8), (FM_VC, 9),
                   (FM_KSL, 10), (FM_KWN, 12), (FM_SWAK, 16), (FM_F, 6)):
        s = seg(i)
        w_fm[:, :, off:off + s.shape[2]] = s
    for off, i in ((TM_CQ, 0), (TM_CKV, 1), (TM_KPE, 2), (TM_FOXV, 5), (TM_VSL, 11), (TM_VWN, 13),
                   (TM_GATE, 14), (TM_SWAV, 17)):
        s = seg(i)
        w_tm[:, :, off:off + s.shape[2]] = s
    kv = w_ukv.reshape(L, 128, 8, 128)
    w_k = np.ascontiguousarray(kv[:, :, :, :64].reshape(L, 128, 512))
    w_v = np.ascontiguousarray(kv[:, :, :, 64:].reshape(L, 128, 512))
    return w_fm, w_tm, w_k, w_v


FUSED = True
L1_IO = dict(pT="out", pTM="out", pTMb="out", ocat="out")
L2_IO = dict(pT="in", pTM="in", pTMb="in", ocat="in")


def build_programs(T):
    nc1, i1 = build(T, stages="AB", io=L1_IO)
    nc2, i2 = build(T, stages="SNC", io=L2_IO)
    return (nc1, i1["_inputs"]), (nc2, i2["_inputs"])


def run_layer(progs, h, W, l, T, last):
    (nc1, in1), (nc2, in2) = progs
    B = h.shape[0]
    base = layer_inputs(None, W, l, T)
    maps1 = [dict({k: base[k] for k in in1 if k != "x"}, x=np.ascontiguousarray(h[b])) for b in range(B)]
    r1 = run_bass_kernel_spmd(nc1, maps1, core_ids=list(range(B))).results
    maps2 = []
    for b in range(B):
        m = {k: base[k] for k in in2 if k in base}
        m["x"] = np.ascontiguousarray(h[b])
        m["pT"], m["pTM"], m["pTMb"] = r1[b]["pT"], r1[b]["pTM"], r1[b]["pTMb"]
        m["ocat_in"] = r1[b]["ocat"]
        maps2.append(m)
    r2 = run_bass_kernel_spmd(nc2, maps2, core_ids=list(range(B))).results
    key = "y" if last else "hn"
    return np.stack([np.asarray(r2[b][key]) for b in range(B)], axis=0)


def kernel(x, norm_attn, w_in, mla_q_norm, mla_w_uq, mla_kv_norm, mla_w_ukv, fox_b_f,
           nsa_cmp_pos, nsa_cmp_w1, nsa_cmp_w2, swa_sinks, group_norm, w_out,
           norm_mlp, w_up, w_down, rel_bias, final_norm):
    W = dict(norm_attn=norm_attn, w_in=w_in, mla_q_norm=mla_q_norm, mla_w_uq=mla_w_uq,
             mla_kv_norm=mla_kv_norm, mla_w_ukv=mla_w_ukv, fox_b_f=fox_b_f, nsa_cmp_pos=nsa_cmp_pos,
             nsa_cmp_w1=nsa_cmp_w1, nsa_cmp_w2=nsa_cmp_w2, swa_sinks=swa_sinks, group_norm=group_norm,
             w_out=w_out, norm_mlp=norm_mlp, w_up=w_up, w_down=w_down, rel_bias=rel_bias,
             final_norm=final_norm)
    W = {k: np.asarray(v) for k, v in W.items()}
    h = np.asarray(x, dtype=np.float32)
    B, T, _ = h.shape
    depth = W["w_in"].shape[0]
    if FUSED:
        nc, info = build(T, n_layers=depth, stages="ABSNC")
        base = layer_inputs(None, W, 0, T, nl=depth)
        maps = [dict({k: base[k] for k in info["_inputs"] if k != "x"}, x=np.ascontiguousarray(h[b]))
                for b in range(B)]
        res = run_bass_kernel_spmd(nc, maps, core_ids=list(range(B))).results
        return np.stack([np.asarray(res[b]["y"]) for b in range(B)], axis=0).astype(np.float32)
    progs = build_programs(T)
    for l in range(depth):
        h = run_layer(progs, h, W, l, T, last=(l == depth - 1))
    return h.astype(np.float32)


def layer_inputs(h, W, l, T, nl=1):
    w_fm, w_tm, w_k, w_v = relayout_weights(np.asarray(W["w_in"][l:l + nl]), np.asarray(W["mla_w_ukv"][l:l + nl]))
    m = dict(w_fm=w_fm, w_tm=w_tm, w_ukv_k=w_k, w_ukv_v=w_v)
    if h is not None:
        m["x"] = np.ascontiguousarray(h, dtype=np.float32)
    for k in ("norm_attn", "mla_q_norm", "mla_w_uq", "mla_kv_norm", "fox_b_f", "group_norm", "w_out", "norm_mlp",
              "w_up", "w_down", "nsa_cmp_pos", "nsa_cmp_w1", "nsa_cmp_w2", "swa_sinks"):
        m[k] = np.ascontiguousarray(np.asarray(W[k])[l:l + nl], dtype=np.float32)
    m["rel_bias"] = np.asarray(W["rel_bias"], np.float32)
    m["final_norm"] = np.asarray(W["final_norm"], np.float32)
    m.update(host_consts(T))
    return m
```

```python
import contextlib
import math
import numpy as np
import ml_dtypes
import concourse.bass as bass
import concourse.mybir as mybir
from concourse.bass_utils import run_bass_kernel_spmd

F32 = mybir.dt.float32
BF16 = mybir.dt.bfloat16
I32 = mybir.dt.int32
AF = mybir.ActivationFunctionType
ALU = mybir.AluOpType
AX = mybir.AxisListType
ENGS = ("pe", "act", "dve", "pool", "sp")

D = 2048
DEPTH = 2
HD = 64
NH = 8
DFF = 8192
EPS = 1e-6
NFM = 2816
NTM = 1536
FM_FOXQ, FM_FOXK, FM_NSAQ, FM_SWAQ = 0, 512, 1024, 1536
FM_KC, FM_VC, FM_KSL, FM_KWN, FM_SWAK, FM_F = 2048, 2176, 2304, 2432, 2560, 2688
TM_CQ, TM_CKV, TM_KPE, TM_FOXV, TM_VSL, TM_VWN, TM_GATE, TM_SWAV = 0, 384, 512, 544, 1056, 1184, 1312, 1336


class Prog:
    ENG_SWITCH = 8000
    DMA_SWITCH = 12000
    LIMIT = 30000

    def __init__(self, nc):
        self.nc = nc
        self.stack = contextlib.ExitStack()
        self.ops = {e: [] for e in ENGS}
        self.count = {e: 0 for e in ENGS}
        self.gen = {e: 0 for e in ENGS}
        self.dgen = {}
        self.seen = {e: {} for e in ENGS}
        self.last_w = {}
        self.readers = {}
        self.dma_cnt = {}
        self.sems = {}
        self.n_inst = 0
        self.rr = 0
        self.rr_name = 0
        self.keymap = {}
        self.pe_entries = {}
        self.pe_marks = []
        self.maxcount = 0

    def sem(self, key):
        if key not in self.sems:
            self.sems[key] = self.stack.enter_context(
                self.nc.semaphore("s%d" % len(self.sems)))
        return self.sems[key]

    def sbuf(self, name, shape, dtype, stack=None):
        st = stack or self.stack
        self.rr_name += 1
        return st.enter_context(self.nc.sbuf_tensor("%s_u%d" % (name, self.rr_name), list(shape), dtype))

    def psum(self, name, shape, dtype, stack=None):
        st = stack or self.stack
        self.rr_name += 1
        return st.enter_context(self.nc.psum_tensor("%s_u%d" % (name, self.rr_name), list(shape), dtype))

    def _pe_value(self, idx):
        if self.pe_marks and self.pe_marks[-1][0] >= idx:
            lo, hi = 0, len(self.pe_marks) - 1
            while lo < hi:
                mid = (lo + hi) // 2
                if self.pe_marks[mid][0] >= idx:
                    hi = mid
                else:
                    lo = mid + 1
            return self.pe_marks[lo][1]
        val = len(self.pe_marks) + 1
        self.pe_marks.append((idx, val))
        self.pe_entries[idx][3] = True
        self.maxcount = max(self.maxcount, val)
        return val

    def _deps(self, eng, reads, writes):
        deps = {}

        def add(d):
            if d is None:
                return
            k, v = d
            if deps.get(k, 0) < v:
                deps[k] = v
        for b in reads:
            add(self.last_w.get(b))
        for b in writes:
            add(self.last_w.get(b))
            for k, v in self.readers.get(b, {}).items():
                add((k, v))
        waits = []
        for k, v in deps.items():
            if k[0] == "eng" and k[1] == "pe":
                if eng == "pe":
                    continue
                v = self._pe_value(v)
            if self.seen[eng].get(k, 0) < v:
                self.seen[eng][k] = v
                waits.append((k, v))
        return waits

    def _commit(self, me, reads, writes):
        for b in writes:
            self.last_w[b] = me
            self.readers[b] = {}
        for b in reads:
            r = self.readers.setdefault(b, {})
            if r.get(me[0], 0) < me[1]:
                r[me[0]] = me[1]

    def op(self, eng, fn, reads=(), writes=()):
        waits = self._deps(eng, reads, writes)
        self.count[eng] += 1
        key = ("eng", eng, self.gen[eng])
        self.sem(key)
        me = (key, self.count[eng])
        rec = ["op", fn, waits, eng != "pe", key]
        if eng == "pe":
            self.pe_entries[self.count[eng]] = rec
        else:
            self.maxcount = max(self.maxcount, self.count[eng])
        self.ops[eng].append(rec)
        self._commit(me, reads, writes)
        self.n_inst += 1 + len(waits)

    def dma(self, eng, out, in_, reads=(), writes=(), semkey=None, **kw):
        if eng is None:
            eng = ("sp", "pool")[self.rr % 2]
            self.rr += 1
        waits = self._deps(eng, reads, writes)
        lkey = semkey if semkey is not None else (writes[0] if writes else reads[0])
        if lkey not in self.keymap:
            self.keymap[lkey] = len(self.keymap)
        idx = self.keymap[lkey]
        key = ("dma", idx, self.dgen.get(idx, 0))
        self.sem(key)
        self.dma_cnt[key] = self.dma_cnt.get(key, 0) + 1
        self.maxcount = max(self.maxcount, 16 * self.dma_cnt[key])
        me = (key, 16 * self.dma_cnt[key])
        self.ops[eng].append(["dma", (out, in_, kw), waits, None, key])
        self._commit(me, reads, writes)
        self.n_inst += 1 + len(waits)

    def barrier(self):
        allk = {}
        for e in ENGS:
            if self.count[e]:
                allk[("eng", e, self.gen[e])] = self._pe_value(self.count[e]) if e == "pe" else self.count[e]
        for k, c in self.dma_cnt.items():
            allk[k] = 16 * c
        assert max(allk.values() or [0]) < self.LIMIT, ("semaphore count too large", max(allk.values()))
        for e in ENGS:
            waits = []
            for k, v in allk.items():
                if self.seen[e].get(k, 0) < v:
                    self.seen[e][k] = v
                    waits.append((k, v))
            if waits:
                self.ops[e].append(["wait", None, waits, None, None])
                self.n_inst += len(waits)
        self.last_w = {}
        self.readers = {}

    def max_sem_count(self):
        return self.maxcount

    def emit(self):
        nc = self.nc
        with nc.Block() as block:
            def body(ename):
                def run(eng):
                    for kind, payload, waits, flag, key in self.ops[ename]:
                        for k, v in waits:
                            eng.wait_ge(self.sems[k], v)
                        if kind == "op":
                            ins = payload(eng)
                            if flag:
                                ins.then_inc(self.sems[key], 1)
                        elif kind == "dma":
                            out, in_, kw = payload
                            eng.dma_start(out=out, in_=in_, **kw).then_inc(self.sems[key], 16)
                return run
            block.tensor(body("pe"))
            block.scalar(body("act"))
            block.vector(body("dve"))
            block.gpsimd(body("pool"))
            block.sync(body("sp"))
        self.ops = {e: [] for e in ENGS}
        for e in ENGS:
            val = len(self.pe_marks) if e == "pe" else self.count[e]
            if val > self.ENG_SWITCH:
                self.gen[e] += 1
                self.count[e] = 0
                if e == "pe":
                    self.pe_marks = []
                    self.pe_entries = {}
        for (tag, idx, g), c in list(self.dma_cnt.items()):
            if g == self.dgen.get(idx, 0) and 16 * c > self.DMA_SWITCH:
                self.dgen[idx] = g + 1
        self.keymap = {}

    def close(self):
        self.stack.close()


def t5_bucket_np(dist):
    d = np.maximum(dist, 0)
    large = 16 + (np.log(np.maximum(d, 1).astype(np.float32) / np.float32(16))
                  / np.float32(math.log(128 / 16)) * np.float32(16)).astype(np.int32)
    large = np.minimum(large, 31)
    return np.where(d < 16, d, large)


def host_consts(T):
    bf = ml_dtypes.bfloat16
    c = {}
    k = np.arange(128)[:, None]
    q = np.arange(128)[None, :]
    c["c_tri"] = (k <= q).astype(bf)
    c["c_ident"] = np.eye(128, dtype=bf)
    c["c_identf"] = np.eye(128, dtype=np.float32)
    inv = (10000.0 ** (-np.arange(0, 32, 2, dtype=np.float32) / np.float32(32))).astype(np.float32)
    ang = np.arange(T, dtype=np.float32)[None, :] * inv[:, None]
    cos, sin = np.cos(ang).astype(np.float32), np.sin(ang).astype(np.float32)
    cos96 = np.ones((96, T), np.float32)
    sin96 = np.zeros((96, T), np.float32)
    cos96[64:80], cos96[80:96] = cos, cos
    sin96[64:80], sin96[80:96] = sin, sin
    c["c_cos96"], c["c_sin96"] = cos96, sin96
    R = np.zeros((96, 96), np.float32)
    for i in range(16):
        R[64 + 16 + i, 64 + i] = -1.0
        R[64 + i, 64 + 16 + i] = 1.0
    c["c_rot96"] = R.astype(bf)
    LW, LC = 512, 4352
    dw = np.arange(LW) - 127
    ohw = np.zeros((32, LW), np.float32)
    ohw[t5_bucket_np(dw), np.arange(LW)] = 1.0
    ohw[:, dw < 0] = 0.0
    c["c_ohw"] = ohw
    vw = np.zeros((16, LW), np.float32)
    vw[0:8] = ((dw >= 0) & (dw < 256)).astype(np.float32)
    vw[8:16] = ((dw >= 0) & (dw < 128)).astype(np.float32)
    c["c_validw"] = vw
    dc = np.arange(LC) - 2063
    ohc = np.zeros((32, LC), np.float32)
    ohc[t5_bucket_np(dc), np.arange(LC)] = 1.0
    ohc[:, dc < 0] = 0.0
    c["c_ohc"] = ohc
    c["c_validc"] = np.broadcast_to((dc >= 0).astype(np.float32), (8, LC)).copy()
    kk = np.arange(T)
    c["c_esel"] = (kk[None, :] // 64 == np.arange(128)[:, None]).astype(bf)
    ci = np.arange(512)[:, None]
    sj = np.arange(128)[None, :]
    ncmp = (T - 32) // 16 + 1
    ov = ((ci * 16 + 31 >= sj * 64) & (ci * 16 <= sj * 64 + 63) & (ci < ncmp))
    c["c_overlap"] = ov.astype(bf)
    qpos = np.arange(T)[:, None]
    cur = qpos // 64
    s = np.arange(128)[None, :]
    forced = (s == 0) | (s == cur) | (s == cur - 1)
    valid = s <= cur
    add = np.where(valid, np.where(forced, np.float32(1e9), np.float32(0.0)), np.float32(-1.0)).astype(np.float32)
    c["c_topadd"] = add.reshape(T // 128, 128, 128)
    return c


def build(T, n_layers=1, stages="ABC", debug=False, io=None):
    NLP = n_layers
    fused = n_layers > 1
    io = io or {}
    nc = bass.Bass("TRN2", target_bir_lowering=False)
    NT = T // 128
    NG = T // 512
    P = Prog(nc)
    dt = nc.dram_tensor
    in_names = []
    dbg = {}
    sA, sB, sS, sN, sC = [c in stages for c in "ABSNC"]

    def din(name, shape, dtype=F32, need=True):
        if not need:
            return None
        in_names.append(name)
        return dt(name, list(shape), dtype, kind="ExternalInput").ap()

    def dsc(name, shape, dtype):
        return dt(name, list(shape), dtype).ap()

    def dout(name, shape, dtype=F32):
        a = dt(name, list(shape), dtype, kind="ExternalOutput").ap()
        dbg[name] = a
        return a

    def mk(name, shape, dtype):
        kind = io.get(name)
        if kind == "in":
            return din(name, shape, dtype)
        if kind == "out" or debug:
            return dout(name, shape, dtype)
        return dsc(name, shape, dtype)

    x = din("x", [T, D], need=sA or sC)
    w_fm = din("w_fm", [NLP, D, NFM], need=sA)
    w_tm = din("w_tm", [NLP, D, NTM], need=sA)
    norm_attn = din("norm_attn", [NLP, D], need=sA)
    q_norm = din("mla_q_norm", [NLP, 384], need=sA)
    w_uq = din("mla_w_uq", [NLP, 384, 768], need=sA)
    kv_norm = din("mla_kv_norm", [NLP, 128], need=sA)
    w_ukv_k = din("w_ukv_k", [NLP, 128, 512], need=sA)
    w_ukv_v = din("w_ukv_v", [NLP, 128, 512], need=sA)
    fox_b = din("fox_b_f", [NLP, 8], need=sB)
    group_norm = din("group_norm", [NLP, D], need=sC)
    w_out = din("w_out", [NLP, D, D], need=sC)
    norm_mlp = din("norm_mlp", [NLP, D], need=sC)
    w_up = din("w_up", [NLP, D, DFF], need=sC)
    w_down = din("w_down", [NLP, DFF, D], need=sC)
    final_norm = din("final_norm", [D], need=sC)
    nsa_pos = din("nsa_cmp_pos", [NLP, 2, 32, 64], need=sN)
    nsa_w1 = din("nsa_cmp_w1", [NLP, 2, 2048, 64], need=sN)
    nsa_w2 = din("nsa_cmp_w2", [NLP, 2, 64, 64], need=sN)
    swa_sinks = din("swa_sinks", [NLP, 8], need=sS)
    rel_bias = din("rel_bias", [32, 16], need=sS or sN)
    if sC:
        y_out = dt("y", [T, D], F32, kind="ExternalOutput").ap()
        if fused:
            hres = dsc("hres", [T, D], F32)
        else:
            hn_out = dt("hn", [T, D], F32, kind="ExternalOutput").ap()
        wb_out = dsc("wb_out", [NLP, D, D], BF16)
        wb_up = dsc("wb_up", [NLP, D, DFF], BF16)
        wb_dn = dsc("wb_dn", [NLP, DFF, D], BF16)
    consts = {}
    hc = host_consts(T)
    need_c = {"c_tri": sB, "c_ident": sA or sC, "c_identf": sN, "c_cos96": sA, "c_sin96": sA, "c_rot96": sA,
              "c_ohw": sS or sN, "c_validw": sS or sN, "c_ohc": sS or sN, "c_validc": sS or sN,
              "c_esel": sN, "c_overlap": sN, "c_topadd": sN}
    for k_, v_ in hc.items():
        consts[k_] = din(k_, v_.shape, BF16 if v_.dtype == ml_dtypes.bfloat16 else F32, need=need_c.get(k_, True))
    ocat_keep = [None]
    vstage_keep = [None]

    if sA:
        wb_fm = dsc("wb_fm", [NLP, D, NFM], BF16)
        wb_tm = dsc("wb_tm", [NLP, D, NTM], BF16)
        wb_uq = dsc("wb_uq", [NLP, 384, 768], BF16)
        wb_ukvk = dsc("wb_ukvk", [NLP, 128, 512], BF16)
        wb_ukvv = dsc("wb_ukvv", [NLP, 128, 512], BF16)
    pT = mk("pT", [NFM - 128, T], BF16)
    fT = mk("fT", [8, T], F32)
    pTM = mk("pTM", [T, NTM], F32)
    pTMb = mk("pTMb", [T, NTM], BF16)
    osel = mk("osel", [T, 512], BF16)
    owin = mk("owin", [T, 512], BF16)
    qT_mla = mk("qT_mla", [8, 96, T], BF16)
    kT_mla = mk("kT_mla", [8, 64, T], BF16)
    kpeT = mk("kpeT", [32, T], BF16)
    v_mla = mk("v_mla", [T, 512], BF16)
    foxq_aug = mk("foxq_aug", [8, 6, T], BF16)
    foxk_aug = mk("foxk_aug", [8, 6, T], BF16)
    if io.get("ocat") == "in":
        ocat_in = din("ocat_in", [T, D], BF16)
        ocat = dsc("ocat", [T, D], BF16)
        P.dma("sp", ocat[:, 0:1024], ocat_in[:, 0:1024], semkey="cp_ocat")
        P.barrier()
    else:
        ocat = mk("ocat", [T, D], BF16)
    ocat_keep[0] = ocat
    dbg["_inputs"] = in_names

    with contextlib.ExitStack() as st:
        gcol = P.sbuf("gcol", [128, 64], F32, st)
        stage_f = [P.sbuf("wst%d" % i, [128, 2816], F32, st) for i in range(2)]
        stage_b = [P.sbuf("wsb%d" % i, [128, 2816], BF16, st) for i in range(2)]
        cnt = [0]

        def prep(src, dst, rows, cols, gain_ap, gkey):
            nch = rows // 128
            if gain_ap is not None:
                P.dma("sp", gcol[:, 0:nch], gain_ap.rearrange("(c p) -> p c", p=128),
                      writes=["gcol"], allow_slow_non_contiguous=True)
            for c in range(nch):
                i = cnt[0] % 2
                cnt[0] += 1
                P.dma("sp", stage_f[i][:, 0:cols], src[c * 128:(c + 1) * 128, :], writes=["wst%d" % i])
                if gain_ap is not None:
                    eng = "dve" if c % 2 == 0 else "pool"
                    P.op(eng, lambda e, i=i, c=c: e.tensor_scalar(
                        out=stage_b[i][:, 0:cols], in0=stage_f[i][:, 0:cols],
                        scalar1=gcol[:, c:c + 1], scalar2=None, op0=ALU.mult),
                        reads=["wst%d" % i, "gcol"], writes=["wsb%d" % i])
                else:
                    P.op("act", lambda e, i=i: e.copy(out=stage_b[i][:, 0:cols], in_=stage_f[i][:, 0:cols]),
                         reads=["wst%d" % i], writes=["wsb%d" % i])
                P.dma("pool", dst[c * 128:(c + 1) * 128, :], stage_b[i][:, 0:cols],
                      reads=["wsb%d" % i], semkey="wprep_st%d" % i)

        for l in range(n_layers):
            if sA:
                prep(w_fm[l], wb_fm[l], D, NFM, norm_attn[l], "ga")
                prep(w_tm[l], wb_tm[l], D, NTM, norm_attn[l], "ga")
                prep(w_uq[l], wb_uq[l], 384, 768, q_norm[l], "gq")
                prep(w_ukv_k[l], wb_ukvk[l], 128, 512, kv_norm[l], "gk")
                prep(w_ukv_v[l], wb_ukvv[l], 128, 512, kv_norm[l], "gk")
            if "C" in stages:
                prep(w_out[l], wb_out[l], D, D, group_norm[l], "gg")
                for c0 in range(0, DFF, 2048):
                    prep(w_up[l][:, c0:c0 + 2048], wb_up[l][:, c0:c0 + 2048], D, 2048, norm_mlp[l], "gm")
                prep(w_down[l], wb_dn[l], DFF, D, None, None)
        P.barrier()
        P.emit()

    for l in range(n_layers):
        hsrc = x if l == 0 else hres
        if "A" in stages:
            with contextlib.ExitStack() as st:
                ident = P.sbuf("ident", [128, 128], BF16, st)
                rot96 = P.sbuf("rot96", [96, 96], BF16, st)
                P.dma("sp", ident[:], consts["c_ident"][:, :], semkey="ld_const")
                P.dma("sp", rot96[:], consts["c_rot96"][:, :], semkey="ld_const")
                rot32 = P.sbuf("rot32", [32, 32], BF16, st)
                P.dma("sp", rot32[:], consts["c_rot96"][64:96, 64:96], semkey="ld_const")
                cos32 = P.sbuf("cos32", [32, 512], F32, st)
                sin32 = P.sbuf("sin32", [32, 512], F32, st)
                hx = [P.sbuf("hx%d" % i, [128, D], F32, st) for i in range(2)]
                ub = [P.sbuf("ub%d" % i, [128, D], BF16, st) for i in range(2)]
                junk = P.sbuf("junk", [128, D], BF16, st)
                ss = P.sbuf("ss", [128, 8], F32, st)
                uT = P.sbuf("uT", [128, 16, 512], BF16, st)
                wsl = [P.sbuf("wsl%d" % i, [128, 16, 512], BF16, st) for i in range(2)]
                pfm = [P.sbuf("pfm%d" % i, [128, 512], BF16, st) for i in range(2)]
                pff = P.sbuf("pff", [128, 512], F32, st)
                ptm = [P.sbuf("ptm%d" % i, [128, 512], F32, st) for i in range(2)]
                ptmb = [P.sbuf("ptmb%d" % i, [128, 512], BF16, st) for i in range(2)]
                mla_in = P.sbuf("mla_in", [128, 4, 544], F32, st)
                cqn = P.sbuf("cqn", [128, 544], BF16, st)
                cT = P.sbuf("cT", [128, 5, 512], BF16, st)
                wuq = P.sbuf("wuq", [128, 3, 768], BF16, st)
                wkk = P.sbuf("wkk", [128, 512], BF16, st)
                wkv = P.sbuf("wkv", [128, 512], BF16, st)
                cos_t = P.sbuf("cos_t", [96, 512], F32, st)
                sin_t = P.sbuf("sin_t", [96, 512], F32, st)
                qsb = P.sbuf("qsb", [96, 512], BF16, st)
                qf1 = P.sbuf("qf1", [96, 512], F32, st)
                qf2 = P.sbuf("qf2", [96, 512], F32, st)
                qo = [P.sbuf("qo%d" % i, [96, 512], BF16, st) for i in range(2)]
                ps_t = [P.psum("ps_t%d" % i, [128, 512], BF16, st) for i in range(2)]
                ps_m = [P.psum("ps_m%d" % i, [128, 512], F32, st) for i in range(3)]
                P.dma("sp", wuq[:], wb_uq[l].rearrange("(c p) n -> p c n", p=128), semkey="ld_const")
                P.dma("sp", wkk[:], wb_ukvk[l], semkey="ld_const")
                P.dma("sp", wkv[:], wb_ukvv[l], semkey="ld_const")
                P.barrier()
                slab_i = [0]
                pm_i = [0]
                pt_i = [0]

                def load_slab(wsrc, c0, ncols):
                    i = slab_i[0] % 2
                    slab_i[0] += 1
                    P.dma(None, wsl[i][:, :, 0:ncols],
                          wsrc[:, c0:c0 + ncols].rearrange("(c p) n -> p c n", p=128),
                          writes=["wsl%d" % i])
                    return i

                for g in range(NG):
                    for tb in range(4):
                        t0 = g * 512 + tb * 128
                        i = tb % 2
                        P.dma("sp", hx[i][:], hsrc[t0:t0 + 128, :], writes=["hx%d" % i])
                        P.op("act", lambda e, i=i, tb=tb: e.activation(
                            out=junk[:], in_=hx[i][:], func=AF.Square, accum_out=ss[:, tb:tb + 1]),
                            reads=["hx%d" % i], writes=["junk", "ss%d" % tb])
                        P.op("dve", lambda e, tb=tb: e.tensor_scalar(
                            out=ss[:, tb:tb + 1], in0=ss[:, tb:tb + 1], scalar1=1.0 / D, scalar2=EPS,
                            op0=ALU.mult, op1=ALU.add), reads=["ss%d" % tb], writes=["ss%d" % tb])
                        P.op("act", lambda e, tb=tb: e.activation(
                            out=ss[:, tb:tb + 1], in_=ss[:, tb:tb + 1], func=AF.Sqrt),
                            reads=["ss%d" % tb], writes=["ss%d" % tb])
                        P.op("dve", lambda e, tb=tb: e.reciprocal(out=ss[:, tb:tb + 1], in_=ss[:, tb:tb + 1]),
                             reads=["ss%d" % tb], writes=["ss%d" % tb])
                        P.op("dve", lambda e, i=i, tb=tb: e.tensor_scalar(
                            out=ub[i][:], in0=hx[i][:], scalar1=ss[:, tb:tb + 1], scalar2=None, op0=ALU.mult),
                            reads=["hx%d" % i, "ss%d" % tb], writes=["ub%d" % i])
                        for c4 in range(4):
                            j = pt_i[0] % 2
                            pt_i[0] += 1
                            for cc in range(4):
                                c = c4 * 4 + cc
                                P.op("pe", lambda e, i=i, c=c, cc=cc, j=j: e.transpose(
                                    out=ps_t[j][:, cc * 128:(cc + 1) * 128], in_=ub[i][:, c * 128:(c + 1) * 128],
                                    identity=ident[:]), reads=["ub%d" % i, "ident"], writes=["ps_t%d" % j])
                            P.op("act" if c4 % 2 else "dve", lambda e, c4=c4, tb=tb, j=j: (
                                e.copy if hasattr(e, "copy") and not hasattr(e, "tensor_copy") else e.tensor_copy)(
                                out=uT[:, c4 * 4:(c4 + 1) * 4, tb * 128:(tb + 1) * 128],
                                in_=ps_t[j][:].rearrange("p (c t) -> p c t", c=4)),
                                reads=["ps_t%d" % j], writes=["uT%d" % tb])
                    uTk = ["uT%d" % tb for tb in range(4)]
                    for s in range(NFM // 512 + 1):
                        ncols = min(512, NFM - s * 512)
                        si = load_slab(wb_fm[l], s * 512, ncols)
                        for c in range(ncols // 128):
                            row0 = s * 512 + c * 128
                            j = pm_i[0] % 3
                            pm_i[0] += 1
                            for kc in range(16):
                                P.op("pe", lambda e, si=si, c=c, kc=kc, j=j: e.matmul(
                                    ps_m[j][:], lhsT=wsl[si][:, kc, c * 128:(c + 1) * 128], rhs=uT[:, kc, :],
                                    start=(kc == 0), stop=(kc == 15)),
                                    reads=["wsl%d" % si] + uTk, writes=["ps_m%d" % j])
                            if row0 == FM_F:
                                P.op("act", lambda e, j=j: e.copy(out=pff[0:8, :], in_=ps_m[j][0:8, :]),
                                     reads=["ps_m%d" % j], writes=["pff"])
                                P.dma("pool", fT[:, g * 512:(g + 1) * 512], pff[0:8, :], reads=["pff"], semkey="st_pff")
                            else:
                                i2 = (row0 // 128) % 2
                                P.op("act" if i2 else "dve", lambda e, j=j, i2=i2: (
                                    e.copy if not hasattr(e, "tensor_copy") else e.tensor_copy)(
                                    out=pfm[i2][:], in_=ps_m[j][:]),
                                    reads=["ps_m%d" % j], writes=["pfm%d" % i2])
                                P.dma("pool", pT[row0:row0 + 128, g * 512:(g + 1) * 512], pfm[i2][:],
                                      reads=["pfm%d" % i2], semkey="st_pfm%d" % i2)
                    for s in range(NTM // 512):
                        si = load_slab(wb_tm[l], s * 512, 512)
                        for tb in range(4):
                            t0 = g * 512 + tb * 128
                            j = pm_i[0] % 3
                            pm_i[0] += 1
                            for kc in range(16):
                                P.op("pe", lambda e, si=si, tb=tb, kc=kc, j=j: e.matmul(
                                    ps_m[j][:], lhsT=uT[:, kc, tb * 128:(tb + 1) * 128], rhs=wsl[si][:, kc, :],
                                    start=(kc == 0), stop=(kc == 15)),
                                    reads=["wsl%d" % si, "uT%d" % tb], writes=["ps_m%d" % j])
                            i2 = tb % 2
                            P.op("act" if i2 else "dve", lambda e, j=j, i2=i2: (
                                e.copy if not hasattr(e, "tensor_copy") else e.tensor_copy)(
                                out=ptm[i2][:], in_=ps_m[j][:]),
                                reads=["ps_m%d" % j], writes=["ptm%d" % i2])
                            P.dma("pool", pTM[t0:t0 + 128, s * 512:(s + 1) * 512], ptm[i2][:],
                                  reads=["ptm%d" % i2], semkey="st_ptm%d" % i2)
                            P.op("pool", lambda e, i2=i2: e.tensor_copy(out=ptmb[i2][:], in_=ptm[i2][:]),
                                 reads=["ptm%d" % i2], writes=["ptmb%d" % i2])
                            P.dma("sp", pTMb[t0:t0 + 128, s * 512:(s + 1) * 512], ptmb[i2][:],
                                  reads=["ptmb%d" % i2], semkey="st_ptmb%d" % i2)
                            if s == 0:
                                P.op("dve", lambda e, i2=i2, tb=tb: e.tensor_copy(
                                    out=mla_in[:, tb, 0:512], in_=ptm[i2][:]),
                                    reads=["ptm%d" % i2], writes=["mla_in%d" % tb])
                            if s == 1:
                                P.op("dve", lambda e, i2=i2, tb=tb: e.tensor_copy(
                                    out=mla_in[:, tb, 512:544], in_=ptm[i2][:, 0:32]),
                                    reads=["ptm%d" % i2], writes=["mla_in%d" % tb])
                    for tb in range(4):
                        for (o0, w, col) in ((0, 384, 4), (384, 128, 5)):
                            P.op("act", lambda e, tb=tb, o0=o0, w=w, col=col: e.activation(
                                out=junk[:, 0:w], in_=mla_in[:, tb, o0:o0 + w], func=AF.Square,
                                accum_out=ss[:, col:col + 1]),
                                reads=["mla_in%d" % tb], writes=["junk", "ssm%d" % col])
                            P.op("dve", lambda e, w=w, col=col: e.tensor_scalar(
                                out=ss[:, col:col + 1], in0=ss[:, col:col + 1], scalar1=1.0 / w, scalar2=EPS,
                                op0=ALU.mult, op1=ALU.add), reads=["ssm%d" % col], writes=["ssm%d" % col])
                            P.op("act", lambda e, col=col: e.activation(
                                out=ss[:, col:col + 1], in_=ss[:, col:col + 1], func=AF.Sqrt),
                                reads=["ssm%d" % col], writes=["ssm%d" % col])
                            P.op("dve", lambda e, col=col: e.reciprocal(out=ss[:, col:col + 1], in_=ss[:, col:col + 1]),
                                 reads=["ssm%d" % col], writes=["ssm%d" % col])
                            P.op("dve", lambda e, tb=tb, o0=o0, w=w, col=col: e.tensor_scalar(
                                out=cqn[:, o0:o0 + w], in0=mla_in[:, tb, o0:o0 + w], scalar1=ss[:, col:col + 1],
                                scalar2=None, op0=ALU.mult),
                                reads=["mla_in%d" % tb, "ssm%d" % col], writes=["cqn"])
                        P.op("dve", lambda e, tb=tb: e.tensor_copy(out=cqn[:, 512:544], in_=mla_in[:, tb, 512:544]),
                             reads=["mla_in%d" % tb], writes=["cqn"])
                        j = pt_i[0] % 2
                        pt_i[0] += 1
                        for c in range(4):
                            P.op("pe", lambda e, c=c, j=j: e.transpose(
                                out=ps_t[j][:, c * 128:(c + 1) * 128], in_=cqn[:, c * 128:(c + 1) * 128],
                                identity=ident[:]), reads=["cqn", "ident"], writes=["ps_t%d" % j])
                        P.op("dve", lambda e, tb=tb, j=j: e.tensor_copy(
                            out=cT[:, 0:4, tb * 128:(tb + 1) * 128],
                            in_=ps_t[j][:].rearrange("p (c t) -> p c t", c=4)),
                            reads=["ps_t%d" % j], writes=["cT%d" % tb])
                        j = pt_i[0] % 2
                        pt_i[0] += 1
                        P.op("pe", lambda e, j=j: e.transpose(
                            out=ps_t[j][0:32, 0:128], in_=cqn[:, 512:544], identity=ident[:]),
                            reads=["cqn", "ident"], writes=["ps_t%d" % j])
                        P.op("dve", lambda e, tb=tb, j=j: e.tensor_copy(
                            out=cT[0:32, 4, tb * 128:(tb + 1) * 128], in_=ps_t[j][0:32, 0:128]),
                            reads=["ps_t%d" % j], writes=["cT%d" % tb])
                    cTk = ["cT%d" % tb for tb in range(4)]
                    P.dma("sp", cos_t[:], consts["c_cos96"][:, g * 512:(g + 1) * 512], writes=["ropetab"])
                    P.dma("sp", sin_t[:], consts["c_sin96"][:, g * 512:(g + 1) * 512], writes=["ropetab"])

                    P.dma("sp", cos32[:], consts["c_cos96"][64:96, g * 512:(g + 1) * 512], writes=["ropetab"])
                    P.dma("sp", sin32[:], consts["c_sin96"][64:96, g * 512:(g + 1) * 512], writes=["ropetab"])

                    def rope_out(src_key, src_ap, rows, rot_ap, rot_key, cos_ap, sin_ap, ckeys, dst_ap):
                        P.op("act", lambda e: e.copy(out=qsb[0:rows, :], in_=src_ap),
                             reads=(cTk if src_key == "cTall" else [src_key]), writes=["qsb"])
                        jj = pm_i[0] % 3
                        pm_i[0] += 1
                        P.op("pe", lambda e, jj=jj: e.matmul(
                            ps_m[jj][0:rows, :], lhsT=rot_ap, rhs=qsb[0:rows, :], start=True, stop=True),
                            reads=["qsb", rot_key], writes=["ps_m%d" % jj])
                        P.op("dve", lambda e: e.tensor_tensor(out=qf1[0:rows, :], in0=qsb[0:rows, :],
                                                               in1=cos_ap, op=ALU.mult),
                             reads=["qsb", ckeys[0]], writes=["qf1"])
                        P.op("dve", lambda e, jj=jj: e.tensor_tensor(out=qf2[0:rows, :], in0=ps_m[jj][0:rows, :],
                                                                      in1=sin_ap, op=ALU.mult),
                             reads=["ps_m%d" % jj, ckeys[1]], writes=["qf2"])
                        oi = pm_i[0] % 2
                        P.op("pool", lambda e, oi=oi: e.tensor_tensor(out=qo[oi][0:rows, :], in0=qf1[0:rows, :],
                                                                       in1=qf2[0:rows, :], op=ALU.add),
                             reads=["qf1", "qf2"], writes=["qo%d" % oi])
                        P.dma("pool", dst_ap, qo[oi][0:rows, :], reads=["qo%d" % oi], semkey="st_qo%d" % oi)

                    for h in range(8):
                        j = pm_i[0] % 3
                        pm_i[0] += 1
                        for kc in range(3):
                            P.op("pe", lambda e, h=h, kc=kc, j=j: e.matmul(
                                ps_m[j][0:96, :], lhsT=wuq[:, kc, h * 96:(h + 1) * 96], rhs=cT[:, kc, :],
                                start=(kc == 0), stop=(kc == 2)),
                                reads=["wuq"] + cTk, writes=["ps_m%d" % j])
                        rope_out("ps_m%d" % j, ps_m[j][0:96, :], 96, rot96[:, :], "rot96", cos_t[:, :], sin_t[:, :],
                                 ("ropetab", "ropetab"), qT_mla[h, :, g * 512:(g + 1) * 512])
                        j = pm_i[0] % 3
                        pm_i[0] += 1
                        P.op("pe", lambda e, h=h, j=j: e.matmul(
                            ps_m[j][0:64, :], lhsT=wkk[:, h * 64:(h + 1) * 64], rhs=cT[:, 3, :],
                            start=True, stop=True), reads=["wkk"] + cTk, writes=["ps_m%d" % j])
                        i2 = h % 2
                        P.op("act", lambda e, j=j, i2=i2: e.copy(out=pfm[i2][0:64, :], in_=ps_m[j][0:64, :]),
                             reads=["ps_m%d" % j], writes=["pfm%d" % i2])
                        P.dma("pool", kT_mla[h, :, g * 512:(g + 1) * 512], pfm[i2][0:64, :],
                              reads=["pfm%d" % i2], semkey="st_pfm%d" % i2)
                    rope_out("cTall", cT[0:32, 4, :], 32, rot32[:, :], "rot32", cos32[:, :], sin32[:, :],
                             ("ropetab", "ropetab"), kpeT[:, g * 512:(g + 1) * 512])
                    for tb in range(4):
                        t0 = g * 512 + tb * 128
                        j = pm_i[0] % 3
                        pm_i[0] += 1
                        P.op("pe", lambda e, tb=tb, j=j: e.matmul(
                            ps_m[j][:], lhsT=cT[:, 3, tb * 128:(tb + 1) * 128], rhs=wkv[:, :], start=True, stop=True),
                            reads=["wkv", "cT%d" % tb], writes=["ps_m%d" % j])
                        i2 = tb % 2
                        P.op("act", lambda e, j=j, i2=i2: e.copy(out=pfm[i2][:], in_=ps_m[j][:]),
                             reads=["ps_m%d" % j], writes=["pfm%d" % i2])
                        P.dma("pool", v_mla[t0:t0 + 128, :], pfm[i2][:], reads=["pfm%d" % i2], semkey="st_pfm%d" % i2)
                P.barrier()
                P.emit()
        if "B" in stages:
            with contextlib.ExitStack() as st:
                fl = P.sbuf("fl", [8, T], F32, st)
                fo = P.sbuf("fo", [8, T], F32, st)
                fr = P.sbuf("fr", [8, T], F32, st)
                fb = [P.sbuf("fb%d" % i, [8, T], BF16, st) for i in range(3)]
                fnb = [P.sbuf("fnb%d" % i, [8, T], BF16, st) for i in range(2)]
                f8 = P.sbuf("f8", [8, T], BF16, st)
                nb = P.sbuf("nb", [8, 1], F32, st)
                P.dma("sp", fl[:], fT[:, :], writes=["fl"])
                P.dma("sp", nb[:], fox_b[l].rearrange("(h o) -> h o", o=1), writes=["nb"])
                P.op("dve", lambda e: e.tensor_scalar(out=nb[:], in0=nb[:], scalar1=-1.0, scalar2=None, op0=ALU.mult),
                     reads=["nb"], writes=["nb"])
                P.op("dve", lambda e: e.memset(fo[:], 1.0), writes=["fo"])
                P.op("pool", lambda e: e.memset(f8[:], 8.0), writes=["f8"])
                P.op("act", lambda e: e.activation(out=fl[:], in_=fl[:], func=AF.Exp, bias=nb[:], scale=-1.0),
                     reads=["fl", "nb"], writes=["fl"])
                P.op("act", lambda e: e.activation(out=fl[:], in_=fl[:], func=AF.Ln, bias=1.0, scale=1.0),
                     reads=["fl"], writes=["fl"])
                P.op("dve", lambda e: e.tensor_tensor_scan(out=fr[:], data0=fo[:], data1=fl[:], initial=0.0,
                                                           op0=ALU.mult, op1=ALU.add),
                     reads=["fl", "fo"], writes=["fr"])
                for i in range(3):
                    P.op("dve", lambda e, i=i: e.tensor_copy(out=fb[i][:], in_=fr[:]), reads=["fr"], writes=["fb%d" % i])
                    P.op("dve", lambda e, i=i: e.tensor_scalar(out=fnb[i % 2][:], in0=fb[i][:], scalar1=-1.0, scalar2=None,
                                                               op0=ALU.mult), reads=["fb%d" % i], writes=["fnb%d" % (i % 2)])
                    if i < 2:
                        P.op("dve", lambda e, i=i: e.tensor_tensor(out=fr[:], in0=fr[:], in1=fb[i][:], op=ALU.subtract),
                             reads=["fr", "fb%d" % i], writes=["fr"])
                    P.dma("sp", foxk_aug[:, i, :], fb[i][:], reads=["fb%d" % i], semkey="st_fb%d" % i)
                    P.dma("sp", foxq_aug[:, 3 + i, :], fnb[i % 2][:], reads=["fnb%d" % (i % 2)], semkey="st_fnb%d" % (i % 2))
                    P.dma("pool", foxk_aug[:, 3 + i, :], f8[:], reads=["f8"], semkey="st_f8")
                    P.dma("pool", foxq_aug[:, i, :], f8[:], reads=["f8"], semkey="st_f8")
                P.barrier()
                P.emit()
            with contextlib.ExitStack() as st:
                tri = P.sbuf("tri", [128, 128], BF16, st)
                P.dma("sp", tri[:], consts["c_tri"][:, :], writes=["tri"])
                qt = [P.sbuf("qt%d" % i, [128, T], BF16, st) for i in range(2)]
                kt = [P.sbuf("kt%d" % i, [128, T], BF16, st) for i in range(2)]
                vt = [P.sbuf("vt%d" % i, [128, NT, 65], BF16, st) for i in range(2)]
                pt = [P.sbuf("pt%d" % i, [128, 512], BF16, st) for i in range(3)]
                rd = P.sbuf("rd", [128, 4], F32, st)
                vstb = P.sbuf("vstb", [128, NT, 64], BF16, st)
                osb = [P.sbuf("osb%d" % i, [128, 64], BF16, st) for i in range(2)]
                ps_s = [P.psum("ps_s%d" % i, [128, 512], F32, st) for i in range(2)]
                ps_o = [P.psum("ps_o%d" % i, [128, 512], F32, st) for i in range(4)]
                for i in range(2):
                    P.op("pool", lambda e, i=i: e.memset(vt[i][:, :, 64:65], 1.0), writes=["vt%d_1" % i])
                heads = []
                for h in range(8):
                    heads.append(dict(q=[(qT_mla[h], 0, 96)], k=[(kT_mla[h], 0, 64), (kpeT, 64, 32)],
                                      v=v_mla[:, h * 64:(h + 1) * 64], d=96, scale=96 ** -0.5, col=h * 64))
                for h in range(8):
                    heads.append(dict(q=[(pT[FM_FOXQ + h * 64:FM_FOXQ + (h + 1) * 64], 0, 64), (foxq_aug[h], 64, 6)],
                                      k=[(pT[FM_FOXK + h * 64:FM_FOXK + (h + 1) * 64], 0, 64), (foxk_aug[h], 64, 6)],
                                      v=pTMb[:, TM_FOXV + h * 64:TM_FOXV + (h + 1) * 64], d=70, scale=0.125, col=512 + h * 64))
                oi = [0]
                for hi, H in enumerate(heads):
                    bi = hi % 2
                    def rkeys(pref, p0, n):
                        ks = []
                        if p0 < 64:
                            ks.append("%s%d_0" % (pref, bi))
                        if p0 + n > 64:
                            ks.append("%s%d_64" % (pref, bi))
                        return ks
                    qk_keys = []
                    for (ap_, p0, n) in H["q"]:
                        P.dma(None, qt[bi][p0:p0 + n, :], ap_, writes=rkeys("qt", p0, n), semkey="ld_qt%d_%d" % (bi, p0))
                        qk_keys += rkeys("qt", p0, n)
                    for (ap_, p0, n) in H["k"]:
                        P.dma(None, kt[bi][p0:p0 + n, :], ap_, writes=rkeys("kt", p0, n), semkey="ld_kt%d_%d" % (bi, p0))
                        qk_keys += rkeys("kt", p0, n)
                    qk_keys = sorted(set(qk_keys))
                    if H["v"] is not None:
                        P.dma(None, vstb[:], H["v"].rearrange("(j p) d -> p j d", p=128), writes=["vstb"])
                        P.op("pool", lambda e, bi=bi: e.tensor_copy(out=vt[bi][:, :, 0:64], in_=vstb[:]),
                             reads=["vstb"], writes=["vt%d" % bi])
                    d = H["d"]
                    steps = [(c, j) for c in range(NG) for j in range(4 * c + 4)]

                    def qk(idx, bi=bi, d=d, qk_keys=qk_keys):
                        c, j = steps[idx]
                        s = idx % 2
                        P.op("pe", lambda e: e.matmul(ps_s[s][:], lhsT=kt[bi][0:d, j * 128:(j + 1) * 128],
                                                      rhs=qt[bi][0:d, c * 512:(c + 1) * 512], start=True, stop=True),
                             reads=qk_keys, writes=["ps_s%d" % s])
                    qk(0)
                    for idx, (c, j) in enumerate(steps):
                        s = idx % 2
                        pi = idx % 3
                        P.op("act", lambda e, s=s, pi=pi, H=H: e.activation(out=pt[pi][:], in_=ps_s[s][:], func=AF.Exp,
                                                                       scale=H["scale"]),
                             reads=["ps_s%d" % s], writes=["pt%d" % pi])
                        if j >= 4 * c:
                            b = j - 4 * c
                            P.op("dve", lambda e, pi=pi, b=b: e.tensor_tensor(
                                out=pt[pi][:, b * 128:(b + 1) * 128], in0=pt[pi][:, b * 128:(b + 1) * 128],
                                in1=tri[:], op=ALU.mult), reads=["pt%d" % pi, "tri"], writes=["pt%d" % pi])
                        if idx + 1 < len(steps):
                            qk(idx + 1)
                        for b in range(4):
                            qb = 4 * c + b
                            if j > qb:
                                continue
                            P.op("pe", lambda e, pi=pi, b=b, j=j, qb=qb, bi=bi: e.matmul(
                                ps_o[b][:, 0:65], lhsT=pt[pi][:, b * 128:(b + 1) * 128], rhs=vt[bi][:, j, :],
                                start=(j == 0), stop=(j == qb)),
                                reads=["pt%d" % pi, "vt%d" % bi, "vt%d_1" % bi], writes=["ps_o%d" % b])
                            if j == qb:
                                P.op("dve", lambda e, b=b: e.reciprocal(out=rd[:, b:b + 1], in_=ps_o[b][:, 64:65]),
                                     reads=["ps_o%d" % b], writes=["rd%d" % b])
                                o2 = oi[0] % 2
                                oi[0] += 1
                                P.op("dve", lambda e, b=b, o2=o2: e.tensor_scalar(
                                    out=osb[o2][:], in0=ps_o[b][:, 0:64], scalar1=rd[:, b:b + 1], scalar2=None,
                                    op0=ALU.mult), reads=["ps_o%d" % b, "rd%d" % b], writes=["osb%d" % o2])
                                P.dma("sp", ocat[qb * 128:(qb + 1) * 128, H["col"]:H["col"] + 64], osb[o2][:],
                                      reads=["osb%d" % o2], semkey="st_osb%d" % o2)
                P.barrier()
                P.emit()
        if "N" in stages or "S" in stages:
            LW, LC = 512, 4352
            if l == 0:
                Fw_h = dt("Fw", [16 * 128 * (LW + 1)], F32)
                Fc_h = dt("Fc", [8 * 128 * (LC + 16)], F32)
                tabw_d = dsc("tabw_d", [16, LW], F32)
                tabc_d = dsc("tabc_d", [8, LC], F32)
            with contextlib.ExitStack() as st:
              if l == 0:
                  rb = P.sbuf("rb", [32, 16], F32, st)
                  ohw = P.sbuf("ohw", [32, LW], F32, st)
                  ohc = P.sbuf("ohc", [32, LC], F32, st)
                  vw = P.sbuf("vw", [16, LW], F32, st)
                  vc_ = P.sbuf("vc_", [8, LC], F32, st)
                  tw = P.sbuf("tw", [16, LW], F32, st)
                  tc_ = P.sbuf("tc_", [8, LC], F32, st)
                  ps = P.psum("ps_tab", [16, 512], F32, st)
                  P.dma("sp", rb[:], rel_bias[:, :], writes=["rb"])
                  P.dma("sp", ohw[:], consts["c_ohw"][:, :], writes=["ohw"])
                  P.dma("sp", ohc[:], consts["c_ohc"][:, :], writes=["ohc"])
                  P.dma("sp", vw[:], consts["c_validw"][:, :], writes=["vw"])
                  P.dma("sp", vc_[:], consts["c_validc"][:, :], writes=["vc_"])
                  P.op("pe", lambda e: e.matmul(ps[:, 0:LW], lhsT=rb[:, :], rhs=ohw[:, :], start=True, stop=True),
                       reads=["rb", "ohw"], writes=["ps_tab"])
                  P.op("act", lambda e: e.activation(out=tw[:], in_=ps[:, 0:LW], func=AF.Exp), reads=["ps_tab"], writes=["tw"])
                  P.op("dve", lambda e: e.tensor_tensor(out=tw[:], in0=tw[:], in1=vw[:], op=ALU.mult),
                       reads=["tw", "vw"], writes=["tw"])
                  P.dma("sp", tabw_d[:, :], tw[:], reads=["tw"], semkey="st_tw")
                  for c0 in range(0, LC, 512):
                      n = min(512, LC - c0)
                      P.op("pe", lambda e, c0=c0, n=n: e.matmul(ps[0:8, 0:n], lhsT=rb[:, 0:8], rhs=ohc[:, c0:c0 + n],
                                                               start=True, stop=True),
                           reads=["rb", "ohc"], writes=["ps_tab"])
                      P.op("act", lambda e, c0=c0, n=n: e.activation(out=tc_[:, c0:c0 + n], in_=ps[0:8, 0:n], func=AF.Exp),
                           reads=["ps_tab"], writes=["tc_"])
                  P.op("dve", lambda e: e.tensor_tensor(out=tc_[:], in0=tc_[:], in1=vc_[:], op=ALU.mult),
                       reads=["tc_", "vc_"], writes=["tc_"])
                  P.dma("sp", tabc_d[:, :], tc_[:], reads=["tc_"], semkey="st_tc")
                  P.barrier()
                  for h in range(16):
                      P.dma(None, bass.AP(Fw_h, h * 128 * (LW + 1), [[LW + 1, 128], [1, LW]]),
                            tabw_d[h].partition_broadcast(128), semkey="st_fw")
                  for h in range(8):
                      P.dma(None, bass.AP(Fc_h, h * 128 * (LC + 16), [[LC + 16, 128], [1, LC]]),
                            tabc_d[h].partition_broadcast(128), semkey="st_fc")
                  P.barrier()
                  P.emit()

            def ewin_ap(h, delta):
                return bass.AP(Fw_h, h * 128 * (LW + 1) + 127 + 128 * delta, [[LW, 128], [1, 128]])

            def ecmp_ap(h, delta):
                return bass.AP(Fc_h, h * 128 * (LC + 16) + 2032 + 128 * delta, [[LC, 128], [1, 128]])

            def run_chunk(R, steps, rhs_q, qkeys, scale, fin):
                firsts, lasts = {}, {}
                for i, s_ in enumerate(steps):
                    for b in s_["blocks"]:
                        firsts.setdefault(b, i)
                        lasts[b] = i

                def qk(i):
                    s_ = steps[i]
                    si = R["si"] % 2
                    R["si"] += 1
                    s_["si"] = si
                    ex = s_.get("extra")
                    P.op("pe", lambda e: e.matmul(R["ps_s"][si][:], lhsT=s_["kl"], rhs=rhs_q, start=True,
                                                  stop=(ex is None)),
                         reads=list(s_["kkeys"]) + list(qkeys), writes=["ps_s%d" % si])
                    if ex is not None:
                        P.op("pe", lambda e: e.matmul(R["ps_s"][si][:], lhsT=ex[0], rhs=ex[1], start=False, stop=True),
                             reads=list(ex[2]), writes=["ps_s%d" % si])
                if not steps:
                    return
                qk(0)
                for i, s_ in enumerate(steps):
                    si = s_["si"]
                    pi = R["pi"] % 3
                    R["pi"] += 1
                    pt = R["pt"][pi]
                    bias = s_.get("bias")
                    if bias is not None:
                        P.op("act", lambda e, si=si, pt=pt, bias=bias: e.activation(
                            out=pt[:], in_=R["ps_s"][si][:], func=AF.Exp, bias=bias[0], scale=scale),
                            reads=["ps_s%d" % si, bias[1]], writes=["pt%d" % pi])
                    else:
                        P.op("act", lambda e, si=si, pt=pt: e.activation(
                            out=pt[:], in_=R["ps_s"][si][:], func=AF.Exp, scale=scale),
                            reads=["ps_s%d" % si], writes=["pt%d" % pi])
                    for b, act in s_["blocks"].items():
                        sub = pt[:, b * 128:(b + 1) * 128]
                        if act[0] == "mul":
                            P.op("dve", lambda e, sub=sub, act=act: e.tensor_tensor(out=sub, in0=sub, in1=act[1], op=ALU.mult),
                                 reads=["pt%d" % pi, act[2]], writes=["pt%d" % pi])
                        elif act[0] == "mulsc":
                            P.op("dve", lambda e, sub=sub, act=act: e.tensor_scalar(out=sub, in0=sub, scalar1=act[1],
                                                                                   scalar2=None, op0=ALU.mult),
                                 reads=["pt%d" % pi, act[2]], writes=["pt%d" % pi])
                        elif act[0] == "mul2":
                            P.op("dve", lambda e, sub=sub, act=act: e.scalar_tensor_tensor(
                                out=sub, in0=sub, scalar=act[1], in1=act[3], op0=ALU.mult, op1=ALU.mult),
                                reads=["pt%d" % pi, act[2], act[4]], writes=["pt%d" % pi])
                    if i + 1 < len(steps):
                        qk(i + 1)
                    for b in s_["blocks"]:
                        P.op("pe", lambda e, b=b, pt=pt, s_=s_, i=i: e.matmul(
                            R["ps_o"][b][:, 0:s_["vn"]], lhsT=pt[:, b * 128:(b + 1) * 128], rhs=s_["v"],
                            start=(firsts[b] == i), stop=(lasts[b] == i)),
                            reads=["pt%d" % pi] + list(s_["vkeys"]), writes=["ps_o%d" % b])
                        if lasts[b] == i:
                            fin(b, R["ps_o"][b])

            def load_v(R, dst, dst_key, src_ap):
                P.dma(None, R["vstb"][:], src_ap.rearrange("(j p) d -> p j d", p=128), writes=["vstb"])
                P.op("pool", lambda e: e.tensor_copy(out=dst[:, :, 0:64], in_=R["vstb"][:]),
                     reads=["vstb"], writes=[dst_key])

            def common_res(st, tag):
                R = dict(si=0, pi=0, oi=0)
                R["pt"] = [P.sbuf("pt%s%d" % (tag, i), [128, 512], BF16, st) for i in range(3)]
                R["ps_s"] = [P.psum("pss%s%d" % (tag, i), [128, 512], F32, st) for i in range(2)]
                R["ps_o"] = [P.psum("pso%s%d" % (tag, i), [128, 512], F32, st) for i in range(4)]
                R["vstb"] = P.sbuf("vstb%s" % tag, [128, NT, 64], BF16, st)
                R["rd"] = P.sbuf("rd%s" % tag, [128, 8], F32, st)
                R["osb"] = [P.sbuf("osb%s%d" % (tag, i), [128, 64], BF16, st) for i in range(2)]
                return R

            if "S" in stages:
                with contextlib.ExitStack() as st:
                    R = common_res(st, "s")
                    esw = P.sbuf("esw", [128, 16, 128], F32, st)
                    for h in range(8):
                        for dl in range(2):
                            P.dma(None, esw[:, h * 2 + dl, :], ewin_ap(8 + h, dl), writes=["esw"])
                    sk = P.sbuf("sk", [128, 8], F32, st)
                    P.dma("sp", sk[:], swa_sinks[l].partition_broadcast(128), writes=["sk"])
                    P.op("act", lambda e: e.activation(out=sk[:], in_=sk[:], func=AF.Exp), reads=["sk"], writes=["sk"])
                    qt = [P.sbuf("qts%d" % i, [64, T], BF16, st) for i in range(2)]
                    kt = P.sbuf("kts", [64, T], BF16, st)
                    vt = P.sbuf("vts", [128, NT, 65], BF16, st)
                    P.op("pool", lambda e: e.memset(vt[:, :, 64:65], 1.0), writes=["vt_1"])
                    for h in range(8):
                        g = h // 4
                        bi = h % 2
                        if h % 4 == 0:
                            P.dma(None, kt[:], pT[FM_SWAK + g * 64:FM_SWAK + (g + 1) * 64], writes=["kt"])
                            load_v(R, vt, "vt", pTMb[:, TM_SWAV + g * 64:TM_SWAV + (g + 1) * 64])
                        P.dma(None, qt[bi][:], pT[FM_SWAQ + h * 64:FM_SWAQ + (h + 1) * 64], writes=["qt%d" % bi])

                        def fin(b, pso, h=h, cbox=None):
                            qb = fin.c * 4 + b
                            P.op("dve", lambda e: e.tensor_scalar(out=R["rd"][:, b:b + 1], in0=pso[:, 64:65],
                                                                  scalar1=sk[:, h:h + 1], scalar2=None, op0=ALU.add),
                                 reads=["ps_o%d" % b, "sk"], writes=["rd%d" % b])
                            P.op("dve", lambda e: e.reciprocal(out=R["rd"][:, b:b + 1], in_=R["rd"][:, b:b + 1]),
                                 reads=["rd%d" % b], writes=["rd%d" % b])
                            o2 = R["oi"] % 2
                            R["oi"] += 1
                            P.op("dve", lambda e: e.tensor_scalar(out=R["osb"][o2][:], in0=pso[:, 0:64],
                                                                  scalar1=R["rd"][:, b:b + 1], scalar2=None, op0=ALU.mult),
                                 reads=["ps_o%d" % b, "rd%d" % b], writes=["osb%d" % o2])
                            P.dma("sp", ocat[qb * 128:(qb + 1) * 128, 1536 + h * 64:1536 + (h + 1) * 64], R["osb"][o2][:],
                                  reads=["osb%d" % o2], semkey="st_osb%d" % o2)
                        for c in range(NG):
                            fin.c = c
                            steps = []
                            for j in range(max(0, 4 * c - 1), 4 * c + 4):
                                blocks = {}
                                for b in range(4):
                                    dl = 4 * c + b - j
                                    if 0 <= dl <= 1:
                                        blocks[b] = ("mul", esw[:, h * 2 + dl, :], "esw")
                                steps.append(dict(kl=kt[:, j * 128:(j + 1) * 128], kkeys=["kt"], v=vt[:, j, :],
                                                  vkeys=["vt", "vt_1"], vn=65, blocks=blocks))
                            run_chunk(R, steps, qt[bi][:, c * 512:(c + 1) * 512], ["qt%d" % bi], 0.125, fin)
                    P.barrier()
                    P.emit()

            if "N" in stages:
                ncmp = (T - 32) // 16 + 1
                NCB = (ncmp + 127) // 128
                with contextlib.ExitStack() as st:
                    R = common_res(st, "n")
                    ps_x = P.psum("ps_x", [128, 512], F32, st)
                    identf = P.sbuf("identf", [128, 128], F32, st)
                    ewn = P.sbuf("ewn", [128, 24, 128], F32, st)
                    ecm = P.sbuf("ecm", [128, 2, 8, 128], F32, st)
                    b31 = P.sbuf("b31", [128, 16], F32, st)
                    e31 = P.sbuf("e31", [128, 16], F32, st)
                    em31 = P.sbuf("em31", [128, 16], F32, st)
                    gates = P.sbuf("gates", [128, NT, 24], F32, st)
                    esel = P.sbuf("esel", [128, T], BF16, st)
                    ovl = P.sbuf("ovl", [128, 4, 128], BF16, st)
                    P.dma("sp", identf[:], consts["c_identf"][:, :], semkey="ld_const")
                    for h in range(8):
                        for dl in range(3):
                            P.dma(None, ewn[:, h * 3 + dl, :], ewin_ap(h, dl), semkey="ld_const")
                    P.dma("sp", b31[:], rel_bias[31].partition_broadcast(128), semkey="ld_const")
                    P.dma("sp", gates[:], pTM[:, TM_GATE:TM_GATE + 24].rearrange("(j p) c -> p j c", p=128), semkey="ld_const")
                    P.dma("sp", esel[:], consts["c_esel"][:, :], semkey="ld_const")
                    P.dma("sp", ovl[:], consts["c_overlap"].rearrange("(j p) s -> p j s", p=128), semkey="ld_const")
                    P.barrier()
                    P.op("act", lambda e: e.activation(out=e31[:], in_=b31[:], func=AF.Exp), reads=["b31"], writes=["e31"])
                    P.op("act", lambda e: e.activation(out=em31[:], in_=b31[:], func=AF.Exp, scale=-1.0),
                         reads=["b31"], writes=["em31"])
                    P.op("act", lambda e: e.activation(out=gates[:], in_=gates[:], func=AF.Exp, scale=-1.0),
                         reads=["gates"], writes=["gates"])
                    P.op("dve", lambda e: e.tensor_scalar(out=gates[:], in0=gates[:], scalar1=1.0, scalar2=None, op0=ALU.add),
                         reads=["gates"], writes=["gates"])
                    P.op("dve", lambda e: e.reciprocal(out=gates[:], in_=gates[:]), reads=["gates"], writes=["gates"])
                    negmt = P.sbuf("negmt", [128, T], BF16, st)
                    impacc = P.sbuf("impacc", [128, 4, 128], F32, st)
                    sc_t = P.sbuf("sc_t", [128, 128], F32, st)
                    m8 = P.sbuf("m8", [128, 8], F32, st)
                    tadd = [P.sbuf("tadd%d" % i, [128, 128], F32, st) for i in range(2)]
                    qt = [P.sbuf("qtn%d" % i, [64, T], BF16, st) for i in range(2)]
                    qch = [P.sbuf("qch%d" % i, [64, 512], BF16, st) for i in range(4)]
                    kt = P.sbuf("ktn", [64, T], BF16, st)
                    vt = P.sbuf("vtn", [128, NT, 65], BF16, st)
                    P.op("pool", lambda e: e.memset(vt[:, :, 64:65], 1.0), writes=["vt_1"])
                    kvT = P.sbuf("kvT", [64, T], BF16, st)
                    w1f = P.sbuf("w1f", [64, 32, 64], F32, st)
                    w1b = P.sbuf("w1b", [64, 32, 64], BF16, st)
                    w2f = P.sbuf("w2f", [64, 64], F32, st)
                    w2b = P.sbuf("w2b", [64, 64], BF16, st)
                    posf = P.sbuf("posf", [64, 32], F32, st)
                    posb = P.sbuf("posb", [64, 32], BF16, st)
                    cbias = P.sbuf("cbias", [64, 1], F32, st)
                    gx = P.sbuf("gx", [64, 512], F32, st)
                    gy = P.sbuf("gy", [64, 512], F32, st)
                    gT = P.sbuf("gT", [64, 512], BF16, st)
                    kcT = P.sbuf("kcT", [64, 512], BF16, st)
                    vca = P.sbuf("vca", [128, 4, 193], BF16, st)
                    P.op("pool", lambda e: e.memset(vca[:, :, 64:65], 1.0), writes=["vca_1"])
                    P.op("pool", lambda e: e.tensor_copy(out=vca[:, :, 65:193], in_=ovl[:]), reads=["ovl"], writes=["vca_o"])
                    P.op("dve", lambda e: e.memset(gT[:], 0.0), writes=["gT"])

                    def compress(which, g, fm_off):
                        P.dma(None, kvT[:], pT[fm_off + g * 64:fm_off + (g + 1) * 64], writes=["kvT"])
                        P.dma("sp", w1f[:], nsa_w1[l, which].rearrange("(l i) o -> i l o", i=64), writes=["cw"])
                        P.dma("sp", w2f[:], nsa_w2[l, which], writes=["cw"])
                        P.dma("sp", posf[:], nsa_pos[l, which].rearrange("l d -> d l"), writes=["cw"],
                              allow_slow_non_contiguous=True)
                        P.op("dve", lambda e: e.tensor_copy(out=w1b[:], in_=w1f[:]), reads=["cw"], writes=["w1b"])
                        P.op("dve", lambda e: e.tensor_copy(out=w2b[:], in_=w2f[:]), reads=["cw"], writes=["w2b"])
                        P.op("dve", lambda e: e.tensor_copy(out=posb[:], in_=posf[:]), reads=["cw"], writes=["posb"])
                        for li in range(32):
                            P.op("pe", lambda e, li=li: e.matmul(ps_x[0:64, 0:1], lhsT=w1b[:, li, :], rhs=posb[:, li:li + 1],
                                                                 start=(li == 0), stop=(li == 31)),
                                 reads=["w1b", "posb"], writes=["ps_x"])
                        P.op("dve", lambda e: e.tensor_copy(out=cbias[:], in_=ps_x[0:64, 0:1]), reads=["ps_x"], writes=["cbias"])
                        for li in range(32):
                            P.op("pe", lambda e, li=li: e.matmul(
                                ps_x[0:64, 0:ncmp], lhsT=w1b[:, li, :],
                                rhs=kvT[:].rearrange("p (c s) -> p c s", s=16)[:, li // 16:li // 16 + ncmp, li % 16],
                                start=(li == 0), stop=(li == 31)),
                                reads=["w1b", "kvT"], writes=["ps_x"])
                        n = ncmp
                        P.op("act", lambda e: e.activation(out=gx[:, 0:n], in_=ps_x[0:64, 0:n], func=AF.Identity,
                                                           bias=cbias[:], scale=1.0),
                             reads=["ps_x", "cbias"], writes=["gx"])
                        P.op("dve", lambda e: e.tensor_tensor(out=gy[:, 0:n], in0=gx[:, 0:n], in1=gx[:, 0:n], op=ALU.mult),
                             reads=["gx"], writes=["gy"])
                        P.op("dve", lambda e: e.tensor_scalar(out=gy[:, 0:n], in0=gy[:, 0:n], scalar1=0.044715, scalar2=1.0,
                                                              op0=ALU.mult, op1=ALU.add), reads=["gy"], writes=["gy"])
                        P.op("dve", lambda e: e.tensor_tensor(out=gy[:, 0:n], in0=gy[:, 0:n], in1=gx[:, 0:n], op=ALU.mult),
                             reads=["gy", "gx"], writes=["gy"])
                        P.op("act", lambda e: e.activation(out=gy[:, 0:n], in_=gy[:, 0:n], func=AF.Tanh, scale=0.7978845608),
                             reads=["gy"], writes=["gy"])
                        P.op("dve", lambda e: e.tensor_scalar(out=gy[:, 0:n], in0=gy[:, 0:n], scalar1=1.0, scalar2=0.5,
                                                              op0=ALU.add, op1=ALU.mult), reads=["gy"], writes=["gy"])
                        P.op("dve", lambda e: e.tensor_tensor(out=gT[:, 0:n], in0=gy[:, 0:n], in1=gx[:, 0:n], op=ALU.mult),
                             reads=["gy", "gx"], writes=["gT"])
                        if which == 0:
                            P.op("pe", lambda e: e.matmul(ps_x[0:64, 0:512], lhsT=w2b[:, :], rhs=gT[:, :], start=True, stop=True),
                                 reads=["w2b", "gT"], writes=["ps_x"])
                            P.op("act", lambda e: e.copy(out=kcT[:], in_=ps_x[0:64, 0:512]), reads=["ps_x"], writes=["kcT"])
                        else:
                            for cb in range(4):
                                P.op("pe", lambda e, cb=cb: e.matmul(ps_x[:, 0:64], lhsT=gT[:, cb * 128:(cb + 1) * 128],
                                                                     rhs=w2b[:, :], start=True, stop=True),
                                     reads=["w2b", "gT"], writes=["ps_x"])
                                P.op("act", lambda e, cb=cb: e.copy(out=vca[:, cb, 0:64], in_=ps_x[:, 0:64]),
                                     reads=["ps_x"], writes=["vca_v"])

                    for g in range(2):
                        compress(0, g, FM_KC)
                        compress(1, g, FM_VC)
                        vkeys_c = ["vca_v", "vca_1", "vca_o"]
                        for c in range(NG):
                            for r in range(4):
                                h = g * 4 + r
                                if c == 0:
                                    pass
                                qi = R["qi"] = R.get("qi", 0) + 1
                                qc = qch[qi % 4]
                                qk_ = "qch%d" % (qi % 4)
                                P.dma(None, qc[:], pT[FM_NSAQ + h * 64:FM_NSAQ + (h + 1) * 64, c * 512:(c + 1) * 512],
                                      writes=[qk_])
                                need = sorted({4 * c + b - 16 * cb for b in range(4) for cb in range(NCB)
                                               if 0 <= 4 * c + b - 16 * cb <= 17})
                                es = qi % 2
                                slot = {dl: k_ for k_, dl in enumerate(need)}
                                for dl in need:
                                    P.dma(None, ecm[:, es, slot[dl], :], ecmp_ap(h, dl), writes=["ecm%d" % es])

                                def fin(b, pso, h=h, c=c, r=r):
                                    qb = 4 * c + b
                                    P.op("dve", lambda e: e.tensor_scalar(out=R["rd"][:, b:b + 1], in0=pso[:, 64:65],
                                                                          scalar1=1e-30, scalar2=None, op0=ALU.max),
                                         reads=["ps_o%d" % b], writes=["rd%d" % b])
                                    P.op("dve", lambda e: e.reciprocal(out=R["rd"][:, b:b + 1], in_=R["rd"][:, b:b + 1]),
                                         reads=["rd%d" % b], writes=["rd%d" % b])
                                    o2 = R["oi"] % 2
                                    R["oi"] += 1
                                    P.op("dve", lambda e: e.tensor_scalar(
                                        out=R["osb"][o2][:], in0=pso[:, 0:64], scalar1=R["rd"][:, b:b + 1],
                                        scalar2=gates[:, qb, h * 3:h * 3 + 1], op0=ALU.mult, op1=ALU.mult),
                                        reads=["ps_o%d" % b, "rd%d" % b, "gates"], writes=["osb%d" % o2])
                                    P.dma("sp", ocat[qb * 128:(qb + 1) * 128, 1024 + h * 64:1024 + (h + 1) * 64],
                                          R["osb"][o2][:], reads=["osb%d" % o2], semkey="st_osb%d" % o2)
                                    if r == 0:
                                        P.op("dve", lambda e: e.tensor_scalar(
                                            out=impacc[:, b, :], in0=pso[:, 65:193], scalar1=R["rd"][:, b:b + 1],
                                            scalar2=None, op0=ALU.mult),
                                            reads=["ps_o%d" % b, "rd%d" % b], writes=["imp%d" % b])
                                    else:
                                        P.op("dve", lambda e: e.scalar_tensor_tensor(
                                            out=impacc[:, b, :], in0=pso[:, 65:193], scalar=R["rd"][:, b:b + 1],
                                            in1=impacc[:, b, :], op0=ALU.mult, op1=ALU.add),
                                            reads=["ps_o%d" % b, "rd%d" % b, "imp%d" % b], writes=["imp%d" % b])
                                steps = []
                                for cb in range(NCB):
                                    dls = [4 * c + b - 16 * cb for b in range(4)]
                                    if max(dls) < 0:
                                        continue
                                    blocks, bias = {}, None
                                    if min(dls) >= 18:
                                        bias = (b31[:, h:h + 1], "b31")
                                        blocks = {b: ("plain",) for b in range(4)}
                                    else:
                                        for b, dl in enumerate(dls):
                                            if dl < 0:
                                                continue
                                            if dl <= 17:
                                                blocks[b] = ("mul", ecm[:, es, slot[dl], :], "ecm%d" % es)
                                            else:
                                                blocks[b] = ("mulsc", e31[:, h:h + 1], "e31")
                                    steps.append(dict(kl=kcT[:, cb * 128:(cb + 1) * 128], kkeys=["kcT"], bias=bias,
                                                      v=vca[:, cb, :], vkeys=vkeys_c, vn=193, blocks=blocks))
                                run_chunk(R, steps, qc[:, :], [qk_], 0.125, fin)
                            for b in range(4):
                                qb = 4 * c + b
                                ti = qb % 2
                                P.dma("sp", tadd[ti][:], consts["c_topadd"][qb], writes=["tadd%d" % ti])
                                P.op("dve", lambda e, b=b, ti=ti: e.tensor_tensor(out=sc_t[:], in0=impacc[:, b, :],
                                                                                   in1=tadd[ti][:], op=ALU.add),
                                     reads=["imp%d" % b, "tadd%d" % ti], writes=["sc_t"])
                                P.op("dve", lambda e: e.max(out=m8[:], in_=sc_t[:]), reads=["sc_t"], writes=["m8"])
                                P.op("dve", lambda e: e.tensor_scalar(out=sc_t[:], in0=sc_t[:], scalar1=m8[:, 7:8], scalar2=1.0,
                                                                      op0=ALU.is_ge, op1=ALU.subtract),
                                     reads=["sc_t", "m8"], writes=["sc_t"])
                                P.op("pe", lambda e: e.transpose(out=ps_x[:, 0:128], in_=sc_t[:], identity=identf[:]),
                                     reads=["sc_t", "identf"], writes=["ps_x"])
                                P.op("act", lambda e, qb=qb: e.activation(out=negmt[:, qb * 128:(qb + 1) * 128],
                                                                           in_=ps_x[:, 0:128], func=AF.Identity, scale=30000.0),
                                     reads=["ps_x"], writes=["negmt%d" % c])
                        for br, (fm_k, tm_v, dst, gcol_) in enumerate(((FM_KSL, TM_VSL, osel, 1), (FM_KWN, TM_VWN, owin, 2))):
                            P.dma(None, kt[:], pT[fm_k + g * 64:fm_k + (g + 1) * 64], writes=["kt"])
                            load_v(R, vt, "vt", pTMb[:, tm_v + g * 64:tm_v + (g + 1) * 64])
                            for r in range(4):
                                h = g * 4 + r
                                bi = r % 2
                                P.dma(None, qt[bi][:], pT[FM_NSAQ + h * 64:FM_NSAQ + (h + 1) * 64], writes=["qt%d" % bi])

                                def fin(b, pso, h=h, dst=dst, gcol_=gcol_):
                                    qb = fin.c * 4 + b
                                    P.op("dve", lambda e: e.reciprocal(out=R["rd"][:, b:b + 1], in_=pso[:, 64:65]),
                                         reads=["ps_o%d" % b], writes=["rd%d" % b])
                                    o2 = R["oi"] % 2
                                    R["oi"] += 1
                                    P.op("dve", lambda e: e.tensor_scalar(
                                        out=R["osb"][o2][:], in0=pso[:, 0:64], scalar1=R["rd"][:, b:b + 1],
                                        scalar2=gates[:, qb, h * 3 + gcol_:h * 3 + gcol_ + 1], op0=ALU.mult, op1=ALU.mult),
                                        reads=["ps_o%d" % b, "rd%d" % b, "gates"], writes=["osb%d" % o2])
                                    P.dma("sp", dst[qb * 128:(qb + 1) * 128, h * 64:(h + 1) * 64], R["osb"][o2][:],
                                          reads=["osb%d" % o2], semkey="st_osb%d" % o2)
                                for c in range(NG):
                                    fin.c = c
                                    steps = []
                                    if br == 0:
                                        for j in range(4 * c + 4):
                                            blocks = {}
                                            for b in range(4):
                                                dl = 4 * c + b - j
                                                if dl < 0:
                                                    continue
                                                if dl <= 1:
                                                    blocks[b] = ("mul2", em31[:, h:h + 1], "em31", ewn[:, h * 3 + dl, :], "ewn")
                                                else:
                                                    blocks[b] = ("plain",)
                                            steps.append(dict(
                                                kl=kt[:, j * 128:(j + 1) * 128], kkeys=["kt"],
                                                extra=(esel[:, j * 128:(j + 1) * 128], negmt[:, c * 512:(c + 1) * 512],
                                                       ["esel", "negmt%d" % c]),
                                                bias=(b31[:, h:h + 1], "b31"), v=vt[:, j, :], vkeys=["vt", "vt_1"], vn=65,
                                                blocks=blocks))
                                    else:
                                        for j in range(max(0, 4 * c - 2), 4 * c + 4):
                                            blocks = {}
                                            for b in range(4):
                                                dl = 4 * c + b - j
                                                if 0 <= dl <= 2:
                                                    blocks[b] = ("mul", ewn[:, h * 3 + dl, :], "ewn")
                                            steps.append(dict(kl=kt[:, j * 128:(j + 1) * 128], kkeys=["kt"], v=vt[:, j, :],
                                                              vkeys=["vt", "vt_1"], vn=65, blocks=blocks))
                                    run_chunk(R, steps, qt[bi][:, c * 512:(c + 1) * 512], ["qt%d" % bi], 0.125, fin)
                    P.barrier()
                    P.emit()
        if "C" in stages:
            ocat = ocat_keep[0]
            last = (l == n_layers - 1)
            with contextlib.ExitStack() as st:
                ident = P.sbuf("identc", [128, 128], BF16, st)
                P.dma("sp", ident[:], consts["c_ident"][:, :], writes=["ident"])
                gfin = P.sbuf("gfin", [128, D], F32, st)
                if True:
                    P.dma("sp", gfin[:], final_norm.partition_broadcast(128), writes=["gfin"])
                ot = [P.sbuf("ot%d" % i, [128, D], BF16, st) for i in range(2)]
                on = P.sbuf("on", [128, D], BF16, st)
                oadd = [P.sbuf("oadd%d" % i, [128, 512], BF16, st) for i in range(2)]
                junk = P.sbuf("junkc", [128, D], BF16, st)
                hh = P.sbuf("hh", [128, 4, D], F32, st)
                xT = P.sbuf("xT", [128, 16, 512], BF16, st)
                actT = P.sbuf("actT", [128, 64, 512], BF16, st)
                rl = [P.sbuf("rl%d" % i, [128, 512], F32, st) for i in range(2)]
                ss = P.sbuf("ssc", [128, 8], F32, st)
                wsl = [P.sbuf("wslc%d" % i, [128, 16, 512], BF16, st) for i in range(2)]
                ps_t = [P.psum("pc_t%d" % i, [128, 512], BF16, st) for i in range(2)]
                ps_m = [P.psum("pc_m%d" % i, [128, 512], F32, st) for i in range(2)]
                ps_a = [P.psum("pc_a%d" % i, [128, 512], F32, st) for i in range(4)]
                cnt = {"slab": 0, "pt": 0, "pm": 0, "rl": 0}

                def slab(src_ap):
                    i = cnt["slab"] % 2
                    cnt["slab"] += 1
                    P.dma(None, wsl[i][:], src_ap.rearrange("(c p) n -> p c n", p=128), writes=["wsl%d" % i])
                    return i

                def rstd_of(col, w):
                    P.op("dve", lambda e: e.tensor_scalar(out=ss[:, col:col + 1], in0=ss[:, col:col + 1],
                                                          scalar1=1.0 / w, scalar2=EPS, op0=ALU.mult, op1=ALU.add),
                         reads=["ss%d" % col], writes=["ss%d" % col])
                    P.op("act", lambda e: e.activation(out=ss[:, col:col + 1], in_=ss[:, col:col + 1], func=AF.Sqrt),
                         reads=["ss%d" % col], writes=["ss%d" % col])
                    P.op("dve", lambda e: e.reciprocal(out=ss[:, col:col + 1], in_=ss[:, col:col + 1]),
                         reads=["ss%d" % col], writes=["ss%d" % col])

                def transpose_into(src_tile, src_key, tb):
                    for c4 in range(4):
                        j = cnt["pt"] % 2
                        cnt["pt"] += 1
                        for cc in range(4):
                            c = c4 * 4 + cc
                            P.op("pe", lambda e, c=c, cc=cc, j=j: e.transpose(
                                out=ps_t[j][:, cc * 128:(cc + 1) * 128], in_=src_tile[:, c * 128:(c + 1) * 128],
                                identity=ident[:]), reads=[src_key, "ident"], writes=["ps_t%d" % j])
                        P.op("dve", lambda e, c4=c4, j=j: e.tensor_copy(
                            out=xT[:, c4 * 4:(c4 + 1) * 4, tb * 128:(tb + 1) * 128],
                            in_=ps_t[j][:].rearrange("p (c t) -> p c t", c=4)),
                            reads=["ps_t%d" % j], writes=["xT%d" % tb])

                xTk = ["xT%d" % tb for tb in range(4)]
                for g in range(NG):
                    for tb in range(4):
                        t0 = g * 512 + tb * 128
                        i = tb % 2
                        P.dma("sp", ot[i][:], ocat[t0:t0 + 128, :], writes=["ot%d" % i])
                        P.dma("sp", hh[:, tb, :], hsrc[t0:t0 + 128, :], writes=["hh%d" % tb])
                        for bi_, src_ in enumerate((osel, owin)):
                            P.dma("sp", oadd[bi_][:], src_[t0:t0 + 128, :], writes=["oadd%d" % bi_])
                            P.op("pool", lambda e, i=i, bi_=bi_: e.tensor_tensor(
                                out=ot[i][:, 1024:1536], in0=ot[i][:, 1024:1536], in1=oadd[bi_][:], op=ALU.add),
                                reads=["ot%d" % i, "oadd%d" % bi_], writes=["ot%d" % i])
                        for gi in range(4):
                            P.op("act", lambda e, i=i, gi=gi: e.activation(
                                out=junk[:, 0:512], in_=ot[i][:, gi * 512:(gi + 1) * 512], func=AF.Square,
                                accum_out=ss[:, gi:gi + 1]), reads=["ot%d" % i], writes=["junk", "ss%d" % gi])
                            rstd_of(gi, 512)
                            P.op("dve", lambda e, i=i, gi=gi: e.tensor_scalar(
                                out=on[:, gi * 512:(gi + 1) * 512], in0=ot[i][:, gi * 512:(gi + 1) * 512],
                                scalar1=ss[:, gi:gi + 1], scalar2=None, op0=ALU.mult),
                                reads=["ot%d" % i, "ss%d" % gi], writes=["on"])
                        transpose_into(on, "on", tb)
                    for s in range(4):
                        si = slab(wb_out[l][:, s * 512:(s + 1) * 512])
                        for tb in range(4):
                            j = cnt["pm"] % 2
                            cnt["pm"] += 1
                            for kc in range(16):
                                P.op("pe", lambda e, si=si, tb=tb, kc=kc, j=j: e.matmul(
                                    ps_m[j][:], lhsT=xT[:, kc, tb * 128:(tb + 1) * 128], rhs=wsl[si][:, kc, :],
                                    start=(kc == 0), stop=(kc == 15)),
                                    reads=["wsl%d" % si, "xT%d" % tb], writes=["ps_m%d" % j])
                            P.op("dve", lambda e, tb=tb, s=s, j=j: e.tensor_tensor(
                                out=hh[:, tb, s * 512:(s + 1) * 512], in0=hh[:, tb, s * 512:(s + 1) * 512],
                                in1=ps_m[j][:], op=ALU.add), reads=["ps_m%d" % j, "hh%d" % tb], writes=["hh%d" % tb])
                    for tb in range(4):
                        P.op("act", lambda e, tb=tb: e.activation(out=junk[:], in_=hh[:, tb, :], func=AF.Square,
                                                                  accum_out=ss[:, 4:5]),
                             reads=["hh%d" % tb], writes=["junk", "ss4"])
                        rstd_of(4, D)
                        P.op("dve", lambda e, tb=tb: e.tensor_scalar(out=on[:], in0=hh[:, tb, :], scalar1=ss[:, 4:5],
                                                                     scalar2=None, op0=ALU.mult),
                             reads=["hh%d" % tb, "ss4"], writes=["on"])
                        transpose_into(on, "on", tb)
                    for s in range(16):
                        si = slab(wb_up[l][:, s * 512:(s + 1) * 512])
                        for c in range(4):
                            j = cnt["pm"] % 2
                            cnt["pm"] += 1
                            for kc in range(16):
                                P.op("pe", lambda e, si=si, c=c, kc=kc, j=j: e.matmul(
                                    ps_m[j][:], lhsT=wsl[si][:, kc, c * 128:(c + 1) * 128], rhs=xT[:, kc, :],
                                    start=(kc == 0), stop=(kc == 15)),
                                    reads=["wsl%d" % si] + xTk, writes=["ps_m%d" % j])
                            ri = cnt["rl"] % 2
                            cnt["rl"] += 1
                            P.op("act", lambda e, j=j, ri=ri: e.activation(out=rl[ri][:], in_=ps_m[j][:], func=AF.Relu),
                                 reads=["ps_m%d" % j], writes=["rl%d" % ri])
                            P.op("dve" if c % 2 else "pool", lambda e, ri=ri, s=s, c=c: e.tensor_tensor(
                                out=actT[:, s * 4 + c, :], in0=rl[ri][:], in1=rl[ri][:], op=ALU.mult),
                                reads=["rl%d" % ri], writes=["actT%d" % (s * 4 + c)])
                    for s in range(4):
                        for kq in range(4):
                            si = slab(wb_dn[l][kq * 2048:(kq + 1) * 2048, s * 512:(s + 1) * 512])
                            for tb in range(4):
                                for kc in range(16):
                                    kk = kq * 16 + kc
                                    P.op("pe", lambda e, si=si, tb=tb, kc=kc, kk=kk: e.matmul(
                                        ps_a[tb][:], lhsT=actT[:, kk, tb * 128:(tb + 1) * 128], rhs=wsl[si][:, kc, :],
                                        start=(kk == 0), stop=(kk == 63)),
                                        reads=["wsl%d" % si, "actT%d" % kk], writes=["ps_a%d" % tb])
                        for tb in range(4):
                            P.op("dve", lambda e, tb=tb, s=s: e.tensor_tensor(
                                out=hh[:, tb, s * 512:(s + 1) * 512], in0=hh[:, tb, s * 512:(s + 1) * 512],
                                in1=ps_a[tb][:], op=ALU.add), reads=["ps_a%d" % tb, "hh%d" % tb], writes=["hh%d" % tb])
                    for tb in range(4):
                        t0 = g * 512 + tb * 128
                        if fused and not last:
                            P.dma("pool", hres[t0:t0 + 128, :], hh[:, tb, :], reads=["hh%d" % tb], semkey="st_hh%d" % tb)
                        if not fused:
                            P.dma("pool", hn_out[t0:t0 + 128, :], hh[:, tb, :], reads=["hh%d" % tb], semkey="st_hh%d" % tb)
                        if last or not fused:
                            P.op("act", lambda e, tb=tb: e.activation(out=junk[:], in_=hh[:, tb, :], func=AF.Square,
                                                                      accum_out=ss[:, 5:6]),
                                 reads=["hh%d" % tb], writes=["junk", "ss5"])
                            rstd_of(5, D)
                            P.op("dve", lambda e, tb=tb: e.tensor_scalar(out=hh[:, tb, :], in0=hh[:, tb, :],
                                                                         scalar1=ss[:, 5:6], scalar2=None, op0=ALU.mult),
                                 reads=["hh%d" % tb, "ss5"], writes=["hh%d" % tb])
                            P.op("pool", lambda e, tb=tb: e.tensor_tensor(out=hh[:, tb, :], in0=hh[:, tb, :],
                                                                          in1=gfin[:], op=ALU.mult),
                                 reads=["hh%d" % tb, "gfin"], writes=["hh%d" % tb])
                            P.dma("pool", y_out[t0:t0 + 128, :], hh[:, tb, :], reads=["hh%d" % tb], semkey="st_hy%d" % tb)
                P.barrier()
                P.emit()
    P.barrier()
    P.emit()
    dbg["_max_sem_count"] = P.max_sem_count()
    dbg["_n_sems"] = len(P.sems)
    P.close()
    return nc, dbg


IN_SPLITS = (384, 128, 32, 512, 512, 512, 8, 512, 128, 128, 128, 128, 128, 128, 24, 512, 128, 128)
IN_OFF = np.concatenate([[0], np.cumsum(IN_SPLITS)]).astype(int)


def relayout_weights(w_in, w_ukv):
    L = w_in.shape[0]
    seg = lambda i: w_in[:, :, IN_OFF[i]:IN_OFF[i + 1]]
    w_fm = np.zeros((L, D, NFM), np.float32)
    w_tm = np.zeros((L, D, NTM), np.float32)
    for off, i in ((FM_FOXQ, 3), (FM_FOXK, 4), (FM_NSAQ, 7), (FM_SWAQ, 15), (FM_KC, 8), (FM_VC, 9),
                   (FM_KSL, 10), (FM_KWN, 12), (FM_SWAK, 16), (FM_F, 6)):
        s = seg(i)
        w_fm[:, :, off:off + s.shape[2]] = s
    for off, i in ((TM_CQ, 0), (TM_CKV, 1), (TM_KPE, 2), (TM_FOXV, 5), (TM_VSL, 11), (TM_VWN, 13),
                   (TM_GATE, 14), (TM_SWAV, 17)):
        s = seg(i)
        w_tm[:, :, off:off + s.shape[2]] = s
    kv = w_ukv.reshape(L, 128, 8, 128)
    w_k = np.ascontiguousarray(kv[:, :, :, :64].reshape(L, 128, 512))
    w_v = np.ascontiguousarray(kv[:, :, :, 64:].reshape(L, 128, 512))
    return w_fm, w_tm, w_k, w_v


FUSED = True
L1_IO = dict(pT="out", pTM="out", pTMb="out", ocat="out")
L2_IO = dict(pT="in", pTM="in", pTMb="in", ocat="in")


def build_programs(T):
    nc1, i1 = build(T, stages="AB", io=L1_IO)
    nc2, i2 = build(T, stages="SNC", io=L2_IO)
    return (nc1, i1["_inputs"]), (nc2, i2["_inputs"])


def run_layer(progs, h, W, l, T, last):
    (nc1, in1), (nc2, in2) = progs
    B = h.shape[0]
    base = layer_inputs(None, W, l, T)
    maps1 = [dict({k: base[k] for k in in1 if k != "x"}, x=np.ascontiguousarray(h[b])) for b in range(B)]
    r1 = run_bass_kernel_spmd(nc1, maps1, core_ids=list(range(B))).results
    maps2 = []
    for b in range(B):
        m = {k: base[k] for k in in2 if k in base}
        m["x"] = np.ascontiguousarray(h[b])
        m["pT"], m["pTM"], m["pTMb"] = r1[b]["pT"], r1[b]["pTM"], r1[b]["pTMb"]
        m["ocat_in"] = r1[b]["ocat"]
        maps2.append(m)
    r2 = run_bass_kernel_spmd(nc2, maps2, core_ids=list(range(B))).results
    key = "y" if last else "hn"
    return np.stack([np.asarray(r2[b][key]) for b in range(B)], axis=0)


def kernel(x, norm_attn, w_in, mla_q_norm, mla_w_uq, mla_kv_norm, mla_w_ukv, fox_b_f,
           nsa_cmp_pos, nsa_cmp_w1, nsa_cmp_w2, swa_sinks, group_norm, w_out,
           norm_mlp, w_up, w_down, rel_bias, final_norm):
    W = dict(norm_attn=norm_attn, w_in=w_in, mla_q_norm=mla_q_norm, mla_w_uq=mla_w_uq,
             mla_kv_norm=mla_kv_norm, mla_w_ukv=mla_w_ukv, fox_b_f=fox_b_f, nsa_cmp_pos=nsa_cmp_pos,
             nsa_cmp_w1=nsa_cmp_w1, nsa_cmp_w2=nsa_cmp_w2, swa_sinks=swa_sinks, group_norm=group_norm,
             w_out=w_out, norm_mlp=norm_mlp, w_up=w_up, w_down=w_down, rel_bias=rel_bias,
             final_norm=final_norm)
    W = {k: np.asarray(v) for k, v in W.items()}
    h = np.asarray(x, dtype=np.float32)
    B, T, _ = h.shape
    depth = W["w_in"].shape[0]
    if FUSED:
        nc, info = build(T, n_layers=depth, stages="ABSNC")
        base = layer_inputs(None, W, 0, T, nl=depth)
        maps = [dict({k: base[k] for k in info["_inputs"] if k != "x"}, x=np.ascontiguousarray(h[b]))
                for b in range(B)]
        res = run_bass_kernel_spmd(nc, maps, core_ids=list(range(B))).results
        return np.stack([np.asarray(res[b]["y"]) for b in range(B)], axis=0).astype(np.float32)
    progs = build_programs(T)
    for l in range(depth):
        h = run_layer(progs, h, W, l, T, last=(l == depth - 1))
    return h.astype(np.float32)


def layer_inputs(h, W, l, T, nl=1):
    w_fm, w_tm, w_k, w_v = relayout_weights(np.asarray(W["w_in"][l:l + nl]), np.asarray(W["mla_w_ukv"][l:l + nl]))
    m = dict(w_fm=w_fm, w_tm=w_tm, w_ukv_k=w_k, w_ukv_v=w_v)
    if h is not None:
        m["x"] = np.ascontiguousarray(h, dtype=np.float32)
    for k in ("norm_attn", "mla_q_norm", "mla_w_uq", "mla_kv_norm", "fox_b_f", "group_norm", "w_out", "norm_mlp",
              "w_up", "w_down", "nsa_cmp_pos", "nsa_cmp_w1", "nsa_cmp_w2", "swa_sinks"):
        m[k] = np.ascontiguousarray(np.asarray(W[k])[l:l + nl], dtype=np.float32)
    m["rel_bias"] = np.asarray(W["rel_bias"], np.float32)
    m["final_norm"] = np.asarray(W["final_norm"], np.float32)
    m.update(host_consts(T))
    return m
```
